# Optimizing a Trainium2 kernel written in Bass

```python
import jax, jax.numpy as jnp
from jax import lax
import numpy as np

D_MODEL = 1024
BATCH = 2
SEQ = 8192
DEPTH = 1

GDN_HEADS = 4
GDN_DK = 128
GDN_DV = 128
HG_HEADS = 4
HG_DK = 128
HG_DV = 128
GDN_QK = GDN_HEADS * GDN_DK
GDN_V = GDN_HEADS * GDN_DV
HG_K = HG_HEADS * HG_DK
HG_V = HG_HEADS * HG_DV
D_MIX = GDN_V + HG_V
D_IN = 2 * GDN_QK + 2 * GDN_V + 2 * GDN_HEADS + 2 * HG_K + 2 * HG_V
CONV_K = 4
CHUNK = 64
N_EXPERTS = 64
N_GROUPS = 8
TOPK_GROUPS = 4
TOP_K = 8
D_EXPERT = 256
D_SHARED = 256
ROUTE_SCALE = 2.5
MOE_BLOCK = 128
EPS = 1e-6

kernel_name = "hybrid_gdn_hgrn2_moe_adaln"


def rms_norm(x, w):
    xf = x.astype(jnp.float32)
    y = xf * lax.rsqrt(jnp.mean(xf * xf, axis=-1, keepdims=True) + EPS)
    return (y * w.astype(jnp.float32)).astype(x.dtype)


def l2_normalize(x):
    return x * lax.rsqrt(jnp.sum(x * x, axis=-1, keepdims=True) + EPS)


def causal_short_conv(x, w):
    T = x.shape[1]
    xp = jnp.pad(x, ((0, 0), (CONV_K - 1, 0), (0, 0)))
    y = sum(xp[:, j:j + T, :] * w[j] for j in range(CONV_K))
    return jax.nn.silu(y)


def to_chunks(t):
    B, T, H, D = t.shape
    return t.reshape(B, T // CHUNK, CHUNK, H, D).transpose(0, 3, 1, 2, 4)


def from_chunks(o):
    N, B, H, C, D = o.shape
    return o.transpose(1, 0, 3, 2, 4).reshape(B, N * C, H, D)


def gated_delta_chunked(q, k, v, g, beta):
    B, T, H, DK = q.shape
    DV = v.shape[-1]
    N = T // CHUNK
    qc, kc, vc = to_chunks(q), to_chunks(k), to_chunks(v)
    gc = jnp.cumsum(g.reshape(B, N, CHUNK, H).transpose(0, 3, 1, 2), axis=-1)
    bc = beta.reshape(B, N, CHUNK, H).transpose(0, 3, 1, 2)
    idx = jnp.arange(CHUNK)
    causal = idx[:, None] >= idx[None, :]
    strict = idx[:, None] > idx[None, :]
    decay = jnp.exp(jnp.where(causal, gc[..., :, None] - gc[..., None, :], -jnp.inf))
    kb = kc * bc[..., None]
    L = jnp.where(strict, jnp.einsum('bhncd,bhnsd->bhncs', kb, kc) * decay, 0.0)
    rhs = jnp.concatenate([vc * bc[..., None], kb * jnp.exp(gc)[..., None]], axis=-1)
    sol = lax.linalg.triangular_solve(L + jnp.eye(CHUNK, dtype=L.dtype), rhs,
                                      left_side=True, lower=True, unit_diagonal=True)
    u, w = sol[..., :DV], sol[..., DV:]
    attn = jnp.einsum('bhncd,bhnsd->bhncs', qc, kc) * decay
    q_dec = qc * jnp.exp(gc)[..., None]
    g_last = gc[..., -1]
    k_tail = kc * jnp.exp(g_last[..., None] - gc)[..., None]

    def step(S, xs):
        u_n, w_n, a_n, qd_n, kt_n, gl_n = xs
        v_new = u_n - jnp.einsum('bhcd,bhdv->bhcv', w_n, S)
        o = jnp.einsum('bhcd,bhdv->bhcv', qd_n, S) + jnp.einsum('bhcs,bhsv->bhcv', a_n, v_new)
        S = S * jnp.exp(gl_n)[..., None, None] + jnp.einsum('bhcd,bhcv->bhdv', kt_n, v_new)
        return S, o

    xs = tuple(jnp.moveaxis(t, 2, 0) for t in (u, w, attn, q_dec, k_tail, g_last))
    S0 = jnp.zeros((B, H, DK, DV), q.dtype)
    _, o = lax.scan(step, S0, xs)
    return from_chunks(o)


def hgrn2_chunked(q, k, v, logf):
    B, T, H, DK = q.shape
    DV = v.shape[-1]
    qc, kc, vc = to_chunks(q), to_chunks(k), to_chunks(v)
    bc = jnp.cumsum(to_chunks(logf), axis=3)
    idx = jnp.arange(CHUNK)
    causal = (idx[:, None] >= idx[None, :])[:, :, None]

    def step(S, xs):
        q_n, k_n, v_n, b_n = xs
        decay = jnp.exp(jnp.where(causal, b_n[..., :, None, :] - b_n[..., None, :, :], -jnp.inf))
        A = jnp.einsum('bhtd,bhsd,bhtsd->bhts', q_n, k_n, decay)
        b_last = b_n[..., -1:, :]
        o = jnp.einsum('bhts,bhsv->bhtv', A, v_n) + jnp.einsum('bhtd,bhdv->bhtv', q_n * jnp.exp(b_n), S)
        S = jnp.exp(b_last[..., 0, :])[..., :, None] * S + jnp.einsum('bhsd,bhsv->bhdv', k_n * jnp.exp(b_last - b_n), v_n)
        return S, o

    xs = tuple(jnp.moveaxis(t, 2, 0) for t in (qc, kc, vc, bc))
    S0 = jnp.zeros((B, H, DK, DV), q.dtype)
    _, o = lax.scan(step, S0, xs)
    return from_chunks(o)


def token_mixer(h, w_in, conv_w, a_log, dt_bias, gdn_norm_w, lb, hg_norm_w, w_out):
    B, T, _ = h.shape
    f32 = jnp.float32
    proj = jnp.einsum('btd,de->bte', h, w_in).astype(f32)
    qkv_w = 2 * GDN_QK + GDN_V
    qkv = causal_short_conv(proj[..., :qkv_w], conv_w.astype(f32))
    qA, kA, vA = jnp.split(qkv, [GDN_QK, 2 * GDN_QK], axis=-1)
    sizes = (GDN_V, GDN_HEADS, GDN_HEADS, HG_K, HG_V, HG_K, HG_V)
    cuts = np.cumsum(sizes)[:-1].tolist()
    gateA, beta_raw, a_raw, f_raw, iB, qB, gateB = jnp.split(proj[..., qkv_w:], cuts, axis=-1)

    qA = l2_normalize(qA.reshape(B, T, GDN_HEADS, GDN_DK)) * (GDN_DK ** -0.5)
    kA = l2_normalize(kA.reshape(B, T, GDN_HEADS, GDN_DK))
    vA = vA.reshape(B, T, GDN_HEADS, GDN_DV)
    beta = jax.nn.sigmoid(beta_raw)
    g_log = -jnp.exp(a_log.astype(f32)) * jax.nn.softplus(a_raw + dt_bias.astype(f32))
    oA = gated_delta_chunked(qA, kA, vA, g_log, beta)
    oA = rms_norm(oA, gdn_norm_w) * jax.nn.silu(gateA.reshape(B, T, GDN_HEADS, GDN_DV))
    oA = oA.reshape(B, T, GDN_V)

    lb = lb.reshape(HG_HEADS, HG_DK)
    f_raw = f_raw.reshape(B, T, HG_HEADS, HG_DK)
    logf = jnp.log(lb + (1.0 - lb) * jax.nn.sigmoid(f_raw))
    kB = (1.0 - lb) * jax.nn.sigmoid(-f_raw)
    qB = jax.nn.silu(qB.reshape(B, T, HG_HEADS, HG_DK))
    oB = hgrn2_chunked(qB, kB, iB.reshape(B, T, HG_HEADS, HG_DV), logf).reshape(B, T, HG_V)
    oB = rms_norm(oB, hg_norm_w) * jax.nn.silu(gateB)

    o = jnp.concatenate([oA, oB], axis=-1).astype(h.dtype)
    return jnp.einsum('bte,ed->btd', o, w_out)


def moe(h, w_router, router_bias, w_gate, w_up, w_down, ws_gate, ws_up, ws_down):
    B, T, D = h.shape
    xt = h.reshape(-1, D)
    M = xt.shape[0]
    scores = jax.nn.sigmoid(xt.astype(jnp.float32) @ w_router.astype(jnp.float32))
    sel = scores + router_bias.astype(jnp.float32)
    group_score = lax.top_k(sel.reshape(M, N_GROUPS, N_EXPERTS // N_GROUPS), 2)[0].sum(-1)
    _, gidx = lax.top_k(group_score, TOPK_GROUPS)
    gmask = jax.nn.one_hot(gidx, N_GROUPS, dtype=jnp.float32).sum(-2) > 0
    emask = jnp.repeat(gmask, N_EXPERTS // N_GROUPS, axis=-1)
    _, eidx = lax.top_k(jnp.where(emask, sel, -jnp.inf), TOP_K)
    wts = jnp.take_along_axis(scores, eidx, axis=-1)
    wts = wts / jnp.sum(wts, axis=-1, keepdims=True) * ROUTE_SCALE
    gates = jnp.einsum('mk,mke->me', wts, jax.nn.one_hot(eidx, N_EXPERTS, dtype=wts.dtype)).astype(h.dtype)

    def expert_block(args):
        x_b, g_b = args
        hg = jnp.einsum('td,edf->tef', x_b, w_gate)
        hu = jnp.einsum('td,edf->tef', x_b, w_up)
        return jnp.einsum('tef,efd->td', jax.nn.silu(hg) * hu * g_b[..., None], w_down)

    routed = lax.map(expert_block, (xt.reshape(-1, MOE_BLOCK, D), gates.reshape(-1, MOE_BLOCK, N_EXPERTS)))
    shared = (jax.nn.silu(xt @ ws_gate) * (xt @ ws_up)) @ ws_down
    return (routed.reshape(M, D) + shared).reshape(B, T, D)


def setup_inputs(seed: int = 0) -> dict:
    key = jax.random.key(seed)
    ks = jax.random.split(key, 24)
    f32 = jnp.float32
    nrm = lambda k, s, sc: jax.random.normal(k, s, f32) * sc
    dt = jnp.exp(jax.random.uniform(ks[8], (DEPTH, GDN_HEADS), f32, np.log(1e-3), np.log(1e-1)))
    return {
        "x": nrm(ks[0], (BATCH, SEQ, D_MODEL), 1.0),
        "c": nrm(ks[1], (BATCH, D_MODEL), 1.0),
        "w_ada": nrm(ks[2], (DEPTH, D_MODEL, 6 * D_MODEL), 0.5 * D_MODEL ** -0.5),
        "b_ada": nrm(ks[3], (DEPTH, 6 * D_MODEL), 0.01),
        "norm1_w": 1.0 + nrm(ks[4], (DEPTH, D_MODEL), 0.02),
        "w_in": nrm(ks[5], (DEPTH, D_MODEL, D_IN), D_MODEL ** -0.5),
        "conv_w": nrm(ks[6], (DEPTH, CONV_K, 2 * GDN_QK + GDN_V), CONV_K ** -0.5),
        "gdn_a_log": jnp.log(jax.random.uniform(ks[7], (DEPTH, GDN_HEADS), f32, 1.0, 16.0)),
        "gdn_dt_bias": dt + jnp.log(-jnp.expm1(-dt)),
        "gdn_norm_w": 1.0 + nrm(ks[9], (DEPTH, GDN_DV), 0.02),
        "hg_lb": nrm(ks[10], (DEPTH + 1, HG_K), 0.1),
        "hg_norm_w": 1.0 + nrm(ks[11], (DEPTH, HG_V), 0.02),
        "w_out": nrm(ks[12], (DEPTH, D_MIX, D_MODEL), D_MIX ** -0.5),
        "norm2_w": 1.0 + nrm(ks[13], (DEPTH, D_MODEL), 0.02),
        "w_router": nrm(ks[14], (DEPTH, D_MODEL, N_EXPERTS), D_MODEL ** -0.5),
        "router_bias": nrm(ks[15], (DEPTH, N_EXPERTS), 0.01),
        "w_gate": nrm(ks[16], (DEPTH, N_EXPERTS, D_MODEL, D_EXPERT), D_MODEL ** -0.5),
        "w_up": nrm(ks[17], (DEPTH, N_EXPERTS, D_MODEL, D_EXPERT), D_MODEL ** -0.5),
        "w_down": nrm(ks[18], (DEPTH, N_EXPERTS, D_EXPERT, D_MODEL), D_EXPERT ** -0.5),
        "ws_gate": nrm(ks[19], (DEPTH, D_MODEL, D_SHARED), D_MODEL ** -0.5),
        "ws_up": nrm(ks[20], (DEPTH, D_MODEL, D_SHARED), D_MODEL ** -0.5),
        "ws_down": nrm(ks[21], (DEPTH, D_SHARED, D_MODEL), D_SHARED ** -0.5),
        "final_norm_w": 1.0 + nrm(ks[22], (D_MODEL,), 0.02),
    }


def reference(x, c, w_ada, b_ada, norm1_w, w_in, conv_w, gdn_a_log, gdn_dt_bias, gdn_norm_w,
              hg_lb, hg_norm_w, w_out, norm2_w, w_router, router_bias, w_gate, w_up, w_down,
              ws_gate, ws_up, ws_down, final_norm_w):
    lb_all = jnp.cumsum(jax.nn.softmax(hg_lb.astype(jnp.float32), axis=0), axis=0)
    c_act = jax.nn.silu(c)
    for l in range(DEPTH):
        mod = jnp.einsum('bd,de->be', c_act, w_ada[l]) + b_ada[l]
        sh1, sc1, g1, sh2, sc2, g2 = jnp.split(mod, 6, axis=-1)
        h = rms_norm(x, norm1_w[l]) * (1.0 + sc1[:, None]) + sh1[:, None]
        x = x + g1[:, None] * token_mixer(h, w_in[l], conv_w[l], gdn_a_log[l], gdn_dt_bias[l],
                                          gdn_norm_w[l], lb_all[l], hg_norm_w[l], w_out[l])
        h = rms_norm(x, norm2_w[l]) * (1.0 + sc2[:, None]) + sh2[:, None]
        x = x + g2[:, None] * moe(h, w_router[l], router_bias[l], w_gate[l], w_up[l], w_down[l],
                                  ws_gate[l], ws_up[l], ws_down[l])
    return rms_norm(x, final_norm_w)
```

```python
from contextlib import ExitStack
import types
import numpy as np
import concourse.bass as bass
import concourse.mybir as mybir
from concourse.bass_utils import run_bass_kernel_spmd

F32 = mybir.dt.float32
BF16 = mybir.dt.bfloat16
AF = mybir.ActivationFunctionType
ALU = mybir.AluOpType
AX = mybir.AxisListType

D = 1024
T = 8192
TS = 2
NW = TS * 128
NT = 64
OWN0 = 48
NOWN = 16
DIN = 4104
OQ, OK_, OV, OGA, OBETA, OA, OF, OI, OQB, OGB = 0, 512, 1024, 1536, 2048, 2052, 2056, 2568, 3080, 3592
EPS = 1e-6
NE = 64
SAME_ENGINE_SYNC = True
import os
SUB = float(os.environ.get("SUB", "9"))


def _snap(fn):
    if fn is None or not getattr(fn, "__closure__", None):
        return fn
    cells = []
    for c in fn.__closure__:
        try:
            cells.append(types.CellType(c.cell_contents))
        except ValueError:
            cells.append(c)
    return types.FunctionType(fn.__code__, fn.__globals__, fn.__name__, fn.__defaults__, tuple(cells))


class Sched:
    ENGS = ("pe", "dve", "act", "pool", "sp")
    BLK = {"pe": "tensor", "dve": "vector", "act": "scalar", "pool": "gpsimd", "sp": "sync"}

    def __init__(self, nc, stack):
        self.nc = nc
        self.ins = {e: [] for e in self.ENGS}
        self.done = {e: 0 for e in self.ENGS}
        self.lastw = {}
        self.readers = {}
        self.dma_cnt = {}
        self.esem = {e: stack.enter_context(nc.semaphore("c_" + e)) for e in self.ENGS}
        self.dsem = {}
        self.stack = stack
        self.cbase = {e: 0 for e in self.ENGS}
        self.cval = {e: {} for e in self.ENGS}
        self.seen = {e: {} for e in self.ENGS}
        self.bar_waits = set()
        self.bar_pending = set()

    def op(self, eng, fn, reads=(), writes=(), dma=None):
        ex = [k for k in reads if isinstance(k, tuple) and k[0] == "bank"]
        if ex:
            reads = [k for k in reads if k not in ex]
            writes = list(writes) + ex
        waits = set()
        for k in reads:
            t = self.lastw.get(k)
            if t is not None:
                waits.add(t)
        for k in writes:
            t = self.lastw.get(k)
            if t is not None:
                waits.add(t)
            for r in self.readers.get(k, ()):
                waits.add(r)
        idx = self.done[eng] + len(self.ins[eng])
        if dma is not None:
            if dma not in self.dsem:
                self.dsem[dma] = self.stack.enter_context(self.nc.semaphore("d_" + dma))
            self.dma_cnt[dma] = self.dma_cnt.get(dma, 0) + 16
            tok = ("d", dma, self.dma_cnt[dma])
        else:
            tok = ("e", eng, idx)
        w2 = set()
        for w in waits:
            if w[0] == "e" and w[2] < self.done[w[1]]:
                continue
            if w[0] == "e" and w[1] == eng and (eng == "pe" or not SAME_ENGINE_SYNC):
                continue
            w2.add(w)
        if eng in self.bar_pending:
            self.bar_pending.discard(eng)
            w2 |= self.bar_waits
        self.ins[eng].append(dict(fn=_snap(fn), waits=w2, tok=tok, dma=dma, idx=idx))
        for k in reads:
            self.readers.setdefault(k, []).append(tok)
        for k in writes:
            self.lastw[k] = tok
            self.readers[k] = []
        return tok

    def emit(self):
        nc = self.nc
        flagged = {e: set() for e in self.ENGS}
        for e in self.ENGS:
            for ins in self.ins[e]:
                for w in ins["waits"]:
                    if w[0] == "e" and w[2] >= self.done[w[1]]:
                        flagged[w[1]].add(w[2])
        lastc = {}
        for e in self.ENGS:
            for ins in self.ins[e]:
                if ins["dma"] is None and ins["fn"] is not None:
                    lastc[e] = ins["idx"]
            if e in lastc:
                flagged[e].add(lastc[e])
        for e in self.ENGS:
            c = self.cbase[e]
            for ins in self.ins[e]:
                if ins["idx"] in flagged[e]:
                    c += 1
                    self.cval[e][ins["idx"]] = c
            self.cbase[e] = c
        with nc.Block() as block:
            for e in self.ENGS:
                lst = self.ins[e]

                def body(engine, e=e, lst=lst):
                    seen = self.seen[e]
                    for ins in lst:
                        need = {}
                        for w in ins["waits"]:
                            if w[0] == "e":
                                key = ("e", w[1])
                                val = self.cval[w[1]][w[2]]
                            else:
                                key = ("d", w[1])
                                val = w[2]
                            need[key] = max(need.get(key, 0), val)
                        for key, val in need.items():
                            if seen.get(key, 0) >= val:
                                continue
                            seen[key] = val
                            sem = self.esem[key[1]] if key[0] == "e" else self.dsem[key[1]]
                            engine.wait_ge(sem, val)
                        if ins["fn"] is None:
                            continue
                        r = ins["fn"](engine)
                        if ins["dma"] is not None:
                            r.then_inc(self.dsem[ins["dma"]], 16)
                        elif ins["idx"] in flagged[e]:
                            r.then_inc(self.esem[e], 1)

                getattr(block, self.BLK[e])(body)
        bw = set()
        for e in self.ENGS:
            self.last_idx = getattr(self, "last_idx", {})
            if e in lastc:
                self.last_idx[e] = lastc[e]
            self.done[e] += len(self.ins[e])
            self.ins[e] = []
        for e, i in getattr(self, "last_idx", {}).items():
            bw.add(("e", e, i))
        for nm, v in self.dma_cnt.items():
            bw.add(("d", nm, v))
        self.bar_waits = bw
        self.bar_pending = set(self.ENGS)


F32R = mybir.dt.float32r
USE_F32R = os.environ.get("NO_F32R") is None
USE_TRP = os.environ.get("NO_TRP") is None


def mmr(e, out, lhsT, rhs, start=True, stop=True):
    if USE_F32R:
        return e.matmul(out, lhsT.bitcast(F32R), rhs.bitcast(F32R), start=start, stop=stop)
    return e.matmul(out, lhsT, rhs, start=start, stop=stop)


def rr(ap):
    return ap.bitcast(F32R) if USE_F32R else ap


R_CHAIN = USE_F32R and os.environ.get("NO_F32R_CHAIN") is None
R_CUM = USE_F32R and os.environ.get("NO_F32R_CUM") is None


def rc(ap):
    return ap.bitcast(F32R) if R_CHAIN else ap


def ru(ap):
    return ap.bitcast(F32R) if R_CUM else ap


CH_MASK = int(os.environ.get("CH_MASK", "14"))


def mm_c(e, out, lhsT, rhs, bit=0):
    if R_CHAIN and (CH_MASK & bit):
        return e.matmul(out, lhsT.bitcast(F32R), rhs.bitcast(F32R), start=True, stop=True)
    return e.matmul(out, lhsT, rhs, start=True, stop=True)


def mm_u(e, out, lhsT, rhs):
    if R_CUM:
        return e.matmul(out, lhsT.bitcast(F32R), rhs.bitcast(F32R), start=True, stop=True)
    return e.matmul(out, lhsT, rhs, start=True, stop=True)


def make_consts():
    c = {}
    p = np.arange(128)
    same = (p[:, None] // 64) == (p[None, :] // 64)
    c["ident"] = np.eye(128, dtype=np.float32)
    c["triu"] = (same & (p[:, None] <= p[None, :])).astype(np.float32)
    c["rev"] = (same & (p[:, None] > p[None, :])).astype(np.float32)
    c["blk"] = same.astype(np.float32)
    c["strictl"] = (same & (p[:, None] > p[None, :])).astype(np.float32)
    c["ones"] = np.ones((128, 128), np.float32)
    ci = np.zeros((128, 128), np.float32)
    ci[:64, 0] = 1.0
    ci[64:, 1] = 1.0
    c["cind"] = ci
    names = ["ident", "triu", "rev", "blk", "strictl", "ones", "cind"]
    arr = np.concatenate([c[n] for n in names], axis=1)
    offs = {n: i * 128 for i, n in enumerate(names)}
    return arr, offs


CONST_ARR, COFF = make_consts()
NCONST = CONST_ARR.shape[1]


def build(debug=(), stage=9, nsi=NT // TS, nex=NE + 1, dne=NE):
    nc = bass.Bass("TRN2", target_bir_lowering=False)
    dbg_outs = {}

    def din(name, shape):
        return nc.dram_tensor(name, list(shape), F32, kind="ExternalInput").ap()

    xseq = din("xseq", [T, D])
    tmask = din("tmask", [128, NT])
    cvec = din("cvec", [128, 8])
    w_ada = din("w_ada", [D, 6 * D])
    b_ada = din("b_ada", [6 * D])
    norm1_w = din("norm1_w", [D])
    w_in = din("w_in", [D, DIN])
    convw = din("convw", [128, 48])
    a_log = din("a_log", [4])
    dt_bias = din("dt_bias", [4])
    gdn_nw = din("gdn_nw", [128])
    lbcol = din("lbcol", [128, 8])
    hg_lb = din("hg_lb", [2, 512])
    hg_nw = din("hg_nw", [512])
    w_out = din("w_out", [D, D])
    norm2_w = din("norm2_w", [D])
    w_router = din("w_router", [D, NE])
    router_bias = din("router_bias", [NE])
    w_gate = din("w_gate", [dne, D, 256])
    w_up = din("w_up", [dne, D, 256])
    w_down = din("w_down", [dne, 256, D])
    ws_gate = din("ws_gate", [D, 256])
    ws_up = din("ws_up", [D, 256])
    ws_down = din("ws_down", [256, D])
    final_w = din("final_w", [D])
    consts = din("consts", [128, NCONST])
    y = nc.dram_tensor("y", [NOWN * 128, D], F32, kind="ExternalOutput").ap()
    x1scr = nc.dram_tensor("x1scr", [NOWN * 128, D], F32)
    win_bf = nc.dram_tensor("win_bf", [D, DIN], BF16)
    wout_bf = nc.dram_tensor("wout_bf", [D, D], BF16)
    wg_bf = nc.dram_tensor("wg_bf", [NE + 1, D, 256], BF16)
    wu_bf = nc.dram_tensor("wu_bf", [NE + 1, D, 256], BF16)
    wd_bf = nc.dram_tensor("wd_bf", [NE + 1, 256, D], BF16)
    h2scr = nc.dram_tensor("h2scr", [NOWN, 128, 8 * 128], BF16)

    top = ExitStack()
    with top:
        S = Sched(nc, top)

        def sb(stack, name, shape, dt=F32):
            return stack.enter_context(nc.sbuf_tensor(name, list(shape), dt))

        banks = [top.enter_context(nc.psum_tensor("bank%d" % i, [128, 512], F32)) for i in range(8)]
        full_rr = [0]
        q_rr = [0]

        def pfull():
            i = full_rr[0] % 3
            full_rr[0] += 1
            return banks[i], ("bank", i)

        def pq():
            i = q_rr[0] % 4
            q_rr[0] += 1
            return banks[4 + i][:, 0:128], ("bank", 4 + i)

        tbank_bf = banks[3][:, :].bitcast(BF16)
        TB = ("bank", 3)

        cst = sb(top, "cst", [128, NCONST])
        cst_bf = sb(top, "cst_bf", [128, 128], BF16)
        modbc = sb(top, "modbc", [128, 6 * D])
        fwbc = sb(top, "fwbc", [128, D])
        gates = sb(top, "gates", [128, NOWN, NE + 1])
        tmk = sb(top, "tmk", [128, NT])
        one_col = sb(top, "one_col", [128, 1])

        def C(n):
            return cst[:, COFF[n]:COFF[n] + 128]

        cstr = sb(top, "cstr", [128, NCONST])

        def CR(n):
            return cstr[:, COFF[n]:COFF[n] + 128]

        def trp(e, out, in_):
            if USE_TRP:
                return e.transpose(out=out, in_=in_, identity=C("ident"))
            return e.matmul(out, in_, C("ident"), start=True, stop=True)

        dma_i = [0]

        def dma_load(out_ap, in_ap, wkeys, eng="sp", rkeys=()):
            k0 = wkeys[0]
            nm = "ld_" + (k0 if isinstance(k0, str) else "_".join(str(z) for z in k0))
            S.op(eng, lambda e: e.dma_start(out=out_ap, in_=in_ap), reads=rkeys, writes=wkeys, dma=nm)

        def dump(name, ap, shape, key, r0=None, r1=None):
            if name not in debug:
                return
            if name not in dbg_outs:
                dbg_outs[name] = nc.dram_tensor("dbg_" + name, list(shape), F32, kind="ExternalOutput").ap()
            t = dbg_outs[name]
            tt = t if r0 is None else t[r0:r1]
            S.op("sp", lambda e: e.dma_start(out=tt, in_=ap), reads=[key] if not isinstance(key, list) else key, dma="dbg_" + name)

        fin_tokens = []

        dma_load(cst[:, :], consts, ["cst"])
        dma_load(tmk[:, :], tmask, ["tmk"])
        S.op("dve", lambda e: e.tensor_copy(out=cst_bf[:, :], in_=C("ident")), reads=["cst"], writes=["cst_bf"])
        S.op("dve", lambda e: e.memset(one_col[:, :], 1.0), writes=["one_col"])
        S.op("dve", lambda e: e.tensor_copy(out=ru(cstr[:, :]), in_=cst[:, :]), reads=["cst"], writes=["cstr"])

        pa = ExitStack()
        with pa:
            wr = sb(pa, "wr", [128, 8, NE])
            wsl = [sb(pa, "wsl%d" % i, [128, 8, 512], BF16) for i in range(3)]
            wsl_rr = [0]

            def flat2(ap_, total):
                return ap_.rearrange("(a b) -> a b", b=2048) if False else ap_

            def cast_flat(dst, src, total, key):
                rows = total // 2048
                r0 = 0
                while r0 < rows:
                    n = min(1024, rows - r0)
                    dma_load(dst[r0:r0 + n, :], src[r0:r0 + n, :], [key], eng="pool")
                    r0 += n

            cast_flat(win_bf[:, :].rearrange("a b -> (a b)").rearrange("(r c) -> r c", c=2048),
                      w_in.rearrange("a b -> (a b)").rearrange("(r c) -> r c", c=2048), D * DIN, "win_bf")
            cast_flat(wout_bf[:, :].rearrange("a b -> (a b)").rearrange("(r c) -> r c", c=2048),
                      w_out.rearrange("a b -> (a b)").rearrange("(r c) -> r c", c=2048), D * D, "wout_bf")

            def flatv(ap_, pat):
                return ap_.rearrange(pat).rearrange("(r c) -> r c", c=2048)

            def cast_experts():
                cast_flat(flatv(wg_bf[0:dne], "e a b -> (e a b)"), flatv(w_gate, "e a b -> (e a b)"), dne * D * 256, "wg_bf")
                cast_flat(flatv(wu_bf[0:dne], "e a b -> (e a b)"), flatv(w_up, "e a b -> (e a b)"), dne * D * 256, "wu_bf")
                cast_flat(flatv(wd_bf[0:dne], "e a b -> (e a b)"), flatv(w_down, "e a b -> (e a b)"), dne * D * 256, "wd_bf")
                cast_flat(flatv(wg_bf[NE], "a b -> (a b)"), flatv(ws_gate, "a b -> (a b)"), D * 256, "wg_bf")
                cast_flat(flatv(wu_bf[NE], "a b -> (a b)"), flatv(ws_up, "a b -> (a b)"), D * 256, "wu_bf")
                cast_flat(flatv(wd_bf[NE], "a b -> (a b)"), flatv(ws_down, "a b -> (a b)"), D * 256, "wd_bf")

            def wload(src_bf, key, col0, n):
                i = wsl_rr[0] % 3
                wsl_rr[0] += 1
                dma_load(wsl[i][:, :, 0:n], src_bf[:, col0:col0 + n].rearrange("(kc p) n -> p kc n", p=128), [("wsl", i)], rkeys=[key])
                return wsl[i], ("wsl", i)

            dma_load(wr[:, :, :], w_router.rearrange("(kc p) n -> p kc n", p=128), ["wr"])

            cw = sb(pa, "cw", [128, 48])
            gnw = sb(pa, "gnw", [128, 128])
            hnw = sb(pa, "hnw", [128, 512])
            rbb = sb(pa, "rbb", [128, NE])
            adt = sb(pa, "adt", [128, 8])
            negA = sb(pa, "negA", [128, 4])
            lbv = sb(pa, "lbv", [128, 4])
            omlv = sb(pa, "omlv", [128, 4])
            lbB = sb(pa, "lbB", [128, 512])
            omlB = sb(pa, "omlB", [128, 512])
            p0 = ExitStack()
            p0.__enter__()
            lbc = sb(p0, "lbc", [128, 8])
            lbb = sb(p0, "lbb", [128, 2, 512])
            n1bc = sb(p0, "n1bc", [128, D])
            n2bc = sb(p0, "n2bc", [128, D])
            cv = sb(p0, "cv", [128, 8])
            cB = sb(p0, "cB", [128, 8, 128])
            wada = [sb(p0, "wada%d" % i, [128, 8, 512]) for i in range(2)]
            dma_load(cw[:, :], convw, ["cw"])
            dma_load(lbc[:, :], lbcol, ["lbc"])
            dma_load(lbb[:, 0, :], hg_lb[0, :].partition_broadcast(128), ["lbb"])
            dma_load(lbb[:, 1, :], hg_lb[1, :].partition_broadcast(128), ["lbb"])
            dma_load(n1bc[:, :], norm1_w.partition_broadcast(128), ["n1bc"])
            dma_load(n2bc[:, :], norm2_w.partition_broadcast(128), ["n2bc"])
            dma_load(fwbc[:, :], final_w.partition_broadcast(128), ["fwbc"])
            dma_load(gnw[:, :], gdn_nw.partition_broadcast(128), ["gnw"])
            dma_load(hnw[:, :], hg_nw.partition_broadcast(128), ["hnw"])
            dma_load(rbb[:, :], router_bias.partition_broadcast(128), ["rbb"])
            dma_load(adt[:, 0:4], a_log.partition_broadcast(128), ["adt"])
            dma_load(adt[:, 4:8], dt_bias.partition_broadcast(128), ["adt"])
            S.op("act", lambda e: e.activation(out=negA[:, :], in_=adt[:, 0:4], func=AF.Exp), reads=["adt"], writes=["negA"])
            S.op("dve", lambda e: e.tensor_scalar(out=negA[:, :], in0=negA[:, :], scalar1=-1.0, scalar2=None, op0=ALU.mult),
                 reads=["negA"], writes=["negA"])
            S.op("dve", lambda e: e.tensor_tensor(out=lbv[:, :], in0=lbc[:, 0:4], in1=lbc[:, 4:8], op=ALU.subtract),
                 reads=["lbc"], writes=["lbv"])
            S.op("act", lambda e: e.activation(out=lbv[:, :], in_=lbv[:, :], func=AF.Sigmoid), reads=["lbv"], writes=["lbv"])
            S.op("dve", lambda e: e.tensor_scalar(out=omlv[:, :], in0=lbv[:, :], scalar1=-1.0, scalar2=1.0, op0=ALU.mult, op1=ALU.add),
                 reads=["lbv"], writes=["omlv"])
            S.op("dve", lambda e: e.tensor_tensor(out=lbB[:, :], in0=lbb[:, 0, :], in1=lbb[:, 1, :], op=ALU.subtract),
                 reads=["lbb"], writes=["lbB"])
            S.op("act", lambda e: e.activation(out=lbB[:, :], in_=lbB[:, :], func=AF.Sigmoid), reads=["lbB"], writes=["lbB"])
            S.op("dve", lambda e: e.tensor_scalar(out=omlB[:, :], in0=lbB[:, :], scalar1=-1.0, scalar2=1.0, op0=ALU.mult, op1=ALU.add),
                 reads=["lbB"], writes=["omlB"])

            dma_load(cv[:, :], cvec, ["cv"])
            S.op("act", lambda e: e.activation(out=cv[:, :], in_=cv[:, :], func=AF.Silu), reads=["cv"], writes=["cv"])
            for kc in range(8):
                S.op("dve", lambda e, kc=kc: e.tensor_scalar(out=cB[:, kc, :], in0=C("ones"), scalar1=cv[:, kc:kc + 1], scalar2=None, op0=ALU.mult),
                     reads=["cst", "cv"], writes=[("cB", kc)])
            dma_load(modbc[:, :], b_ada.partition_broadcast(128), ["modbc"])
            for g in range(12):
                wa = wada[g % 2]
                wk = ("wada", g % 2)
                dma_load(wa[:, :, :], w_ada[:, g * 512:(g + 1) * 512].rearrange("(kc p) n -> p kc n", p=128), [wk])
                pb, pk = pfull()
                for kc in range(8):
                    S.op("pe", lambda e, kc=kc, wa=wa, pb=pb: e.matmul(pb[:, :], cB[:, kc, :], wa[:, kc, :], start=(kc == 0), stop=(kc == 7)),
                         reads=[("cB", kc), wk], writes=[pk])
                S.op("dve", lambda e, g=g, pb=pb: e.tensor_tensor(out=modbc[:, g * 512:(g + 1) * 512], in0=pb[:, :], in1=modbc[:, g * 512:(g + 1) * 512], op=ALU.add),
                     reads=[pk, "modbc"], writes=["modbc"])
            SH1, SC1, G1, SH2, SC2, G2 = [modbc[:, i * D:(i + 1) * D] for i in range(6)]
            S.op("dve", lambda e: e.scalar_tensor_tensor(out=SC1, in0=SC1, scalar=1.0, in1=n1bc[:, :], op0=ALU.add, op1=ALU.mult),
                 reads=["modbc", "n1bc"], writes=["modbc"])
            S.op("dve", lambda e: e.scalar_tensor_tensor(out=SC2, in0=SC2, scalar=1.0, in1=n2bc[:, :], op0=ALU.add, op1=ALU.mult),
                 reads=["modbc", "n2bc"], writes=["modbc"])

            S.emit()
            p0.close()
            cast_experts()

            xt = [sb(pa, "xt%d" % i, [128, D]) for i in range(2)]
            sq = sb(pa, "sq", [128, D])
            hb = sb(pa, "hb", [128, D], BF16)
            hT = sb(pa, "hT", [128, 8, NW], BF16)
            pj = sb(pa, "pj", [128, 12, NW + 3])
            fT = sb(pa, "fT", [128, 4, NW])
            qbT = sb(pa, "qbT", [128, 4, NW])
            cvb = sb(pa, "cvb", [128, 12, NW])
            for g in range(12):
                S.op("pool", lambda e, g=g: e.memset(pj[:, g, 0:3], 0.0), writes=[("pj", g)])
            tmpf = sb(pa, "tmpf", [128, NW])
            tmpg = sb(pa, "tmpg", [128, NW])
            tsq = sb(pa, "tsq", [128, NW])
            Sg = [sb(pa, "Sg%d" % h, [128, 128]) for h in range(4)]
            Sh = [sb(pa, "Sh%d" % h, [128, 128]) for h in range(4)]
            for h in range(4):
                S.op("dve", lambda e, h=h: e.tensor_scalar(out=rc(Sg[h][:, :]), in0=C("ones"), scalar1=0.0, scalar2=None, op0=ALU.mult), reads=["cst"], writes=[("Sg", h)])
                S.op("pool", lambda e, h=h: e.memset(Sh[h][:, :], 0.0), writes=[("Sh", h)])
            ba = sb(pa, "ba", [128, 8])
            beta = sb(pa, "beta", [128, 4])
            gg = sb(pa, "gg", [128, 4])
            gc = sb(pa, "gc", [128, 4])
            gl = sb(pa, "gl", [128, 4])
            egc = sb(pa, "egc", [128, 4])
            ekt = sb(pa, "ekt", [128, 4])
            bek = sb(pa, "bek", [128, 4])
            Gb = sb(pa, "Gb", [128, 128])
            gcb = sb(pa, "gcb", [128, 128])
            glb = sb(pa, "glb", [128, 2])
            dec = sb(pa, "dec", [128, 128])
            decT = sb(pa, "decT", [128, 128])
            ktok = sb(pa, "ktok", [128, 128])
            kT2 = sb(pa, "kT2", [128, 128])
            vtok = sb(pa, "vtok", [128, 128])
            kbg = sb(pa, "kbg", [128, 128])
            bv = sb(pa, "bv", [128, 128])
            ktl = sb(pa, "ktl", [128, 128])
            Pm = [sb(pa, "Pm%d" % i, [128, 128]) for i in range(2)]
            Qm = [sb(pa, "Qm%d" % i, [128, 128]) for i in range(2)]
            Rm = [sb(pa, "Rm%d" % i, [128, 128]) for i in range(2)]
            ut = sb(pa, "ut", [128, 128])
            wT = sb(pa, "wT", [128, 128])
            vnew = sb(pa, "vnew", [128, 128])
            qdT = sb(pa, "qdT", [128, 128])
            atT = sb(pa, "atT", [128, 128])
            otok = sb(pa, "otok", [128, D])
            ob = sb(pa, "ob", [128, D], BF16)
            oT = sb(pa, "oT", [128, 8, 128], BF16)
            gateS = sb(pa, "gateS", [128, D])
            fv = sb(pa, "fv", [128, 512])
            logf = sb(pa, "logf", [128, 512])
            kh = sb(pa, "kh", [128, 512])
            ktail = sb(pa, "ktail", [128, 512])
            vh = sb(pa, "vh", [128, 512])
            ebl = sb(pa, "ebl", [128, 4, 2])
            ebT = sb(pa, "ebT", [128, 128])
            enbT = sb(pa, "enbT", [128, 128])
            qtT = sb(pa, "qtT", [128, 128])
            ktT = sb(pa, "ktT", [128, 128])
            AT = sb(pa, "AT", [128, 128])
            stat = sb(pa, "stat", [128, 16])
            x1 = sb(pa, "x1", [128, D])
            h2f = sb(pa, "h2f", [128, D])
            h2b = sb(pa, "h2b", [128, D], BF16)
            h2Tf = sb(pa, "h2Tf", [128, 8, 128])
            h2Tt = sb(pa, "h2Tt", [128, 8 * 128], BF16)
            rsc = sb(pa, "rsc", [128, NE])
            rsel = sb(pa, "rsel", [128, NE])
            rtmp = sb(pa, "rtmp", [128, NE])
            rg8 = sb(pa, "rg8", [128, 8, 4])

            mulmask = C("blk")

            for si in range(NT // TS - nsi, NT // TS):
                if stage < 1:
                    break
                full_s = (si * TS >= OWN0)
                for j in range(TS):
                    ti = si * TS + j
                    xb = xt[ti % 2]
                    xk = ("xt", ti % 2)
                    dma_load(xb[:, :], xseq[ti * 128:(ti + 1) * 128, :], [xk])
                    S.op("act", lambda e, xb=xb: e.activation(out=sq[:, :], in_=xb[:, :], func=AF.Square, accum_out=stat[:, 0:1]),
                         reads=[xk], writes=["sq", "stat0"])
                    S.op("dve", lambda e: e.tensor_scalar(out=stat[:, 1:2], in0=stat[:, 0:1], scalar1=1.0 / D, scalar2=EPS, op0=ALU.mult, op1=ALU.add),
                         reads=["stat0"], writes=["stat1"])
                    S.op("act", lambda e: e.activation(out=stat[:, 1:2], in_=stat[:, 1:2], func=AF.Sqrt), reads=["stat1"], writes=["stat1"])
                    S.op("dve", lambda e: e.reciprocal(out=stat[:, 1:2], in_=stat[:, 1:2]), reads=["stat1"], writes=["stat1"])
                    S.op("dve", lambda e, xb=xb: e.scalar_tensor_tensor(out=sq[:, :], in0=xb[:, :], scalar=stat[:, 1:2], in1=SC1, op0=ALU.mult, op1=ALU.mult),
                         reads=[xk, "stat1", "modbc"], writes=["sq"])
                    S.op("dve", lambda e: e.tensor_tensor(out=sq[:, :], in0=sq[:, :], in1=SH1, op=ALU.add), reads=["sq", "modbc"], writes=["sq"])
                    S.op("dve", lambda e, ti=ti: e.tensor_scalar(out=hb[:, :], in0=sq[:, :], scalar1=tmk[:, ti:ti + 1], scalar2=None, op0=ALU.mult),
                         reads=["sq", "tmk"], writes=["hb"])
                    if OWN0 <= ti < OWN0 + 2:
                        dump("hchk", sq[:, :], [256, D], "sq", (ti - OWN0) * 128, (ti - OWN0 + 1) * 128)
                    for kc in range(8):
                        S.op("pe", lambda e, kc=kc: e.transpose(out=tbank_bf[:, kc * 128:(kc + 1) * 128], in_=hb[:, kc * 128:(kc + 1) * 128], identity=cst_bf[:, :]),
                             reads=["hb", "cst_bf"], writes=[TB])
                    S.op("act", lambda e, j=j: e.activation(out=hT[:, :, j * 128:(j + 1) * 128],
                                                            in_=tbank_bf.rearrange("p (k t) -> p k t", k=8), func=AF.Copy),
                         reads=[TB], writes=[("hT", j)])
                hTk = [("hT", j) for j in range(TS)]

                if stage < 2:
                    continue
                def proj_fm(wsb, wk, c0, evac):
                    pb, pk = pfull()
                    for kc in range(8):
                        S.op("pe", lambda e, kc=kc, pb=pb: e.matmul(pb[:, 0:NW], wsb[:, kc, c0:c0 + 128], hT[:, kc, :], start=(kc == 0), stop=(kc == 7)),
                             reads=[wk] + hTk, writes=[pk])
                    evac(pb, pk)

                groups = list(range(4, 12)) if (si + 1) * TS < OWN0 else list(range(12))
                for g in groups:
                    if g % 4 == 0:
                        wsb, wk = wload(win_bf, "win_bf", g * 128, 512)
                    def ev(pb, pk, g=g):
                        S.op("act", lambda e: e.activation(out=pj[:, g, 3:NW + 3], in_=pb[:, 0:NW], func=AF.Copy),
                             reads=[pk], writes=[("pj", g)])
                    proj_fm(wsb, wk, (g % 4) * 128, ev)
                    S.op("dve", lambda e, g=g: e.tensor_scalar(out=tmpf[:, :], in0=pj[:, g, 3:NW + 3], scalar1=cw[:, g * 4 + 3:g * 4 + 4], scalar2=None, op0=ALU.mult),
                         reads=[("pj", g), "cw"], writes=["tmpf"])
                    for tap in (2, 1, 0):
                        S.op("dve", lambda e, g=g, tap=tap: e.scalar_tensor_tensor(out=tmpf[:, :], in0=pj[:, g, tap:tap + NW], scalar=cw[:, g * 4 + tap:g * 4 + tap + 1],
                                                                                   in1=tmpf[:, :], op0=ALU.mult, op1=ALU.add),
                             reads=[("pj", g), "cw", "tmpf"], writes=["tmpf"])
                    S.op("pool", lambda e, g=g: e.tensor_copy(out=pj[:, g, 0:3], in_=pj[:, g, NW:NW + 3]), reads=[("pj", g)], writes=[("pj", g)])
                    S.op("act", lambda e, g=g: e.activation(out=cvb[:, g, :], in_=tmpf[:, :], func=AF.Silu), reads=["tmpf"], writes=[("cvb", g)])
                    if g < 8:
                        S.op("act", lambda e, g=g: e.activation(out=ru(tsq[:, :]), in_=cvb[:, g, :], func=AF.Square), reads=[("cvb", g)], writes=["tsq"])
                        pb, pk = pfull()
                        S.op("pe", lambda e, pb=pb: mm_u(e, pb[:, 0:NW], CR("ones"), tsq[:, :]), reads=["cstr", "tsq"], writes=[pk])
                        sc = EPS if g >= 4 else EPS
                        S.op("dve", lambda e, pb=pb: e.tensor_scalar(out=tmpg[:, :], in0=pb[:, 0:NW], scalar1=EPS, scalar2=None, op0=ALU.add), reads=[pk], writes=["tmpg"])
                        S.op("act", lambda e: e.activation(out=tmpg[:, :], in_=tmpg[:, :], func=AF.Sqrt), reads=["tmpg"], writes=["tmpg"])
                        S.op("dve", lambda e: e.reciprocal(out=tmpg[:, :], in_=tmpg[:, :]), reads=["tmpg"], writes=["tmpg"])
                        if g < 4:
                            S.op("dve", lambda e, g=g: e.scalar_tensor_tensor(out=cvb[:, g, :], in0=cvb[:, g, :], scalar=128.0 ** -0.5, in1=tmpg[:, :], op0=ALU.mult, op1=ALU.mult),
                                 reads=[("cvb", g), "tmpg"], writes=[("cvb", g)])
                        else:
                            S.op("dve", lambda e, g=g: e.tensor_tensor(out=rc(cvb[:, g, :]), in0=cvb[:, g, :], in1=tmpg[:, :], op=ALU.mult),
                                 reads=[("cvb", g), "tmpg"], writes=[("cvb", g)])
                if full_s:
                    wsb, wk = wload(win_bf, "win_bf", OF, 512)
                    for h in range(4):
                        def evf(pb, pk, h=h):
                            S.op("act", lambda e: e.activation(out=fT[:, h, :], in_=pb[:, 0:NW], func=AF.Sigmoid, scale=-1.0), reads=[pk], writes=[("fT", h)])
                        proj_fm(wsb, wk, h * 128, evf)
                    wsb, wk = wload(win_bf, "win_bf", OQB, 512)
                    for h in range(4):
                        def evq(pb, pk, h=h):
                            S.op("act", lambda e: e.activation(out=qbT[:, h, :], in_=pb[:, 0:NW], func=AF.Silu), reads=[pk], writes=[("qbT", h)])
                        proj_fm(wsb, wk, h * 128, evq)

                if si * TS == OWN0:
                    dump("cvb", cvb[:, :, :].rearrange("p g t -> p (g t)"), [128, 12 * NW], [("cvb", g) for g in range(12)])
                if stage < 3:
                    continue
                for j in range(TS):
                    ti = si * TS + j
                    full = ti >= OWN0
                    tsl = slice(j * 128, (j + 1) * 128)

                    def proj_tm(col0, n):
                        wsb, wk = wload(win_bf, "win_bf", col0, n)
                        pb, pk = pfull()
                        for kc in range(8):
                            S.op("pe", lambda e, kc=kc, pb=pb: e.matmul(pb[:, 0:n], hT[:, kc, tsl], wsb[:, kc, 0:n], start=(kc == 0), stop=(kc == 7)),
                                 reads=[wk, ("hT", j)], writes=[pk])
                        return pb, pk

                    pb, pk = proj_tm(OBETA, 8)
                    S.op("act", lambda e, pb=pb: e.activation(out=beta[:, :], in_=pb[:, 0:4], func=AF.Sigmoid), reads=[pk], writes=["beta"])
                    S.op("dve", lambda e, pb=pb: e.tensor_tensor(out=ba[:, 4:8], in0=pb[:, 4:8], in1=adt[:, 4:8], op=ALU.add), reads=[pk, "adt"], writes=["ba"])
                    S.op("act", lambda e: e.activation(out=ba[:, 4:8], in_=ba[:, 4:8], func=AF.Exp), reads=["ba"], writes=["ba"])
                    S.op("act", lambda e: e.activation(out=ba[:, 4:8], in_=ba[:, 4:8], func=AF.Ln, bias=one_col[:, 0:1]), reads=["ba", "one_col"], writes=["ba"])
                    S.op("dve", lambda e: e.tensor_tensor(out=ru(gg[:, :]), in0=ba[:, 4:8], in1=negA[:, :], op=ALU.mult), reads=["ba", "negA"], writes=["gg"])
                    if stage == 3 and SUB < 1:
                        continue
                    p1, k1 = pq()
                    S.op("pe", lambda e, p1=p1: mm_u(e, p1[:, 0:4], CR("triu"), gg[:, :]), reads=["cstr", "gg"], writes=[k1])
                    S.op("dve", lambda e, p1=p1: e.tensor_copy(out=gc[:, :], in_=p1[:, 0:4]), reads=[k1], writes=["gc"])
                    p2, k2 = pq()
                    S.op("pe", lambda e, p2=p2: mm_u(e, p2[:, 0:4], CR("blk"), gg[:, :]), reads=["cstr", "gg"], writes=[k2])
                    S.op("dve", lambda e, p2=p2: e.tensor_tensor(out=gl[:, :], in0=p2[:, 0:4], in1=gc[:, :], op=ALU.subtract), reads=[k2, "gc"], writes=["gl"])
                    S.op("act", lambda e: e.activation(out=ekt[:, :], in_=gl[:, :], func=AF.Exp), reads=["gl"], writes=["ekt"])
                    S.op("act", lambda e: e.activation(out=egc[:, :], in_=gc[:, :], func=AF.Exp), reads=["gc"], writes=["egc"])
                    S.op("dve", lambda e: e.tensor_tensor(out=bek[:, :], in0=egc[:, :], in1=beta[:, :], op=ALU.mult), reads=["egc", "beta"], writes=["bek"])

                    if OWN0 <= ti < OWN0 + 2:
                        r0 = (ti - OWN0) * 128
                        dump("beta", beta[:, :], [256, 4], "beta", r0, r0 + 128)
                        dump("gg", gg[:, :], [256, 4], "gg", r0, r0 + 128)
                        dump("gc", gc[:, :], [256, 4], "gc", r0, r0 + 128)
                    if stage == 3 and SUB < 2:
                        continue
                    if full:
                        pbA, pkA = proj_tm(OGA, 512)
                        S.op("act", lambda e, pbA=pbA: e.activation(out=gateS[:, 0:512], in_=pbA[:, :], func=AF.Silu), reads=[pkA], writes=["gateA"])
                        pbB, pkB = proj_tm(OGB, 512)
                        S.op("act", lambda e, pbB=pbB: e.activation(out=gateS[:, 512:1024], in_=pbB[:, :], func=AF.Silu), reads=[pkB], writes=["gateB"])

                    for h in range(4 if stage >= 4 else 0):
                        gq, gk, gv = h, 4 + h, 8 + h
                        kTt = cvb[:, gk, tsl]
                        qTt = cvb[:, gq, tsl]
                        p, k_ = pq()
                        S.op("pe", lambda e, p=p, kTt=kTt: trp(e, p, kTt), reads=[("cvb", gk), "cst"], writes=[k_])
                        S.op("act", lambda e, p=p: e.activation(out=ktok[:, :], in_=p, func=AF.Copy), reads=[k_], writes=["ktok"])
                        p, k_ = pq()
                        S.op("pe", lambda e, p=p, gv=gv: trp(e, p, cvb[:, gv, tsl]), reads=[("cvb", gv), "cst"], writes=[k_])
                        S.op("dve", lambda e, p=p, h=h: e.tensor_scalar(out=rr(bv[:, :]), in0=p, scalar1=beta[:, h:h + 1], scalar2=None, op0=ALU.mult),
                             reads=[k_, "beta"], writes=["bv"])
                        S.op("dve", lambda e, h=h: e.tensor_scalar(out=rr(kbg[:, :]), in0=ktok[:, :], scalar1=bek[:, h:h + 1], scalar2=None, op0=ALU.mult),
                             reads=["ktok", "bek"], writes=["kbg"])
                        S.op("dve", lambda e, h=h: e.tensor_scalar(out=rc(ktl[:, :]), in0=ktok[:, :], scalar1=ekt[:, h:h + 1], scalar2=None, op0=ALU.mult),
                             reads=["ktok", "ekt"], writes=["ktl"])
                        if stage == 4 and SUB < 1:
                            continue
                        S.op("dve", lambda e, h=h: e.tensor_scalar(out=ru(Gb[:, :]), in0=C("ones"), scalar1=gg[:, h:h + 1], scalar2=None, op0=ALU.mult),
                             reads=["cst", "gg"], writes=["Gb"])
                        pg, kg = pq()
                        S.op("pe", lambda e, pg=pg: mm_u(e, pg, Gb[:, :], CR("triu")), reads=["Gb", "cstr"], writes=[kg])
                        pl, kl = pq()
                        S.op("pe", lambda e, pl=pl: mm_u(e, pl[:, 0:2], Gb[:, :], CR("cind")[:, 0:2]), reads=["Gb", "cstr"], writes=[kl])
                        S.op("act", lambda e, pl=pl: e.activation(out=glb[:, :], in_=pl[:, 0:2], func=AF.Exp), reads=[kl], writes=["glb"])
                        S.op("dve", lambda e, pg=pg, h=h: e.tensor_scalar(out=dec[:, :], in0=pg, scalar1=gc[:, h:h + 1], scalar2=0.0, op0=ALU.subtract, op1=ALU.max),
                             reads=[kg, "gc"], writes=["dec"])
                        S.op("act", lambda e: e.activation(out=dec[:, :], in_=dec[:, :], func=AF.Exp, scale=-1.0), reads=["dec"], writes=["dec"])
                        if full:
                            S.op("dve", lambda e, pg=pg, h=h: e.tensor_scalar(out=decT[:, :], in0=pg, scalar1=gc[:, h:h + 1], scalar2=0.0, op0=ALU.subtract, op1=ALU.min),
                                 reads=[kg, "gc"], writes=["decT"])
                            S.op("act", lambda e: e.activation(out=decT[:, :], in_=decT[:, :], func=AF.Exp), reads=["decT"], writes=["decT"])
                            S.op("act", lambda e, pg=pg: e.activation(out=qdT[:, :], in_=pg, func=AF.Exp), reads=[kg], writes=["qdT"])
                            S.op("dve", lambda e, qTt=qTt: e.tensor_tensor(out=qdT[:, :], in0=qdT[:, :], in1=qTt, op=ALU.mult), reads=["qdT", ("cvb", gq)], writes=["qdT"])
                        if stage == 4 and SUB < 2:
                            continue
                        pkk, kkk = pq()
                        S.op("act", lambda e, kTt=kTt: e.activation(out=rc(kT2[:, :]), in_=kTt, func=AF.Copy), reads=[("cvb", gk)], writes=["kT2"])
                        S.op("pe", lambda e, pkk=pkk, kTt=kTt: mm_c(e, pkk, kTt, kT2[:, :], 1), reads=[("cvb", gk), "kT2"], writes=[kkk])
                        S.op("dve", lambda e, pkk=pkk, h=h: e.scalar_tensor_tensor(out=rr(Pm[0][:, :]), in0=pkk, scalar=beta[:, h:h + 1], in1=dec[:, :], op0=ALU.mult, op1=ALU.mult),
                             reads=[kkk, "beta", "dec"], writes=[("Pm", 0)])
                        S.op("dve", lambda e: e.tensor_tensor(out=rr(Pm[0][:, :]), in0=Pm[0][:, :], in1=C("strictl"), op=ALU.mult), reads=[("Pm", 0), "cst"], writes=[("Pm", 0)])
                        if stage == 4 and SUB < 2.2:
                            continue
                        if full:
                            pa_, ka_ = pq()
                            S.op("pe", lambda e, pa_=pa_, kTt=kTt, qTt=qTt: e.matmul(pa_, kTt, qTt, start=True, stop=True), reads=[("cvb", gk), ("cvb", gq)], writes=[ka_])
                            S.op("dve", lambda e, pa_=pa_: e.tensor_tensor(out=atT[:, :], in0=pa_, in1=decT[:, :], op=ALU.mult), reads=[ka_, "decT"], writes=["atT"])
                            S.op("dve", lambda e: e.tensor_tensor(out=atT[:, :], in0=atT[:, :], in1=C("triu"), op=ALU.mult), reads=["atT", "cst"], writes=["atT"])
                        if stage == 4 and SUB < 2.4:
                            continue
                        p, k_ = pq()
                        S.op("pe", lambda e, p=p: trp(e, p, Pm[0][:, :]), reads=[("Pm", 0), "cst"], writes=[k_])
                        S.op("act", lambda e, p=p: e.activation(out=rr(Qm[0][:, :]), in_=p, func=AF.Copy), reads=[k_], writes=[("Qm", 0)])
                        S.op("dve", lambda e, p=p: e.scalar_tensor_tensor(out=rr(Rm[0][:, :]), in0=p, scalar=-1.0, in1=C("ident"), op0=ALU.mult, op1=ALU.add), reads=[k_, "cst"], writes=[("Rm", 0)])
                        cur = 0
                        if stage == 4 and SUB < 2.6:
                            continue
                        for lev in range(1, 6):
                            nxt = 1 - cur
                            if lev < 5:
                                p, k_ = pq()
                                S.op("pe", lambda e, p=p, cur=cur: mmr(e, p, Pm[cur][:, :], Qm[cur][:, :], start=True, stop=True),
                                     reads=[("Pm", cur), ("Qm", cur)], writes=[k_])
                                S.op("act", lambda e, p=p, nxt=nxt: e.activation(out=rr(Qm[nxt][:, :]), in_=p, func=AF.Copy), reads=[k_], writes=[("Qm", nxt)])
                            p, k_ = pq()
                            S.op("pe", lambda e, p=p, cur=cur: mmr(e, p, Qm[cur][:, :], Pm[cur][:, :], start=True, stop=True),
                                 reads=[("Pm", cur), ("Qm", cur)], writes=[k_])
                            S.op("dve", lambda e, p=p, nxt=nxt: e.tensor_copy(out=rr(Pm[nxt][:, :]), in_=p), reads=[k_], writes=[("Pm", nxt)])
                            p, k_ = pq()
                            S.op("pe", lambda e, p=p, cur=cur, nxt=nxt: mmr(e, p, Pm[nxt][:, :], Rm[cur][:, :], start=True, stop=True),
                                 reads=[("Pm", nxt), ("Rm", cur)], writes=[k_])
                            S.op("dve", lambda e, p=p, cur=cur, nxt=nxt: e.tensor_tensor(out=rr(Rm[nxt][:, :]), in0=p, in1=Rm[cur][:, :], op=ALU.add),
                                 reads=[k_, ("Rm", cur)], writes=[("Rm", nxt)])
                            cur = nxt
                        if stage == 4 and SUB < 2.8:
                            continue
                        TT = Rm[cur]
                        TTk = ("Rm", cur)
                        p, k_ = pq()
                        S.op("pe", lambda e, p=p, TT=TT: mmr(e, p, TT[:, :], bv[:, :], start=True, stop=True), reads=[TTk, "bv"], writes=[k_])
                        S.op("act", lambda e, p=p: e.activation(out=ut[:, :], in_=p, func=AF.Copy), reads=[k_], writes=["ut"])
                        p, k_ = pq()
                        S.op("pe", lambda e, p=p, TT=TT: mmr(e, p, kbg[:, :], TT[:, :], start=True, stop=True), reads=[TTk, "kbg"], writes=[k_])
                        S.op("act", lambda e, p=p: e.activation(out=rc(wT[:, :]), in_=p, func=AF.Copy), reads=[k_], writes=["wT"])
                        if stage == 4 and SUB < 3:
                            continue
                        if full:
                            po, ko = pfull()
                            po = po[:, 0:128]
                        for c in range(2):
                            cs = slice(c * 64, (c + 1) * 64)
                            p, k_ = pq()
                            S.op("pe", lambda e, p=p, cs=cs, h=h: (mm_c(e, p, wT[:, :], Sg[h][:, :], 2) if (R_CHAIN and (CH_MASK & 2)) else e.matmul(p[cs, :], wT[:, cs], Sg[h][:, :], start=True, stop=True)), reads=["wT", ("Sg", h)], writes=[k_])
                            S.op("dve", lambda e, p=p, cs=cs: e.scalar_tensor_tensor(out=rc(vnew[cs, :]), in0=p[cs, :], scalar=-1.0, in1=ut[cs, :], op0=ALU.mult, op1=ALU.add),
                                 reads=[k_, "ut"], writes=[("vnew", c)])
                            if full:
                                S.op("pe", lambda e, po=po, cs=cs, h=h: e.matmul(po[cs, :], qdT[:, cs], Sg[h][:, :], start=True, stop=False),
                                     reads=["qdT", ("Sg", h)], writes=[ko])
                                S.op("pe", lambda e, po=po, cs=cs: e.matmul(po[cs, :], atT[cs, cs], vnew[cs, :], start=False, stop=True),
                                     reads=["atT", ("vnew", c)], writes=[ko])
                            p2_, k2_ = pq()
                            S.op("pe", lambda e, p2_=p2_, cs=cs: mm_c(e, p2_, ktl[cs, :], vnew[cs, :], 4), reads=["ktl", ("vnew", c)], writes=[k2_])
                            S.op("act", lambda e, c=c, h=h: e.activation(out=rc(Sg[h][:, :]), in_=Sg[h][:, :], func=AF.Copy, scale=glb[:, c:c + 1]),
                                 reads=["glb", ("Sg", h)], writes=[("Sg", h)])
                            S.op("dve", lambda e, p2_=p2_, h=h: e.tensor_tensor(out=rc(Sg[h][:, :]), in0=p2_, in1=Sg[h][:, :], op=ALU.add),
                                 reads=[k2_, ("Sg", h)], writes=[("Sg", h)])
                        if full:
                            S.op("act", lambda e, po=po, h=h: e.activation(out=otok[:, h * 128:(h + 1) * 128], in_=po, func=AF.Copy), reads=[ko], writes=[("otok", h)])

                    if stage < 5:
                        continue
                    pbf, pkf = proj_tm(OF, 512)
                    S.op("act", lambda e, pbf=pbf: e.activation(out=fv[:, :], in_=pbf[:, :], func=AF.Sigmoid), reads=[pkf], writes=["fv"])
                    S.op("dve", lambda e: e.tensor_tensor(out=fv[:, :], in0=fv[:, :], in1=omlB[:, :], op=ALU.mult), reads=["fv", "omlB"], writes=["fv"])
                    S.op("dve", lambda e: e.tensor_tensor(out=fv[:, :], in0=fv[:, :], in1=lbB[:, :], op=ALU.add), reads=["fv", "lbB"], writes=["fv"])
                    S.op("act", lambda e: e.activation(out=ru(logf[:, :]), in_=fv[:, :], func=AF.Ln), reads=["fv"], writes=["logf"])
                    S.op("dve", lambda e: e.tensor_scalar(out=kh[:, :], in0=fv[:, :], scalar1=-1.0, scalar2=1.0, op0=ALU.mult, op1=ALU.add), reads=["fv"], writes=["kh"])
                    pbi, pki = proj_tm(OI, 512)
                    S.op("act", lambda e, pbi=pbi: e.activation(out=rc(vh[:, :]), in_=pbi[:, :], func=AF.Copy), reads=[pki], writes=["vh"])
                    pbr, pkr = pfull()
                    S.op("pe", lambda e, pbr=pbr: mm_u(e, pbr[:, :], CR("rev"), logf[:, :]), reads=["cstr", "logf"], writes=[pkr])
                    S.op("act", lambda e, pbr=pbr: e.activation(out=rc(ktail[:, :]), in_=pbr[:, :], func=AF.Exp), reads=[pkr], writes=["ktail"])
                    S.op("dve", lambda e: e.tensor_tensor(out=rc(ktail[:, :]), in0=ktail[:, :], in1=kh[:, :], op=ALU.mult), reads=["ktail", "kh"], writes=["ktail"])
                    if OWN0 <= ti < OWN0 + 2:
                        r0 = (ti - OWN0) * 128
                        dump("logf", logf[:, :], [256, 512], "logf", r0, r0 + 128)
                        dump("kh", kh[:, :], [256, 512], "kh", r0, r0 + 128)
                        dump("vh", vh[:, :], [256, 512], "vh", r0, r0 + 128)
                    for h in range(4):
                        hs = slice(h * 128, (h + 1) * 128)
                        p, k_ = pq()
                        S.op("pe", lambda e, p=p, hs=hs: mm_u(e, p[:, 0:2], logf[:, hs], CR("cind")[:, 0:2]), reads=["logf", "cstr"], writes=[k_])
                        S.op("act", lambda e, p=p, h=h: e.activation(out=ebl[:, h, :], in_=p[:, 0:2], func=AF.Exp), reads=[k_], writes=[("ebl", h)])
                        if full:
                            p, k_ = pq()
                            S.op("pe", lambda e, p=p, hs=hs: mm_u(e, p, logf[:, hs], CR("triu")), reads=["logf", "cstr"], writes=[k_])
                            S.op("act", lambda e, p=p: e.activation(out=ebT[:, :], in_=p, func=AF.Exp), reads=[k_], writes=["ebT"])
                            S.op("act", lambda e, p=p: e.activation(out=enbT[:, :], in_=p, func=AF.Exp, scale=-1.0), reads=[k_], writes=["enbT"])
                            S.op("dve", lambda e, h=h: e.tensor_tensor(out=qtT[:, :], in0=qbT[:, h, tsl], in1=ebT[:, :], op=ALU.mult), reads=[("qbT", h), "ebT"], writes=["qtT"])
                            S.op("dve", lambda e, h=h: e.scalar_tensor_tensor(out=ktT[:, :], in0=fT[:, h, tsl], scalar=omlv[:, h:h + 1], in1=enbT[:, :], op0=ALU.mult, op1=ALU.mult),
                                 reads=[("fT", h), "omlv", "enbT"], writes=["ktT"])
                            p, k_ = pq()
                            S.op("pe", lambda e, p=p: e.matmul(p, ktT[:, :], qtT[:, :], start=True, stop=True), reads=["ktT", "qtT"], writes=[k_])
                            S.op("dve", lambda e, p=p: e.tensor_tensor(out=AT[:, :], in0=p, in1=C("triu"), op=ALU.mult), reads=[k_, "cst"], writes=["AT"])
                            po, ko = pfull()
                            po = po[:, 0:128]
                        for c in range(2):
                            cs = slice(c * 64, (c + 1) * 64)
                            if full:
                                S.op("pe", lambda e, po=po, cs=cs, h=h: e.matmul(po[cs, :], qtT[:, cs], Sh[h][:, :], start=True, stop=False),
                                     reads=["qtT", ("Sh", h)], writes=[ko])
                                S.op("pe", lambda e, po=po, cs=cs, hs=hs: e.matmul(po[cs, :], AT[cs, cs], vh[cs, hs], start=False, stop=True),
                                     reads=["AT", "vh"], writes=[ko])
                            p2_, k2_ = pq()
                            S.op("pe", lambda e, p2_=p2_, cs=cs, hs=hs: mm_c(e, p2_, ktail[cs, hs], vh[cs, hs], 8), reads=["ktail", "vh"], writes=[k2_])
                            S.op("act", lambda e, c=c, h=h: e.activation(out=Sh[h][:, :], in_=Sh[h][:, :], func=AF.Copy, scale=ebl[:, h, c:c + 1]),
                                 reads=[("ebl", h), ("Sh", h)], writes=[("Sh", h)])
                            S.op("dve", lambda e, p2_=p2_, h=h: e.tensor_tensor(out=Sh[h][:, :], in0=p2_, in1=Sh[h][:, :], op=ALU.add),
                                 reads=[k2_, ("Sh", h)], writes=[("Sh", h)])
                        if full:
                            S.op("act", lambda e, po=po, h=h: e.activation(out=otok[:, 512 + h * 128:512 + (h + 1) * 128], in_=po, func=AF.Copy), reads=[ko], writes=[("otok", 4 + h)])

                    if not full or stage < 6:
                        continue
                    oi = ti - OWN0
                    ok_all = [("otok", i) for i in range(8)]
                    dump("otok", otok[:, :], [NOWN * 128, D], ok_all, oi * 128, (oi + 1) * 128)
                    for h in range(4):
                        S.op("act", lambda e, h=h: e.activation(out=sq[:, 0:128], in_=otok[:, h * 128:(h + 1) * 128], func=AF.Square, accum_out=stat[:, 2 + h:3 + h]),
                             reads=[("otok", h)], writes=["sq", ("statA", h)])
                    S.op("dve", lambda e: e.tensor_scalar(out=stat[:, 2:6], in0=stat[:, 2:6], scalar1=1.0 / 128, scalar2=EPS, op0=ALU.mult, op1=ALU.add),
                         reads=[("statA", h) for h in range(4)], writes=["statA"])
                    S.op("act", lambda e: e.activation(out=stat[:, 2:6], in_=stat[:, 2:6], func=AF.Sqrt), reads=["statA"], writes=["statA"])
                    S.op("dve", lambda e: e.reciprocal(out=stat[:, 2:6], in_=stat[:, 2:6]), reads=["statA"], writes=["statA"])
                    for h in range(4):
                        S.op("dve", lambda e, h=h: e.scalar_tensor_tensor(out=otok[:, h * 128:(h + 1) * 128], in0=otok[:, h * 128:(h + 1) * 128], scalar=stat[:, 2 + h:3 + h],
                                                                          in1=gnw[:, :], op0=ALU.mult, op1=ALU.mult),
                             reads=[("otok", h), "statA", "gnw"], writes=[("otok", h)])
                    S.op("act", lambda e: e.activation(out=sq[:, 0:512], in_=otok[:, 512:1024], func=AF.Square, accum_out=stat[:, 6:7]),
                         reads=ok_all[4:], writes=["sq", "statB"])
                    S.op("dve", lambda e: e.tensor_scalar(out=stat[:, 6:7], in0=stat[:, 6:7], scalar1=1.0 / 512, scalar2=EPS, op0=ALU.mult, op1=ALU.add),
                         reads=["statB"], writes=["statB"])
                    S.op("act", lambda e: e.activation(out=stat[:, 6:7], in_=stat[:, 6:7], func=AF.Sqrt), reads=["statB"], writes=["statB"])
                    S.op("dve", lambda e: e.reciprocal(out=stat[:, 6:7], in_=stat[:, 6:7]), reads=["statB"], writes=["statB"])
                    S.op("dve", lambda e: e.scalar_tensor_tensor(out=otok[:, 512:1024], in0=otok[:, 512:1024], scalar=stat[:, 6:7], in1=hnw[:, :], op0=ALU.mult, op1=ALU.mult),
                         reads=ok_all[4:] + ["statB", "hnw"], writes=ok_all[4:])
                    S.op("dve", lambda e: e.tensor_tensor(out=ob[:, :], in0=otok[:, :], in1=gateS[:, :], op=ALU.mult),
                         reads=ok_all + ["gateA", "gateB"], writes=["ob"])
                    for kc in range(8):
                        S.op("pe", lambda e, kc=kc: e.transpose(out=tbank_bf[:, kc * 128:(kc + 1) * 128], in_=ob[:, kc * 128:(kc + 1) * 128], identity=cst_bf[:, :]),
                             reads=["ob", "cst_bf"], writes=[TB])
                    S.op("act", lambda e: e.activation(out=oT[:, :, :], in_=tbank_bf.rearrange("p (k t) -> p k t", k=8), func=AF.Copy), reads=[TB], writes=["oT"])
                    xb = xt[ti % 2]
                    xk = ("xt", ti % 2)
                    dma_load(xb[:, :], xseq[ti * 128:(ti + 1) * 128, :], [xk])
                    for half in range(2):
                        wsb, wk = wload(wout_bf, "wout_bf", half * 512, 512)
                        pb, pk = pfull()
                        for kc in range(8):
                            S.op("pe", lambda e, kc=kc, pb=pb, wsb=wsb: e.matmul(pb[:, :], oT[:, kc, :], wsb[:, kc, :], start=(kc == 0), stop=(kc == 7)),
                                 reads=["oT", wk], writes=[pk])
                        hsl = slice(half * 512, (half + 1) * 512)
                        S.op("dve", lambda e, pb=pb, hsl=hsl: e.tensor_tensor(out=x1[:, hsl], in0=pb[:, :], in1=G1[:, hsl], op=ALU.mult), reads=[pk, "modbc"], writes=[("x1", half)])
                        S.op("dve", lambda e, hsl=hsl, xb=xb: e.tensor_tensor(out=x1[:, hsl], in0=x1[:, hsl], in1=xb[:, hsl], op=ALU.add), reads=[("x1", half), xk], writes=[("x1", half)])
                    x1k = [("x1", 0), ("x1", 1)]
                    dump("x1all", x1[:, :], [NOWN * 128, D], x1k, oi * 128, (oi + 1) * 128)
                    S.op("sp", lambda e, oi=oi: e.dma_start(out=x1scr[oi * 128:(oi + 1) * 128, :], in_=x1[:, :]), reads=x1k, dma="x1st")
                    S.op("act", lambda e: e.activation(out=sq[:, :], in_=x1[:, :], func=AF.Square, accum_out=stat[:, 8:9]), reads=x1k, writes=["sq", "stat8"])
                    S.op("dve", lambda e: e.tensor_scalar(out=stat[:, 8:9], in0=stat[:, 8:9], scalar1=1.0 / D, scalar2=EPS, op0=ALU.mult, op1=ALU.add), reads=["stat8"], writes=["stat8"])
                    S.op("act", lambda e: e.activation(out=stat[:, 8:9], in_=stat[:, 8:9], func=AF.Sqrt), reads=["stat8"], writes=["stat8"])
                    S.op("dve", lambda e: e.reciprocal(out=stat[:, 8:9], in_=stat[:, 8:9]), reads=["stat8"], writes=["stat8"])
                    S.op("dve", lambda e: e.scalar_tensor_tensor(out=h2f[:, :], in0=x1[:, :], scalar=stat[:, 8:9], in1=SC2, op0=ALU.mult, op1=ALU.mult),
                         reads=x1k + ["stat8", "modbc"], writes=["h2f"])
                    S.op("dve", lambda e: e.tensor_tensor(out=h2f[:, :], in0=h2f[:, :], in1=SH2, op=ALU.add), reads=["h2f", "modbc"], writes=["h2f"])
                    S.op("act", lambda e: e.activation(out=h2b[:, :], in_=h2f[:, :], func=AF.Copy), reads=["h2f"], writes=["h2b"])
                    for kc in range(8):
                        S.op("pe", lambda e, kc=kc: e.transpose(out=tbank_bf[:, kc * 128:(kc + 1) * 128], in_=h2b[:, kc * 128:(kc + 1) * 128], identity=cst_bf[:, :]),
                             reads=["h2b", "cst_bf"], writes=[TB])
                    S.op("act", lambda e: e.activation(out=h2Tt[:, :], in_=tbank_bf, func=AF.Copy), reads=[TB], writes=["h2Tt"])
                    S.op("sp", lambda e, oi=oi: e.dma_start(out=h2scr[oi], in_=h2Tt[:, :]), reads=["h2Tt"], dma="h2st")
                    for kc in range(8):
                        p, k_ = pq()
                        S.op("pe", lambda e, p=p, kc=kc: trp(e, p, h2f[:, kc * 128:(kc + 1) * 128]), reads=["h2f", "cst"], writes=[k_])
                        S.op("act", lambda e, p=p, kc=kc: e.activation(out=h2Tf[:, kc, :], in_=p, func=AF.Copy), reads=[k_], writes=[("h2Tf", kc)])
                    p, k_ = pq()
                    for kc in range(8):
                        S.op("pe", lambda e, p=p, kc=kc: e.matmul(p[:, 0:NE], h2Tf[:, kc, :], wr[:, kc, :], start=(kc == 0), stop=(kc == 7)),
                             reads=[("h2Tf", kc), "wr"], writes=[k_])
                    if stage < 7:
                        continue
                    S.op("act", lambda e, p=p: e.activation(out=rsc[:, :], in_=p[:, 0:NE], func=AF.Sigmoid), reads=[k_], writes=["rsc"])
                    S.op("dve", lambda e: e.tensor_tensor(out=rsel[:, :], in0=rsc[:, :], in1=rbb[:, :], op=ALU.add), reads=["rsc", "rbb"], writes=["rsel"])
                    rs3 = rsel[:, :].rearrange("p (g k) -> p g k", g=8)
                    rt3 = rtmp[:, :].rearrange("p (g k) -> p g k", g=8)
                    S.op("dve", lambda e: e.tensor_reduce(out=rg8[:, :, 0], in_=rs3, axis=AX.X, op=ALU.max), reads=["rsel"], writes=["rg8a"])
                    S.op("dve", lambda e: e.tensor_tensor(out=rt3, in0=rs3, in1=rg8[:, :, 0:1].to_broadcast([128, 8, 8]), op=ALU.is_equal),
                         reads=["rsel", "rg8a"], writes=["rtmp"])
                    S.op("dve", lambda e: e.scalar_tensor_tensor(out=rtmp[:, :], in0=rtmp[:, :], scalar=-1.0e4, in1=rsel[:, :], op0=ALU.mult, op1=ALU.add),
                         reads=["rtmp", "rsel"], writes=["rtmp"])
                    S.op("dve", lambda e: e.tensor_reduce(out=rg8[:, :, 1], in_=rt3, axis=AX.X, op=ALU.max), reads=["rtmp"], writes=["rg8b"])
                    S.op("dve", lambda e: e.tensor_tensor(out=rg8[:, :, 2], in0=rg8[:, :, 0], in1=rg8[:, :, 1], op=ALU.add), reads=["rg8a", "rg8b"], writes=["rg8c"])
                    S.op("dve", lambda e: e.tensor_copy(out=stat[:, 8:16], in_=rg8[:, :, 2]), reads=["rg8c", "stat8"], writes=["stat8"])
                    S.op("dve", lambda e: e.max(out=rtmp[:, 0:8], in_=stat[:, 8:16]), reads=["stat8"], writes=["rtmp"])
                    S.op("dve", lambda e: e.tensor_scalar(out=rg8[:, :, 3], in0=rg8[:, :, 2], scalar1=rtmp[:, 3:4], scalar2=None, op0=ALU.is_ge), reads=["rg8c", "rtmp"], writes=["rg8d"])
                    S.op("dve", lambda e: e.tensor_tensor(out=rt3, in0=rs3, in1=rg8[:, :, 3:4].to_broadcast([128, 8, 8]), op=ALU.mult), reads=["rsel", "rg8d"], writes=["rtmp"])
                    S.op("dve", lambda e: e.tensor_scalar(out=rg8[:, :, 2], in0=rg8[:, :, 3], scalar1=1.0e4, scalar2=-1.0e4, op0=ALU.mult, op1=ALU.add), reads=["rg8d"], writes=["rg8c"])
                    S.op("dve", lambda e: e.tensor_tensor(out=rt3, in0=rt3, in1=rg8[:, :, 2:3].to_broadcast([128, 8, 8]), op=ALU.add), reads=["rtmp", "rg8c"], writes=["rtmp"])
                    S.op("dve", lambda e: e.max(out=stat[:, 8:16], in_=rtmp[:, :]), reads=["rtmp"], writes=["stat8"])
                    S.op("dve", lambda e: e.tensor_scalar(out=rsel[:, :], in0=rtmp[:, :], scalar1=stat[:, 15:16], scalar2=None, op0=ALU.is_ge), reads=["rtmp", "stat8"], writes=["rsel"])
                    S.op("dve", lambda e: e.tensor_tensor(out=rsel[:, :], in0=rsel[:, :], in1=rsc[:, :], op=ALU.mult), reads=["rsel", "rsc"], writes=["rsel"])
                    S.op("dve", lambda e: e.tensor_reduce(out=stat[:, 7:8], in_=rsel[:, :], axis=AX.X, op=ALU.add), reads=["rsel"], writes=["stat7"])
                    S.op("dve", lambda e: e.reciprocal(out=stat[:, 7:8], in_=stat[:, 7:8]), reads=["stat7"], writes=["stat7"])
                    S.op("dve", lambda e, oi=oi: e.tensor_scalar(out=gates[:, oi, 0:NE], in0=rsel[:, :], scalar1=stat[:, 7:8], scalar2=2.5, op0=ALU.mult, op1=ALU.mult),
                         reads=["rsel", "stat7"], writes=["gates"])
                    S.op("dve", lambda e, oi=oi: e.memset(gates[:, oi, NE:NE + 1], 1.0), writes=["gates"])
                    dump("gatesall", gates[:, oi, :], [NOWN * 128, NE + 1], "gates", oi * 128, (oi + 1) * 128)
            S.emit()

        pbk = ExitStack()
        with pbk:
            acc = sb(pbk, "acc", [128, NOWN, D])
            h2T = sb(pbk, "h2T", [128, 8, NOWN * 128], BF16)
            for tl in range(NOWN):
                dma_load(h2T[:, :, tl * 128:(tl + 1) * 128], h2scr[tl].rearrange("p (k t) -> p k t", k=8), ["h2T"], rkeys=[])
            for i in range(NOWN):
                S.op("pool", lambda e, i=i: e.memset(acc[:, i, :], 0.0), writes=[("acc", i)])
            NSLOT = 3
            wg = [sb(pbk, "wg%d" % i, [128, 8, 256], BF16) for i in range(NSLOT)]
            wu = [sb(pbk, "wu%d" % i, [128, 8, 256], BF16) for i in range(NSLOT)]
            wd = [sb(pbk, "wd%d" % i, [128, 2, D], BF16) for i in range(NSLOT)]
            actT = [sb(pbk, "actT%d" % i, [128, 2, NOWN * 128], BF16) for i in range(2)]
            sg = [sb(pbk, "sg%d" % i, [128, 512]) for i in range(2)]
            sgi = [0]
            pending = []
            for ex in range(nex):
                sl = ex % NSLOT
                dma_load(wg[sl][:, :, :], wg_bf[ex].rearrange("(kc p) n -> p kc n", p=128), [("wg", sl)], rkeys=["wg_bf"])
                dma_load(wu[sl][:, :, :], wu_bf[ex].rearrange("(kc p) n -> p kc n", p=128), [("wu", sl)], rkeys=["wu_bf"])
                dma_load(wd[sl][:, :, :], wd_bf[ex].rearrange("(fc p) n -> p fc n", p=128), [("wd", sl)], rkeys=["wd_bf"])
                aT = actT[ex % 2]
                aK = ("actT", ex % 2)
                for nb in range(NOWN // 4):
                    nsl = slice(nb * 512, (nb + 1) * 512)
                    for fm in range(2):
                        fsl = slice(fm * 128, (fm + 1) * 128)
                        b0 = (2 * (nb * 2 + fm)) % 4
                        pg_, kg_ = banks[b0], ("bank", b0)
                        pu_, ku_ = banks[b0 + 1], ("bank", b0 + 1)
                        for kc in range(8):
                            S.op("pe", lambda e, kc=kc, pg_=pg_, sl=sl, fsl=fsl, nsl=nsl: e.matmul(pg_[:, :], wg[sl][:, kc, fsl], h2T[:, kc, nsl], start=(kc == 0), stop=(kc == 7)),
                                 reads=[("wg", sl), "h2T"], writes=[kg_])
                        for kc in range(8):
                            S.op("pe", lambda e, kc=kc, pu_=pu_, sl=sl, fsl=fsl, nsl=nsl: e.matmul(pu_[:, :], wu[sl][:, kc, fsl], h2T[:, kc, nsl], start=(kc == 0), stop=(kc == 7)),
                                 reads=[("wu", sl), "h2T"], writes=[ku_])
                        sgb = sg[sgi[0] % 2]
                        sgk = ("sg", sgi[0] % 2)
                        sgi[0] += 1
                        S.op("act", lambda e, pg_=pg_, sgb=sgb: e.activation(out=sgb[:, :], in_=pg_[:, :], func=AF.Silu), reads=[kg_], writes=[sgk])
                        S.op("dve", lambda e, pu_=pu_, sgb=sgb, aT=aT, fm=fm, nsl=nsl: e.tensor_tensor(out=aT[:, fm, nsl], in0=pu_[:, :], in1=sgb[:, :], op=ALU.mult),
                             reads=[ku_, sgk], writes=[aK])
                        for _ in range(4):
                            if pending:
                                pending.pop(0)()
                def mk_step(tl, half, aT=aT, aK=aK, sl=sl, ex=ex):
                    def step():
                        tsl = slice(tl * 128, (tl + 1) * 128)
                        bi = 4 + (tl * 2 + half) % 4
                        po_, ko_ = banks[bi], ("bank", bi)
                        for fm in range(2):
                            S.op("pe", lambda e, po_=po_, aT=aT, fm=fm, tsl=tsl, sl=sl, half=half: e.matmul(po_[:, :], aT[:, fm, tsl], wd[sl][:, fm, half * 512:(half + 1) * 512],
                                                                                                              start=(fm == 0), stop=(fm == 1)),
                                 reads=[aK, ("wd", sl)], writes=[ko_])
                        hsl = slice(half * 512, (half + 1) * 512)
                        S.op("dve", lambda e, po_=po_, tl=tl, ex=ex, hsl=hsl: e.scalar_tensor_tensor(out=acc[:, tl, hsl], in0=po_[:, :], scalar=gates[:, tl, ex:ex + 1], in1=acc[:, tl, hsl],
                                                                                                      op0=ALU.mult, op1=ALU.add),
                             reads=[ko_, "gates", ("acc", tl)], writes=[("acc", tl)])
                    return step
                while pending:
                    pending.pop(0)()
                pending.extend(mk_step(tl, half) for tl in range(NOWN) for half in range(2))
            while pending:
                pending.pop(0)()
            for tl in range(NOWN):
                dump("moe", acc[:, tl, :], [NOWN * 128, D], ("acc", tl), tl * 128, (tl + 1) * 128)
            xr = [sb(pbk, "xr%d" % i, [128, D]) for i in range(2)]
            fsq = sb(pbk, "fsq", [128, D])
            fst = sb(pbk, "fst", [128, 2])
            for tl in range(NOWN):
                xb = xr[tl % 2]
                xk = ("xr", tl % 2)
                S.op("sp", lambda e, xb=xb, tl=tl: e.dma_start(out=xb[:, :], in_=x1scr[tl * 128:(tl + 1) * 128, :]), reads=[], writes=[xk], dma="xr%d" % (tl % 2))
                S.op("dve", lambda e, tl=tl: e.tensor_tensor(out=acc[:, tl, :], in0=acc[:, tl, :], in1=G2, op=ALU.mult), reads=[("acc", tl), "modbc"], writes=[("acc", tl)])
                S.op("dve", lambda e, tl=tl, xb=xb: e.tensor_tensor(out=acc[:, tl, :], in0=acc[:, tl, :], in1=xb[:, :], op=ALU.add), reads=[("acc", tl), xk], writes=[("acc", tl)])
                S.op("act", lambda e, tl=tl: e.activation(out=fsq[:, :], in_=acc[:, tl, :], func=AF.Square, accum_out=fst[:, 0:1]), reads=[("acc", tl)], writes=["fsq", "fst"])
                S.op("dve", lambda e: e.tensor_scalar(out=fst[:, 1:2], in0=fst[:, 0:1], scalar1=1.0 / D, scalar2=EPS, op0=ALU.mult, op1=ALU.add), reads=["fst"], writes=["fst1"])
                S.op("act", lambda e: e.activation(out=fst[:, 1:2], in_=fst[:, 1:2], func=AF.Sqrt), reads=["fst1"], writes=["fst1"])
                S.op("dve", lambda e: e.reciprocal(out=fst[:, 1:2], in_=fst[:, 1:2]), reads=["fst1"], writes=["fst1"])
                S.op("dve", lambda e, tl=tl: e.scalar_tensor_tensor(out=acc[:, tl, :], in0=acc[:, tl, :], scalar=fst[:, 1:2], in1=fwbc[:, :], op0=ALU.mult, op1=ALU.mult),
                     reads=[("acc", tl), "fst1", "fwbc"], writes=[("acc", tl)])
                S.op("sp", lambda e, tl=tl: e.dma_start(out=y[tl * 128:(tl + 1) * 128, :], in_=acc[:, tl, :]), reads=[("acc", tl)], dma="yst")
            fin = [("d", nm, v) for nm, v in S.dma_cnt.items()]
            S.ins["sp"].append(dict(fn=None, waits=set(fin), tok=None, dma=None, idx=S.done["sp"] + len(S.ins["sp"])))
            S.emit()
    if os.environ.get('SCHED_DEBUG'):
        print('cbase', S.cbase, 'dma', S.dma_cnt, 'done', S.done)
    return nc, dbg_outs


_CACHE = {}


def prep_inputs(inp, dne=NE):
    f = lambda a: np.ascontiguousarray(np.asarray(a, dtype=np.float32))
    x = f(inp["x"])
    c = f(inp["c"])
    conv_w = f(inp["conv_w"])[0]
    convw = conv_w.reshape(4, 12, 128).transpose(2, 1, 0).reshape(128, 48)
    hg_lb = f(inp["hg_lb"])
    lbcol = np.concatenate([hg_lb[0].reshape(4, 128).T, hg_lb[1].reshape(4, 128).T], axis=1)
    shared = {
        "w_ada": f(inp["w_ada"])[0], "b_ada": f(inp["b_ada"])[0], "norm1_w": f(inp["norm1_w"])[0],
        "w_in": f(inp["w_in"])[0], "convw": f(convw), "a_log": f(inp["gdn_a_log"])[0], "dt_bias": f(inp["gdn_dt_bias"])[0],
        "gdn_nw": f(inp["gdn_norm_w"])[0], "lbcol": f(lbcol), "hg_lb": hg_lb, "hg_nw": f(inp["hg_norm_w"])[0],
        "w_out": f(inp["w_out"])[0], "norm2_w": f(inp["norm2_w"])[0], "w_router": f(inp["w_router"])[0],
        "router_bias": f(inp["router_bias"])[0], "w_gate": f(inp["w_gate"])[0][:dne], "w_up": f(inp["w_up"])[0][:dne],
        "w_down": f(inp["w_down"])[0][:dne], "ws_gate": f(inp["ws_gate"])[0], "ws_up": f(inp["ws_up"])[0],
        "ws_down": f(inp["ws_down"])[0], "final_w": f(inp["final_norm_w"]), "consts": CONST_ARR,
    }
    maps = []
    for core in range(8):
        b, j = core // 4, core % 4
        nreal = (j + 1) * 2048
        xs = np.zeros((T, D), np.float32)
        xs[T - nreal:] = x[b, :nreal]
        m = np.zeros(T, np.float32)
        m[T - nreal:] = 1.0
        d = dict(shared)
        d["xseq"] = xs
        d["tmask"] = np.ascontiguousarray(m.reshape(NT, 128).T)
        d["cvec"] = np.ascontiguousarray(c[b].reshape(8, 128).T)
        maps.append(d)
    return maps


def kernel(**inputs):
    if "nc" not in _CACHE:
        _CACHE["nc"] = build()[0]
    nc = _CACHE["nc"]
    maps = prep_inputs(inputs)
    res = run_bass_kernel_spmd(nc, maps, core_ids=list(range(8)))
    out = np.zeros((2, T, D), np.float32)
    for core in range(8):
        b, j = core // 4, core % 4
        out[b, j * 2048:(j + 1) * 2048] = res.results[core]["y"]
    return out
```

```python
from contextlib import ExitStack
import types
import numpy as np
import concourse.bass as bass
import concourse.mybir as mybir
from concourse.bass_utils import run_bass_kernel_spmd

F32 = mybir.dt.float32
BF16 = mybir.dt.bfloat16
AF = mybir.ActivationFunctionType
ALU = mybir.AluOpType
AX = mybir.AxisListType

D = 1024
T = 8192
TS = 2
NW = TS * 128
NT = 64
OWN0 = 48
NOWN = 16
DIN = 4104
OQ, OK_, OV, OGA, OBETA, OA, OF, OI, OQB, OGB = 0, 512, 1024, 1536, 2048, 2052, 2056, 2568, 3080, 3592
EPS = 1e-6
NE = 64
SAME_ENGINE_SYNC = True
import os
SUB = float(os.environ.get("SUB", "9"))


def _snap(fn):
    if fn is None or not getattr(fn, "__closure__", None):
        return fn
    cells = []
    for c in fn.__closure__:
        try:
            cells.append(types.CellType(c.cell_contents))
        except ValueError:
            cells.append(c)
    return types.FunctionType(fn.__code__, fn.__globals__, fn.__name__, fn.__defaults__, tuple(cells))


class Sched:
    ENGS = ("pe", "dve", "act", "pool", "sp")
    BLK = {"pe": "tensor", "dve": "vector", "act": "scalar", "pool": "gpsimd", "sp": "sync"}

    def __init__(self, nc, stack):
        self.nc = nc
        self.ins = {e: [] for e in self.ENGS}
        self.done = {e: 0 for e in self.ENGS}
        self.lastw = {}
        self.readers = {}
        self.dma_cnt = {}
        self.esem = {e: stack.enter_context(nc.semaphore("c_" + e)) for e in self.ENGS}
        self.dsem = {}
        self.stack = stack
        self.cbase = {e: 0 for e in self.ENGS}
        self.cval = {e: {} for e in self.ENGS}
        self.seen = {e: {} for e in self.ENGS}
        self.bar_waits = set()
        self.bar_pending = set()

    def op(self, eng, fn, reads=(), writes=(), dma=None):
        ex = [k for k in reads if isinstance(k, tuple) and k[0] == "bank"]
        if ex:
            reads = [k for k in reads if k not in ex]
            writes = list(writes) + ex
        waits = set()
        for k in reads:
            t = self.lastw.get(k)
            if t is not None:
                waits.add(t)
        for k in writes:
            t = self.lastw.get(k)
            if t is not None:
                waits.add(t)
            for r in self.readers.get(k, ()):
                waits.add(r)
        idx = self.done[eng] + len(self.ins[eng])
        if dma is not None:
            if dma not in self.dsem:
                self.dsem[dma] = self.stack.enter_context(self.nc.semaphore("d_" + dma))
            self.dma_cnt[dma] = self.dma_cnt.get(dma, 0) + 16
            tok = ("d", dma, self.dma_cnt[dma])
        else:
            tok = ("e", eng, idx)
        w2 = set()
        for w in waits:
            if w[0] == "e" and w[2] < self.done[w[1]]:
                continue
            if w[0] == "e" and w[1] == eng and (eng == "pe" or not SAME_ENGINE_SYNC):
                continue
            w2.add(w)
        if eng in self.bar_pending:
            self.bar_pending.discard(eng)
            w2 |= self.bar_waits
        self.ins[eng].append(dict(fn=_snap(fn), waits=w2, tok=tok, dma=dma, idx=idx))
        for k in reads:
            self.readers.setdefault(k, []).append(tok)
        for k in writes:
            self.lastw[k] = tok
            self.readers[k] = []
        return tok

    def emit(self):
        nc = self.nc
        flagged = {e: set() for e in self.ENGS}
        for e in self.ENGS:
            for ins in self.ins[e]:
                for w in ins["waits"]:
                    if w[0] == "e" and w[2] >= self.done[w[1]]:
                        flagged[w[1]].add(w[2])
        lastc = {}
        for e in self.ENGS:
            for ins in self.ins[e]:
                if ins["dma"] is None and ins["fn"] is not None:
                    lastc[e] = ins["idx"]
            if e in lastc:
                flagged[e].add(lastc[e])
        for e in self.ENGS:
            c = self.cbase[e]
            for ins in self.ins[e]:
                if ins["idx"] in flagged[e]:
                    c += 1
                    self.cval[e][ins["idx"]] = c
            self.cbase[e] = c
        with nc.Block() as block:
            for e in self.ENGS:
                lst = self.ins[e]

                def body(engine, e=e, lst=lst):
                    seen = self.seen[e]
                    for ins in lst:
                        need = {}
                        for w in ins["waits"]:
                            if w[0] == "e":
                                key = ("e", w[1])
                                val = self.cval[w[1]][w[2]]
                            else:
                                key = ("d", w[1])
                                val = w[2]
                            need[key] = max(need.get(key, 0), val)
                        for key, val in need.items():
                            if seen.get(key, 0) >= val:
                                continue
                            seen[key] = val
                            sem = self.esem[key[1]] if key[0] == "e" else self.dsem[key[1]]
                            engine.wait_ge(sem, val)
                        if ins["fn"] is None:
                            continue
                        r = ins["fn"](engine)
                        if ins["dma"] is not None:
                            r.then_inc(self.dsem[ins["dma"]], 16)
                        elif ins["idx"] in flagged[e]:
                            r.then_inc(self.esem[e], 1)

                getattr(block, self.BLK[e])(body)
        bw = set()
        for e in self.ENGS:
            self.last_idx = getattr(self, "last_idx", {})
            if e in lastc:
                self.last_idx[e] = lastc[e]
            self.done[e] += len(self.ins[e])
            self.ins[e] = []
        for e, i in getattr(self, "last_idx", {}).items():
            bw.add(("e", e, i))
        for nm, v in self.dma_cnt.items():
            bw.add(("d", nm, v))
        self.bar_waits = bw
        self.bar_pending = set(self.ENGS)


F32R = mybir.dt.float32r
USE_F32R = os.environ.get("NO_F32R") is None
USE_TRP = os.environ.get("NO_TRP") is None


def mmr(e, out, lhsT, rhs, start=True, stop=True):
    if USE_F32R:
        return e.matmul(out, lhsT.bitcast(F32R), rhs.bitcast(F32R), start=start, stop=stop)
    return e.matmul(out, lhsT, rhs, start=start, stop=stop)


def rr(ap):
    return ap.bitcast(F32R) if USE_F32R else ap


R_CHAIN = USE_F32R and os.environ.get("NO_F32R_CHAIN") is None
R_CUM = USE_F32R and os.environ.get("NO_F32R_CUM") is None


def rc(ap):
    return ap.bitcast(F32R) if R_CHAIN else ap


def ru(ap):
    return ap.bitcast(F32R) if R_CUM else ap


CH_MASK = int(os.environ.get("CH_MASK", "14"))


def mm_c(e, out, lhsT, rhs, bit=0):
    if R_CHAIN and (CH_MASK & bit):
        return e.matmul(out, lhsT.bitcast(F32R), rhs.bitcast(F32R), start=True, stop=True)
    return e.matmul(out, lhsT, rhs, start=True, stop=True)


def mm_u(e, out, lhsT, rhs):
    if R_CUM:
        return e.matmul(out, lhsT.bitcast(F32R), rhs.bitcast(F32R), start=True, stop=True)
    return e.matmul(out, lhsT, rhs, start=True, stop=True)


def make_consts():
    c = {}
    p = np.arange(128)
    same = (p[:, None] // 64) == (p[None, :] // 64)
    c["ident"] = np.eye(128, dtype=np.float32)
    c["triu"] = (same & (p[:, None] <= p[None, :])).astype(np.float32)
    c["rev"] = (same & (p[:, None] > p[None, :])).astype(np.float32)
    c["blk"] = same.astype(np.float32)
    c["strictl"] = (same & (p[:, None] > p[None, :])).astype(np.float32)
    c["ones"] = np.ones((128, 128), np.float32)
    ci = np.zeros((128, 128), np.float32)
    ci[:64, 0] = 1.0
    ci[64:, 1] = 1.0
    c["cind"] = ci
    names = ["ident", "triu", "rev", "blk", "strictl", "ones", "cind"]
    arr = np.concatenate([c[n] for n in names], axis=1)
    offs = {n: i * 128 for i, n in enumerate(names)}
    return arr, offs


CONST_ARR, COFF = make_consts()
NCONST = CONST_ARR.shape[1]


def build(debug=(), stage=9, nsi=NT // TS, nex=NE + 1, dne=NE):
    nc = bass.Bass("TRN2", target_bir_lowering=False)
    dbg_outs = {}

    def din(name, shape):
        return nc.dram_tensor(name, list(shape), F32, kind="ExternalInput").ap()

    xseq = din("xseq", [T, D])
    tmask = din("tmask", [128, NT])
    cvec = din("cvec", [128, 8])
    w_ada = din("w_ada", [D, 6 * D])
    b_ada = din("b_ada", [6 * D])
    norm1_w = din("norm1_w", [D])
    w_in = din("w_in", [D, DIN])
    convw = din("convw", [128, 48])
    a_log = din("a_log", [4])
    dt_bias = din("dt_bias", [4])
    gdn_nw = din("gdn_nw", [128])
    lbcol = din("lbcol", [128, 8])
    hg_lb = din("hg_lb", [2, 512])
    hg_nw = din("hg_nw", [512])
    w_out = din("w_out", [D, D])
    norm2_w = din("norm2_w", [D])
    w_router = din("w_router", [D, NE])
    router_bias = din("router_bias", [NE])
    w_gate = din("w_gate", [dne, D, 256])
    w_up = din("w_up", [dne, D, 256])
    w_down = din("w_down", [dne, 256, D])
    ws_gate = din("ws_gate", [D, 256])
    ws_up = din("ws_up", [D, 256])
    ws_down = din("ws_down", [256, D])
    final_w = din("final_w", [D])
    consts = din("consts", [128, NCONST])
    y = nc.dram_tensor("y", [NOWN * 128, D], F32, kind="ExternalOutput").ap()
    x1scr = nc.dram_tensor("x1scr", [NOWN * 128, D], F32)
    win_bf = nc.dram_tensor("win_bf", [D, DIN], BF16)
    wout_bf = nc.dram_tensor("wout_bf", [D, D], BF16)
    wg_bf = nc.dram_tensor("wg_bf", [NE + 1, D, 256], BF16)
    wu_bf = nc.dram_tensor("wu_bf", [NE + 1, D, 256], BF16)
    wd_bf = nc.dram_tensor("wd_bf", [NE + 1, 256, D], BF16)
    h2scr = nc.dram_tensor("h2scr", [NOWN, 128, 8 * 128], BF16)

    top = ExitStack()
    with top:
        S = Sched(nc, top)

        def sb(stack, name, shape, dt=F32):
            return stack.enter_context(nc.sbuf_tensor(name, list(shape), dt))

        banks = [top.enter_context(nc.psum_tensor("bank%d" % i, [128, 512], F32)) for i in range(8)]
        full_rr = [0]
        q_rr = [0]

        def pfull():
            i = full_rr[0] % 3
            full_rr[0] += 1
            return banks[i], ("bank", i)

        def pq():
            i = q_rr[0] % 4
            q_rr[0] += 1
            return banks[4 + i][:, 0:128], ("bank", 4 + i)

        tbank_bf = banks[3][:, :].bitcast(BF16)
        TB = ("bank", 3)

        cst = sb(top, "cst", [128, NCONST])
        cst_bf = sb(top, "cst_bf", [128, 128], BF16)
        modbc = sb(top, "modbc", [128, 6 * D])
        fwbc = sb(top, "fwbc", [128, D])
        gates = sb(top, "gates", [128, NOWN, NE + 1])
        tmk = sb(top, "tmk", [128, NT])
        one_col = sb(top, "one_col", [128, 1])

        def C(n):
            return cst[:, COFF[n]:COFF[n] + 128]

        cstr = sb(top, "cstr", [128, NCONST])

        def CR(n):
            return cstr[:, COFF[n]:COFF[n] + 128]

        def trp(e, out, in_):
            if USE_TRP:
                return e.transpose(out=out, in_=in_, identity=C("ident"))
            return e.matmul(out, in_, C("ident"), start=True, stop=True)

        dma_i = [0]

        def dma_load(out_ap, in_ap, wkeys, eng="sp", rkeys=()):
            k0 = wkeys[0]
            nm = "ld_" + (k0 if isinstance(k0, str) else "_".join(str(z) for z in k0))
            S.op(eng, lambda e: e.dma_start(out=out_ap, in_=in_ap), reads=rkeys, writes=wkeys, dma=nm)

        def dump(name, ap, shape, key, r0=None, r1=None):
            if name not in debug:
                return
            if name not in dbg_outs:
                dbg_outs[name] = nc.dram_tensor("dbg_" + name, list(shape), F32, kind="ExternalOutput").ap()
            t = dbg_outs[name]
            tt = t if r0 is None else t[r0:r1]
            S.op("sp", lambda e: e.dma_start(out=tt, in_=ap), reads=[key] if not isinstance(key, list) else key, dma="dbg_" + name)

        fin_tokens = []

        dma_load(cst[:, :], consts, ["cst"])
        dma_load(tmk[:, :], tmask, ["tmk"])
        S.op("dve", lambda e: e.tensor_copy(out=cst_bf[:, :], in_=C("ident")), reads=["cst"], writes=["cst_bf"])
        S.op("dve", lambda e: e.memset(one_col[:, :], 1.0), writes=["one_col"])
        S.op("dve", lambda e: e.tensor_copy(out=ru(cstr[:, :]), in_=cst[:, :]), reads=["cst"], writes=["cstr"])

        pa = ExitStack()
        with pa:
            wr = sb(pa, "wr", [128, 8, NE])
            wsl = [sb(pa, "wsl%d" % i, [128, 8, 512], BF16) for i in range(3)]
            wsl_rr = [0]

            def flat2(ap_, total):
                return ap_.rearrange("(a b) -> a b", b=2048) if False else ap_

            def cast_flat(dst, src, total, key):
                rows = total // 2048
                r0 = 0
                while r0 < rows:
                    n = min(1024, rows - r0)
                    dma_load(dst[r0:r0 + n, :], src[r0:r0 + n, :], [key], eng="pool")
                    r0 += n

            cast_flat(win_bf[:, :].rearrange("a b -> (a b)").rearrange("(r c) -> r c", c=2048),
                      w_in.rearrange("a b -> (a b)").rearrange("(r c) -> r c", c=2048), D * DIN, "win_bf")
            cast_flat(wout_bf[:, :].rearrange("a b -> (a b)").rearrange("(r c) -> r c", c=2048),
                      w_out.rearrange("a b -> (a b)").rearrange("(r c) -> r c", c=2048), D * D, "wout_bf")

            def flatv(ap_, pat):
                return ap_.rearrange(pat).rearrange("(r c) -> r c", c=2048)

            def cast_experts():
                cast_flat(flatv(wg_bf[0:dne], "e a b -> (e a b)"), flatv(w_gate, "e a b -> (e a b)"), dne * D * 256, "wg_bf")
                cast_flat(flatv(wu_bf[0:dne], "e a b -> (e a b)"), flatv(w_up, "e a b -> (e a b)"), dne * D * 256, "wu_bf")
                cast_flat(flatv(wd_bf[0:dne], "e a b -> (e a b)"), flatv(w_down, "e a b -> (e a b)"), dne * D * 256, "wd_bf")
                cast_flat(flatv(wg_bf[NE], "a b -> (a b)"), flatv(ws_gate, "a b -> (a b)"), D * 256, "wg_bf")
                cast_flat(flatv(wu_bf[NE], "a b -> (a b)"), flatv(ws_up, "a b -> (a b)"), D * 256, "wu_bf")
                cast_flat(flatv(wd_bf[NE], "a b -> (a b)"), flatv(ws_down, "a b -> (a b)"), D * 256, "wd_bf")

            def wload(src_bf, key, col0, n):
                i = wsl_rr[0] % 3
                wsl_rr[0] += 1
                dma_load(wsl[i][:, :, 0:n], src_bf[:, col0:col0 + n].rearrange("(kc p) n -> p kc n", p=128), [("wsl", i)], rkeys=[key])
                return wsl[i], ("wsl", i)

            dma_load(wr[:, :, :], w_router.rearrange("(kc p) n -> p kc n", p=128), ["wr"])

            cw = sb(pa, "cw", [128, 48])
            gnw = sb(pa, "gnw", [128, 128])
            hnw = sb(pa, "hnw", [128, 512])
            rbb = sb(pa, "rbb", [128, NE])
            adt = sb(pa, "adt", [128, 8])
            negA = sb(pa, "negA", [128, 4])
            lbv = sb(pa, "lbv", [128, 4])
            omlv = sb(pa, "omlv", [128, 4])
            lbB = sb(pa, "lbB", [128, 512])
            omlB = sb(pa, "omlB", [128, 512])
            p0 = ExitStack()
            p0.__enter__()
            lbc = sb(p0, "lbc", [128, 8])
            lbb = sb(p0, "lbb", [128, 2, 512])
            n1bc = sb(p0, "n1bc", [128, D])
            n2bc = sb(p0, "n2bc", [128, D])
            cv = sb(p0, "cv", [128, 8])
            cB = sb(p0, "cB", [128, 8, 128])
            wada = [sb(p0, "wada%d" % i, [128, 8, 512]) for i in range(2)]
            dma_load(cw[:, :], convw, ["cw"])
            dma_load(lbc[:, :], lbcol, ["lbc"])
            dma_load(lbb[:, 0, :], hg_lb[0, :].partition_broadcast(128), ["lbb"])
            dma_load(lbb[:, 1, :], hg_lb[1, :].partition_broadcast(128), ["lbb"])
            dma_load(n1bc[:, :], norm1_w.partition_broadcast(128), ["n1bc"])
            dma_load(n2bc[:, :], norm2_w.partition_broadcast(128), ["n2bc"])
            dma_load(fwbc[:, :], final_w.partition_broadcast(128), ["fwbc"])
            dma_load(gnw[:, :], gdn_nw.partition_broadcast(128), ["gnw"])
            dma_load(hnw[:, :], hg_nw.partition_broadcast(128), ["hnw"])
            dma_load(rbb[:, :], router_bias.partition_broadcast(128), ["rbb"])
            dma_load(adt[:, 0:4], a_log.partition_broadcast(128), ["adt"])
            dma_load(adt[:, 4:8], dt_bias.partition_broadcast(128), ["adt"])
            S.op("act", lambda e: e.activation(out=negA[:, :], in_=adt[:, 0:4], func=AF.Exp), reads=["adt"], writes=["negA"])
            S.op("dve", lambda e: e.tensor_scalar(out=negA[:, :], in0=negA[:, :], scalar1=-1.0, scalar2=None, op0=ALU.mult),
                 reads=["negA"], writes=["negA"])
            S.op("dve", lambda e: e.tensor_tensor(out=lbv[:, :], in0=lbc[:, 0:4], in1=lbc[:, 4:8], op=ALU.subtract),
                 reads=["lbc"], writes=["lbv"])
            S.op("act", lambda e: e.activation(out=lbv[:, :], in_=lbv[:, :], func=AF.Sigmoid), reads=["lbv"], writes=["lbv"])
            S.op("dve", lambda e: e.tensor_scalar(out=omlv[:, :], in0=lbv[:, :], scalar1=-1.0, scalar2=1.0, op0=ALU.mult, op1=ALU.add),
                 reads=["lbv"], writes=["omlv"])
            S.op("dve", lambda e: e.tensor_tensor(out=lbB[:, :], in0=lbb[:, 0, :], in1=lbb[:, 1, :], op=ALU.subtract),
                 reads=["lbb"], writes=["lbB"])
            S.op("act", lambda e: e.activation(out=lbB[:, :], in_=lbB[:, :], func=AF.Sigmoid), reads=["lbB"], writes=["lbB"])
            S.op("dve", lambda e: e.tensor_scalar(out=omlB[:, :], in0=lbB[:, :], scalar1=-1.0, scalar2=1.0, op0=ALU.mult, op1=ALU.add),
                 reads=["lbB"], writes=["omlB"])

            dma_load(cv[:, :], cvec, ["cv"])
            S.op("act", lambda e: e.activation(out=cv[:, :], in_=cv[:, :], func=AF.Silu), reads=["cv"], writes=["cv"])
            for kc in range(8):
                S.op("dve", lambda e, kc=kc: e.tensor_scalar(out=cB[:, kc, :], in0=C("ones"), scalar1=cv[:, kc:kc + 1], scalar2=None, op0=ALU.mult),
                     reads=["cst", "cv"], writes=[("cB", kc)])
            dma_load(modbc[:, :], b_ada.partition_broadcast(128), ["modbc"])
            for g in range(12):
                wa = wada[g % 2]
                wk = ("wada", g % 2)
                dma_load(wa[:, :, :], w_ada[:, g * 512:(g + 1) * 512].rearrange("(kc p) n -> p kc n", p=128), [wk])
                pb, pk = pfull()
                for kc in range(8):
                    S.op("pe", lambda e, kc=kc, wa=wa, pb=pb: e.matmul(pb[:, :], cB[:, kc, :], wa[:, kc, :], start=(kc == 0), stop=(kc == 7)),
                         reads=[("cB", kc), wk], writes=[pk])
                S.op("dve", lambda e, g=g, pb=pb: e.tensor_tensor(out=modbc[:, g * 512:(g + 1) * 512], in0=pb[:, :], in1=modbc[:, g * 512:(g + 1) * 512], op=ALU.add),
                     reads=[pk, "modbc"], writes=["modbc"])
            SH1, SC1, G1, SH2, SC2, G2 = [modbc[:, i * D:(i + 1) * D] for i in range(6)]
            S.op("dve", lambda e: e.scalar_tensor_tensor(out=SC1, in0=SC1, scalar=1.0, in1=n1bc[:, :], op0=ALU.add, op1=ALU.mult),
                 reads=["modbc", "n1bc"], writes=["modbc"])
            S.op("dve", lambda e: e.scalar_tensor_tensor(out=SC2, in0=SC2, scalar=1.0, in1=n2bc[:, :], op0=ALU.add, op1=ALU.mult),
                 reads=["modbc", "n2bc"], writes=["modbc"])

            S.emit()
            p0.close()
            cast_experts()

            xt = [sb(pa, "xt%d" % i, [128, D]) for i in range(2)]
            sq = sb(pa, "sq", [128, D])
            hb = sb(pa, "hb", [128, D], BF16)
            hT = sb(pa, "hT", [128, 8, NW], BF16)
            pj = sb(pa, "pj", [128, 12, NW + 3])
            fT = sb(pa, "fT", [128, 4, NW])
            qbT = sb(pa, "qbT", [128, 4, NW])
            cvb = sb(pa, "cvb", [128, 12, NW])
            for g in range(12):
                S.op("pool", lambda e, g=g: e.memset(pj[:, g, 0:3], 0.0), writes=[("pj", g)])
            tmpf = sb(pa, "tmpf", [128, NW])
            tmpg = sb(pa, "tmpg", [128, NW])
            tsq = sb(pa, "tsq", [128, NW])
            Sg = [sb(pa, "Sg%d" % h, [128, 128]) for h in range(4)]
            Sh = [sb(pa, "Sh%d" % h, [128, 128]) for h in range(4)]
            for h in range(4):
                S.op("dve", lambda e, h=h: e.tensor_scalar(out=rc(Sg[h][:, :]), in0=C("ones"), scalar1=0.0, scalar2=None, op0=ALU.mult), reads=["cst"], writes=[("Sg", h)])
                S.op("pool", lambda e, h=h: e.memset(Sh[h][:, :], 0.0), writes=[("Sh", h)])
            ba = sb(pa, "ba", [128, 8])
            beta = sb(pa, "beta", [128, 4])
            gg = sb(pa, "gg", [128, 4])
            gc = sb(pa, "gc", [128, 4])
            gl = sb(pa, "gl", [128, 4])
            egc = sb(pa, "egc", [128, 4])
            ekt = sb(pa, "ekt", [128, 4])
            bek = sb(pa, "bek", [128, 4])
            Gb = sb(pa, "Gb", [128, 128])
            gcb = sb(pa, "gcb", [128, 128])
            glb = sb(pa, "glb", [128, 2])
            dec = sb(pa, "dec", [128, 128])
            decT = sb(pa, "decT", [128, 128])
            ktok = sb(pa, "ktok", [128, 128])
            kT2 = sb(pa, "kT2", [128, 128])
            vtok = sb(pa, "vtok", [128, 128])
            kbg = sb(pa, "kbg", [128, 128])
            bv = sb(pa, "bv", [128, 128])
            ktl = sb(pa, "ktl", [128, 128])
            Pm = [sb(pa, "Pm%d" % i, [128, 128]) for i in range(2)]
            Qm = [sb(pa, "Qm%d" % i, [128, 128]) for i in range(2)]
            Rm = [sb(pa, "Rm%d" % i, [128, 128]) for i in range(2)]
            ut = sb(pa, "ut", [128, 128])
            wT = sb(pa, "wT", [128, 128])
            vnew = sb(pa, "vnew", [128, 128])
            qdT = sb(pa, "qdT", [128, 128])
            atT = sb(pa, "atT", [128, 128])
            otok = sb(pa, "otok", [128, D])
            ob = sb(pa, "ob", [128, D], BF16)
            oT = sb(pa, "oT", [128, 8, 128], BF16)
            gateS = sb(pa, "gateS", [128, D])
            fv = sb(pa, "fv", [128, 512])
            logf = sb(pa, "logf", [128, 512])
            kh = sb(pa, "kh", [128, 512])
            ktail = sb(pa, "ktail", [128, 512])
            vh = sb(pa, "vh", [128, 512])
            ebl = sb(pa, "ebl", [128, 4, 2])
            ebT = sb(pa, "ebT", [128, 128])
            enbT = sb(pa, "enbT", [128, 128])
            qtT = sb(pa, "qtT", [128, 128])
            ktT = sb(pa, "ktT", [128, 128])
            AT = sb(pa, "AT", [128, 128])
            stat = sb(pa, "stat", [128, 16])
            x1 = sb(pa, "x1", [128, D])
            h2f = sb(pa, "h2f", [128, D])
            h2b = sb(pa, "h2b", [128, D], BF16)
            h2Tf = sb(pa, "h2Tf", [128, 8, 128])
            h2Tt = sb(pa, "h2Tt", [128, 8 * 128], BF16)
            rsc = sb(pa, "rsc", [128, NE])
            rsel = sb(pa, "rsel", [128, NE])
            rtmp = sb(pa, "rtmp", [128, NE])
            rg8 = sb(pa, "rg8", [128, 8, 4])

            def _two(name, first):
                return [first, sb(pa, name + "_b", [128, 128])]
            ktok_s = _two("ktok", ktok); bv_s = _two("bv", bv); kbg_s = _two("kbg", kbg); ktl_s = _two("ktl", ktl)
            Gb_s = _two("Gb", Gb); dec_s = _two("dec", dec); decT_s = _two("decT", decT); kT2_s = _two("kT2", kT2)
            ut_s = _two("ut", ut); wT_s = _two("wT", wT); vnew_s = _two("vnew", vnew); qdT_s = _two("qdT", qdT); atT_s = _two("atT", atT)
            glb_s = [glb, sb(pa, "glb_b", [128, 2])]
            Pm_s = [Pm, [sb(pa, "Pm_b%d" % i, [128, 128]) for i in range(2)]]
            Qm_s = [Qm, [sb(pa, "Qm_b%d" % i, [128, 128]) for i in range(2)]]
            Rm_s = [Rm, [sb(pa, "Rm_b%d" % i, [128, 128]) for i in range(2)]]
            mulmask = C("blk")
            if os.environ.get("SBUF_DEBUG"):
                print("SBUF remaining", nc.sbuf_bytes_remaining)

            for si in range(NT // TS - nsi, NT // TS):
                if stage < 1:
                    break
                full_s = (si * TS >= OWN0)
                for j in range(TS):
                    ti = si * TS + j
                    xb = xt[ti % 2]
                    xk = ("xt", ti % 2)
                    dma_load(xb[:, :], xseq[ti * 128:(ti + 1) * 128, :], [xk])
                    S.op("act", lambda e, xb=xb: e.activation(out=sq[:, :], in_=xb[:, :], func=AF.Square, accum_out=stat[:, 0:1]),
                         reads=[xk], writes=["sq", "stat0"])
                    S.op("dve", lambda e: e.tensor_scalar(out=stat[:, 1:2], in0=stat[:, 0:1], scalar1=1.0 / D, scalar2=EPS, op0=ALU.mult, op1=ALU.add),
                         reads=["stat0"], writes=["stat1"])
                    S.op("act", lambda e: e.activation(out=stat[:, 1:2], in_=stat[:, 1:2], func=AF.Sqrt), reads=["stat1"], writes=["stat1"])
                    S.op("dve", lambda e: e.reciprocal(out=stat[:, 1:2], in_=stat[:, 1:2]), reads=["stat1"], writes=["stat1"])
                    S.op("dve", lambda e, xb=xb: e.scalar_tensor_tensor(out=sq[:, :], in0=xb[:, :], scalar=stat[:, 1:2], in1=SC1, op0=ALU.mult, op1=ALU.mult),
                         reads=[xk, "stat1", "modbc"], writes=["sq"])
                    S.op("dve", lambda e: e.tensor_tensor(out=sq[:, :], in0=sq[:, :], in1=SH1, op=ALU.add), reads=["sq", "modbc"], writes=["sq"])
                    S.op("dve", lambda e, ti=ti: e.tensor_scalar(out=hb[:, :], in0=sq[:, :], scalar1=tmk[:, ti:ti + 1], scalar2=None, op0=ALU.mult),
                         reads=["sq", "tmk"], writes=["hb"])
                    if OWN0 <= ti < OWN0 + 2:
                        dump("hchk", sq[:, :], [256, D], "sq", (ti - OWN0) * 128, (ti - OWN0 + 1) * 128)
                    for kc in range(8):
                        S.op("pe", lambda e, kc=kc: e.transpose(out=tbank_bf[:, kc * 128:(kc + 1) * 128], in_=hb[:, kc * 128:(kc + 1) * 128], identity=cst_bf[:, :]),
                             reads=["hb", "cst_bf"], writes=[TB])
                    S.op("act", lambda e, j=j: e.activation(out=hT[:, :, j * 128:(j + 1) * 128],
                                                            in_=tbank_bf.rearrange("p (k t) -> p k t", k=8), func=AF.Copy),
                         reads=[TB], writes=[("hT", j)])
                hTk = [("hT", j) for j in range(TS)]

                if stage < 2:
                    continue
                def proj_fm(wsb, wk, c0, evac):
                    pb, pk = pfull()
                    for kc in range(8):
                        S.op("pe", lambda e, kc=kc, pb=pb: e.matmul(pb[:, 0:NW], wsb[:, kc, c0:c0 + 128], hT[:, kc, :], start=(kc == 0), stop=(kc == 7)),
                             reads=[wk] + hTk, writes=[pk])
                    evac(pb, pk)

                groups = list(range(4, 12)) if (si + 1) * TS < OWN0 else list(range(12))
                for g in groups:
                    if g % 4 == 0:
                        wsb, wk = wload(win_bf, "win_bf", g * 128, 512)
                    def ev(pb, pk, g=g):
                        S.op("act", lambda e: e.activation(out=pj[:, g, 3:NW + 3], in_=pb[:, 0:NW], func=AF.Copy),
                             reads=[pk], writes=[("pj", g)])
                    proj_fm(wsb, wk, (g % 4) * 128, ev)
                    S.op("dve", lambda e, g=g: e.tensor_scalar(out=tmpf[:, :], in0=pj[:, g, 3:NW + 3], scalar1=cw[:, g * 4 + 3:g * 4 + 4], scalar2=None, op0=ALU.mult),
                         reads=[("pj", g), "cw"], writes=["tmpf"])
                    for tap in (2, 1, 0):
                        S.op("dve", lambda e, g=g, tap=tap: e.scalar_tensor_tensor(out=tmpf[:, :], in0=pj[:, g, tap:tap + NW], scalar=cw[:, g * 4 + tap:g * 4 + tap + 1],
                                                                                   in1=tmpf[:, :], op0=ALU.mult, op1=ALU.add),
                             reads=[("pj", g), "cw", "tmpf"], writes=["tmpf"])
                    S.op("pool", lambda e, g=g: e.tensor_copy(out=pj[:, g, 0:3], in_=pj[:, g, NW:NW + 3]), reads=[("pj", g)], writes=[("pj", g)])
                    S.op("act", lambda e, g=g: e.activation(out=cvb[:, g, :], in_=tmpf[:, :], func=AF.Silu), reads=["tmpf"], writes=[("cvb", g)])
                    if g < 8:
                        S.op("act", lambda e, g=g: e.activation(out=ru(tsq[:, :]), in_=cvb[:, g, :], func=AF.Square), reads=[("cvb", g)], writes=["tsq"])
                        pb, pk = pfull()
                        S.op("pe", lambda e, pb=pb: mm_u(e, pb[:, 0:NW], CR("ones"), tsq[:, :]), reads=["cstr", "tsq"], writes=[pk])
                        sc = EPS if g >= 4 else EPS
                        S.op("dve", lambda e, pb=pb: e.tensor_scalar(out=tmpg[:, :], in0=pb[:, 0:NW], scalar1=EPS, scalar2=None, op0=ALU.add), reads=[pk], writes=["tmpg"])
                        S.op("act", lambda e: e.activation(out=tmpg[:, :], in_=tmpg[:, :], func=AF.Sqrt), reads=["tmpg"], writes=["tmpg"])
                        S.op("dve", lambda e: e.reciprocal(out=tmpg[:, :], in_=tmpg[:, :]), reads=["tmpg"], writes=["tmpg"])
                        if g < 4:
                            S.op("dve", lambda e, g=g: e.scalar_tensor_tensor(out=cvb[:, g, :], in0=cvb[:, g, :], scalar=128.0 ** -0.5, in1=tmpg[:, :], op0=ALU.mult, op1=ALU.mult),
                                 reads=[("cvb", g), "tmpg"], writes=[("cvb", g)])
                        else:
                            S.op("dve", lambda e, g=g: e.tensor_tensor(out=rc(cvb[:, g, :]), in0=cvb[:, g, :], in1=tmpg[:, :], op=ALU.mult),
                                 reads=[("cvb", g), "tmpg"], writes=[("cvb", g)])
                if full_s:
                    wsb, wk = wload(win_bf, "win_bf", OF, 512)
                    for h in range(4):
                        def evf(pb, pk, h=h):
                            S.op("act", lambda e: e.activation(out=fT[:, h, :], in_=pb[:, 0:NW], func=AF.Sigmoid, scale=-1.0), reads=[pk], writes=[("fT", h)])
                        proj_fm(wsb, wk, h * 128, evf)
                    wsb, wk = wload(win_bf, "win_bf", OQB, 512)
                    for h in range(4):
                        def evq(pb, pk, h=h):
                            S.op("act", lambda e: e.activation(out=qbT[:, h, :], in_=pb[:, 0:NW], func=AF.Silu), reads=[pk], writes=[("qbT", h)])
                        proj_fm(wsb, wk, h * 128, evq)

                if si * TS == OWN0:
                    dump("cvb", cvb[:, :, :].rearrange("p g t -> p (g t)"), [128, 12 * NW], [("cvb", g) for g in range(12)])
                if stage < 3:
                    continue
                for j in range(TS):
                    ti = si * TS + j
                    full = ti >= OWN0
                    tsl = slice(j * 128, (j + 1) * 128)

                    def proj_tm(col0, n):
                        wsb, wk = wload(win_bf, "win_bf", col0, n)
                        pb, pk = pfull()
                        for kc in range(8):
                            S.op("pe", lambda e, kc=kc, pb=pb: e.matmul(pb[:, 0:n], hT[:, kc, tsl], wsb[:, kc, 0:n], start=(kc == 0), stop=(kc == 7)),
                                 reads=[wk, ("hT", j)], writes=[pk])
                        return pb, pk

                    pb, pk = proj_tm(OBETA, 8)
                    S.op("act", lambda e, pb=pb: e.activation(out=beta[:, :], in_=pb[:, 0:4], func=AF.Sigmoid), reads=[pk], writes=["beta"])
                    S.op("dve", lambda e, pb=pb: e.tensor_tensor(out=ba[:, 4:8], in0=pb[:, 4:8], in1=adt[:, 4:8], op=ALU.add), reads=[pk, "adt"], writes=["ba"])
                    S.op("act", lambda e: e.activation(out=ba[:, 4:8], in_=ba[:, 4:8], func=AF.Exp), reads=["ba"], writes=["ba"])
                    S.op("act", lambda e: e.activation(out=ba[:, 4:8], in_=ba[:, 4:8], func=AF.Ln, bias=one_col[:, 0:1]), reads=["ba", "one_col"], writes=["ba"])
                    S.op("dve", lambda e: e.tensor_tensor(out=ru(gg[:, :]), in0=ba[:, 4:8], in1=negA[:, :], op=ALU.mult), reads=["ba", "negA"], writes=["gg"])
                    if stage == 3 and SUB < 1:
                        continue
                    p1, k1 = pq()
                    S.op("pe", lambda e, p1=p1: mm_u(e, p1[:, 0:4], CR("triu"), gg[:, :]), reads=["cstr", "gg"], writes=[k1])
                    S.op("dve", lambda e, p1=p1: e.tensor_copy(out=gc[:, :], in_=p1[:, 0:4]), reads=[k1], writes=["gc"])
                    p2, k2 = pq()
                    S.op("pe", lambda e, p2=p2: mm_u(e, p2[:, 0:4], CR("blk"), gg[:, :]), reads=["cstr", "gg"], writes=[k2])
                    S.op("dve", lambda e, p2=p2: e.tensor_tensor(out=gl[:, :], in0=p2[:, 0:4], in1=gc[:, :], op=ALU.subtract), reads=[k2, "gc"], writes=["gl"])
                    S.op("act", lambda e: e.activation(out=ekt[:, :], in_=gl[:, :], func=AF.Exp), reads=["gl"], writes=["ekt"])
                    S.op("act", lambda e: e.activation(out=egc[:, :], in_=gc[:, :], func=AF.Exp), reads=["gc"], writes=["egc"])
                    S.op("dve", lambda e: e.tensor_tensor(out=bek[:, :], in0=egc[:, :], in1=beta[:, :], op=ALU.mult), reads=["egc", "beta"], writes=["bek"])

                    if OWN0 <= ti < OWN0 + 2:
                        r0 = (ti - OWN0) * 128
                        dump("beta", beta[:, :], [256, 4], "beta", r0, r0 + 128)
                        dump("gg", gg[:, :], [256, 4], "gg", r0, r0 + 128)
                        dump("gc", gc[:, :], [256, 4], "gc", r0, r0 + 128)
                    if stage == 3 and SUB < 2:
                        continue
                    if full:
                        pbA, pkA = proj_tm(OGA, 512)
                        S.op("act", lambda e, pbA=pbA: e.activation(out=gateS[:, 0:512], in_=pbA[:, :], func=AF.Silu), reads=[pkA], writes=["gateA"])
                        pbB, pkB = proj_tm(OGB, 512)
                        S.op("act", lambda e, pbB=pbB: e.activation(out=gateS[:, 512:1024], in_=pbB[:, :], func=AF.Silu), reads=[pkB], writes=["gateB"])

                    def gdn_gen(h, bs):
                        gq, gk, gv = h, 4 + h, 8 + h
                        kTt = cvb[:, gk, tsl]
                        qTt = cvb[:, gq, tsl]
                        yield
                        p, k_ = pq()
                        S.op("pe", lambda e, p=p, kTt=kTt: trp(e, p, kTt), reads=[("cvb", gk), "cst"], writes=[k_])
                        S.op("act", lambda e, p=p: e.activation(out=ktok_s[bs][:, :], in_=p, func=AF.Copy), reads=[k_], writes=[("ktok", bs)])
                        yield
                        p, k_ = pq()
                        S.op("pe", lambda e, p=p, gv=gv: trp(e, p, cvb[:, gv, tsl]), reads=[("cvb", gv), "cst"], writes=[k_])
                        S.op("dve", lambda e, p=p, h=h: e.tensor_scalar(out=rr(bv_s[bs][:, :]), in0=p, scalar1=beta[:, h:h + 1], scalar2=None, op0=ALU.mult),
                             reads=[k_, "beta"], writes=[("bv", bs)])
                        S.op("dve", lambda e, h=h: e.tensor_scalar(out=rr(kbg_s[bs][:, :]), in0=ktok_s[bs][:, :], scalar1=bek[:, h:h + 1], scalar2=None, op0=ALU.mult),
                             reads=[("ktok", bs), "bek"], writes=[("kbg", bs)])
                        S.op("dve", lambda e, h=h: e.tensor_scalar(out=rc(ktl_s[bs][:, :]), in0=ktok_s[bs][:, :], scalar1=ekt[:, h:h + 1], scalar2=None, op0=ALU.mult),
                             reads=[("ktok", bs), "ekt"], writes=[("ktl", bs)])
                        if stage == 4 and SUB < 1:
                            return
                        S.op("dve", lambda e, h=h: e.tensor_scalar(out=ru(Gb_s[bs][:, :]), in0=C("ones"), scalar1=gg[:, h:h + 1], scalar2=None, op0=ALU.mult),
                             reads=["cst", "gg"], writes=[("Gb", bs)])
                        yield
                        pg, kg = pq()
                        S.op("pe", lambda e, pg=pg: mm_u(e, pg, Gb_s[bs][:, :], CR("triu")), reads=[("Gb", bs), "cstr"], writes=[kg])
                        yield
                        pl, kl = pq()
                        S.op("pe", lambda e, pl=pl: mm_u(e, pl[:, 0:2], Gb_s[bs][:, :], CR("cind")[:, 0:2]), reads=[("Gb", bs), "cstr"], writes=[kl])
                        S.op("act", lambda e, pl=pl: e.activation(out=glb_s[bs][:, :], in_=pl[:, 0:2], func=AF.Exp), reads=[kl], writes=[("glb", bs)])
                        S.op("dve", lambda e, pg=pg, h=h: e.tensor_scalar(out=dec_s[bs][:, :], in0=pg, scalar1=gc[:, h:h + 1], scalar2=0.0, op0=ALU.subtract, op1=ALU.max),
                             reads=[kg, "gc"], writes=[("dec", bs)])
                        S.op("act", lambda e: e.activation(out=dec_s[bs][:, :], in_=dec_s[bs][:, :], func=AF.Exp, scale=-1.0), reads=[("dec", bs)], writes=[("dec", bs)])
                        if full:
                            S.op("dve", lambda e, pg=pg, h=h: e.tensor_scalar(out=decT_s[bs][:, :], in0=pg, scalar1=gc[:, h:h + 1], scalar2=0.0, op0=ALU.subtract, op1=ALU.min),
                                 reads=[kg, "gc"], writes=[("decT", bs)])
                            S.op("act", lambda e: e.activation(out=decT_s[bs][:, :], in_=decT_s[bs][:, :], func=AF.Exp), reads=[("decT", bs)], writes=[("decT", bs)])
                            S.op("act", lambda e, pg=pg: e.activation(out=qdT_s[bs][:, :], in_=pg, func=AF.Exp), reads=[kg], writes=[("qdT", bs)])
                            S.op("dve", lambda e, qTt=qTt: e.tensor_tensor(out=qdT_s[bs][:, :], in0=qdT_s[bs][:, :], in1=qTt, op=ALU.mult), reads=[("qdT", bs), ("cvb", gq)], writes=[("qdT", bs)])
                        if stage == 4 and SUB < 2:
                            return
                        yield
                        pkk, kkk = pq()
                        S.op("act", lambda e, kTt=kTt: e.activation(out=rc(kT2_s[bs][:, :]), in_=kTt, func=AF.Copy), reads=[("cvb", gk)], writes=[("kT2", bs)])
                        S.op("pe", lambda e, pkk=pkk, kTt=kTt: mm_c(e, pkk, kTt, kT2_s[bs][:, :], 1), reads=[("cvb", gk), ("kT2", bs)], writes=[kkk])
                        S.op("dve", lambda e, pkk=pkk, h=h: e.scalar_tensor_tensor(out=rr(Pm_s[bs][0][:, :]), in0=pkk, scalar=beta[:, h:h + 1], in1=dec_s[bs][:, :], op0=ALU.mult, op1=ALU.mult),
                             reads=[kkk, "beta", ("dec", bs)], writes=[("Pm", bs, 0)])
                        S.op("dve", lambda e: e.tensor_tensor(out=rr(Pm_s[bs][0][:, :]), in0=Pm_s[bs][0][:, :], in1=C("strictl"), op=ALU.mult), reads=[("Pm", bs, 0), "cst"], writes=[("Pm", bs, 0)])
                        if stage == 4 and SUB < 2.2:
                            return
                        if full:
                            yield
                            pa_, ka_ = pq()
                            S.op("pe", lambda e, pa_=pa_, kTt=kTt, qTt=qTt: e.matmul(pa_, kTt, qTt, start=True, stop=True), reads=[("cvb", gk), ("cvb", gq)], writes=[ka_])
                            S.op("dve", lambda e, pa_=pa_: e.tensor_tensor(out=atT_s[bs][:, :], in0=pa_, in1=decT_s[bs][:, :], op=ALU.mult), reads=[ka_, ("decT", bs)], writes=[("atT", bs)])
                            S.op("dve", lambda e: e.tensor_tensor(out=atT_s[bs][:, :], in0=atT_s[bs][:, :], in1=C("triu"), op=ALU.mult), reads=[("atT", bs), "cst"], writes=[("atT", bs)])
                        if stage == 4 and SUB < 2.4:
                            return
                        yield
                        p, k_ = pq()
                        S.op("pe", lambda e, p=p: trp(e, p, Pm_s[bs][0][:, :]), reads=[("Pm", bs, 0), "cst"], writes=[k_])
                        S.op("act", lambda e, p=p: e.activation(out=rr(Qm_s[bs][0][:, :]), in_=p, func=AF.Copy), reads=[k_], writes=[("Qm", bs, 0)])
                        S.op("dve", lambda e, p=p: e.scalar_tensor_tensor(out=rr(Rm_s[bs][0][:, :]), in0=p, scalar=-1.0, in1=C("ident"), op0=ALU.mult, op1=ALU.add), reads=[k_, "cst"], writes=[("Rm", bs, 0)])
                        cur = 0
                        if stage == 4 and SUB < 2.6:
                            return
                        for lev in range(1, 6):
                            nxt = 1 - cur
                            if lev < 5:
                                yield
                                p, k_ = pq()
                                S.op("pe", lambda e, p=p, cur=cur: mmr(e, p, Pm_s[bs][cur][:, :], Qm_s[bs][cur][:, :], start=True, stop=True),
                                     reads=[("Pm", bs, cur), ("Qm", bs, cur)], writes=[k_])
                                S.op("act", lambda e, p=p, nxt=nxt: e.activation(out=rr(Qm_s[bs][nxt][:, :]), in_=p, func=AF.Copy), reads=[k_], writes=[("Qm", bs, nxt)])
                            yield
                            p, k_ = pq()
                            S.op("pe", lambda e, p=p, cur=cur: mmr(e, p, Qm_s[bs][cur][:, :], Pm_s[bs][cur][:, :], start=True, stop=True),
                                 reads=[("Pm", bs, cur), ("Qm", bs, cur)], writes=[k_])
                            S.op("dve", lambda e, p=p, nxt=nxt: e.tensor_copy(out=rr(Pm_s[bs][nxt][:, :]), in_=p), reads=[k_], writes=[("Pm", bs, nxt)])
                            yield
                            p, k_ = pq()
                            S.op("pe", lambda e, p=p, cur=cur, nxt=nxt: mmr(e, p, Pm_s[bs][nxt][:, :], Rm_s[bs][cur][:, :], start=True, stop=True),
                                 reads=[("Pm", bs, nxt), ("Rm", bs, cur)], writes=[k_])
                            S.op("dve", lambda e, p=p, cur=cur, nxt=nxt: e.tensor_tensor(out=rr(Rm_s[bs][nxt][:, :]), in0=p, in1=Rm_s[bs][cur][:, :], op=ALU.add),
                                 reads=[k_, ("Rm", bs, cur)], writes=[("Rm", bs, nxt)])
                            cur = nxt
                        if stage == 4 and SUB < 2.8:
                            return
                        TT = Rm_s[bs][cur]
                        TTk = ("Rm", bs, cur)
                        yield
                        p, k_ = pq()
                        S.op("pe", lambda e, p=p, TT=TT: mmr(e, p, TT[:, :], bv_s[bs][:, :], start=True, stop=True), reads=[TTk, ("bv", bs)], writes=[k_])
                        S.op("act", lambda e, p=p: e.activation(out=ut_s[bs][:, :], in_=p, func=AF.Copy), reads=[k_], writes=[("ut", bs)])
                        yield
                        p, k_ = pq()
                        S.op("pe", lambda e, p=p, TT=TT: mmr(e, p, kbg_s[bs][:, :], TT[:, :], start=True, stop=True), reads=[TTk, ("kbg", bs)], writes=[k_])
                        S.op("act", lambda e, p=p: e.activation(out=rc(wT_s[bs][:, :]), in_=p, func=AF.Copy), reads=[k_], writes=[("wT", bs)])
                        if stage == 4 and SUB < 3:
                            return
                        if full:
                            po, ko = pfull()
                            po = po[:, 0:128]
                        for c in range(2):
                            cs = slice(c * 64, (c + 1) * 64)
                            yield
                            p, k_ = pq()
                            S.op("pe", lambda e, p=p, cs=cs, h=h: (mm_c(e, p, wT_s[bs][:, :], Sg[h][:, :], 2) if (R_CHAIN and (CH_MASK & 2)) else e.matmul(p[cs, :], wT_s[bs][:, cs], Sg[h][:, :], start=True, stop=True)), reads=[("wT", bs), ("Sg", h)], writes=[k_])
                            S.op("dve", lambda e, p=p, cs=cs: e.scalar_tensor_tensor(out=rc(vnew_s[bs][cs, :]), in0=p[cs, :], scalar=-1.0, in1=ut_s[bs][cs, :], op0=ALU.mult, op1=ALU.add),
                                 reads=[k_, ("ut", bs)], writes=[(("vnew", bs), c)])
                            if full:
                                S.op("pe", lambda e, po=po, cs=cs, h=h: e.matmul(po[cs, :], qdT_s[bs][:, cs], Sg[h][:, :], start=True, stop=False),
                                     reads=[("qdT", bs), ("Sg", h)], writes=[ko])
                                S.op("pe", lambda e, po=po, cs=cs: e.matmul(po[cs, :], atT_s[bs][cs, cs], vnew_s[bs][cs, :], start=False, stop=True),
                                     reads=[("atT", bs), (("vnew", bs), c)], writes=[ko])
                            yield
                            p2_, k2_ = pq()
                            S.op("pe", lambda e, p2_=p2_, cs=cs: mm_c(e, p2_, ktl_s[bs][cs, :], vnew_s[bs][cs, :], 4), reads=[("ktl", bs), (("vnew", bs), c)], writes=[k2_])
                            S.op("act", lambda e, c=c, h=h: e.activation(out=rc(Sg[h][:, :]), in_=Sg[h][:, :], func=AF.Copy, scale=glb_s[bs][:, c:c + 1]),
                                 reads=[("glb", bs), ("Sg", h)], writes=[("Sg", h)])
                            S.op("dve", lambda e, p2_=p2_, h=h: e.tensor_tensor(out=rc(Sg[h][:, :]), in0=p2_, in1=Sg[h][:, :], op=ALU.add),
                                 reads=[k2_, ("Sg", h)], writes=[("Sg", h)])
                        if full:
                            S.op("act", lambda e, po=po, h=h: e.activation(out=otok[:, h * 128:(h + 1) * 128], in_=po, func=AF.Copy), reads=[ko], writes=[("otok", h)])

                    for pair in (((0, 1), (2, 3)) if stage >= 4 else ()):
                        gens = [gdn_gen(hh, ii) for ii, hh in enumerate(pair)]
                        while gens:
                            for g_ in list(gens):
                                try:
                                    next(g_)
                                except StopIteration:
                                    gens.remove(g_)
                    if stage < 5:
                        continue
                    pbf, pkf = proj_tm(OF, 512)
                    S.op("act", lambda e, pbf=pbf: e.activation(out=fv[:, :], in_=pbf[:, :], func=AF.Sigmoid), reads=[pkf], writes=["fv"])
                    S.op("dve", lambda e: e.tensor_tensor(out=fv[:, :], in0=fv[:, :], in1=omlB[:, :], op=ALU.mult), reads=["fv", "omlB"], writes=["fv"])
                    S.op("dve", lambda e: e.tensor_tensor(out=fv[:, :], in0=fv[:, :], in1=lbB[:, :], op=ALU.add), reads=["fv", "lbB"], writes=["fv"])
                    S.op("act", lambda e: e.activation(out=ru(logf[:, :]), in_=fv[:, :], func=AF.Ln), reads=["fv"], writes=["logf"])
                    S.op("dve", lambda e: e.tensor_scalar(out=kh[:, :], in0=fv[:, :], scalar1=-1.0, scalar2=1.0, op0=ALU.mult, op1=ALU.add), reads=["fv"], writes=["kh"])
                    pbi, pki = proj_tm(OI, 512)
                    S.op("act", lambda e, pbi=pbi: e.activation(out=rc(vh[:, :]), in_=pbi[:, :], func=AF.Copy), reads=[pki], writes=["vh"])
                    pbr, pkr = pfull()
                    S.op("pe", lambda e, pbr=pbr: mm_u(e, pbr[:, :], CR("rev"), logf[:, :]), reads=["cstr", "logf"], writes=[pkr])
                    S.op("act", lambda e, pbr=pbr: e.activation(out=rc(ktail[:, :]), in_=pbr[:, :], func=AF.Exp), reads=[pkr], writes=["ktail"])
                    S.op("dve", lambda e: e.tensor_tensor(out=rc(ktail[:, :]), in0=ktail[:, :], in1=kh[:, :], op=ALU.mult), reads=["ktail", "kh"], writes=["ktail"])
                    if OWN0 <= ti < OWN0 + 2:
                        r0 = (ti - OWN0) * 128
                        dump("logf", logf[:, :], [256, 512], "logf", r0, r0 + 128)
                        dump("kh", kh[:, :], [256, 512], "kh", r0, r0 + 128)
                        dump("vh", vh[:, :], [256, 512], "vh", r0, r0 + 128)
                    for h in range(4):
                        hs = slice(h * 128, (h + 1) * 128)
                        p, k_ = pq()
                        S.op("pe", lambda e, p=p, hs=hs: mm_u(e, p[:, 0:2], logf[:, hs], CR("cind")[:, 0:2]), reads=["logf", "cstr"], writes=[k_])
                        S.op("act", lambda e, p=p, h=h: e.activation(out=ebl[:, h, :], in_=p[:, 0:2], func=AF.Exp), reads=[k_], writes=[("ebl", h)])
                        if full:
                            p, k_ = pq()
                            S.op("pe", lambda e, p=p, hs=hs: mm_u(e, p, logf[:, hs], CR("triu")), reads=["logf", "cstr"], writes=[k_])
                            S.op("act", lambda e, p=p: e.activation(out=ebT[:, :], in_=p, func=AF.Exp), reads=[k_], writes=["ebT"])
                            S.op("act", lambda e, p=p: e.activation(out=enbT[:, :], in_=p, func=AF.Exp, scale=-1.0), reads=[k_], writes=["enbT"])
                            S.op("dve", lambda e, h=h: e.tensor_tensor(out=qtT[:, :], in0=qbT[:, h, tsl], in1=ebT[:, :], op=ALU.mult), reads=[("qbT", h), "ebT"], writes=["qtT"])
                            S.op("dve", lambda e, h=h: e.scalar_tensor_tensor(out=ktT[:, :], in0=fT[:, h, tsl], scalar=omlv[:, h:h + 1], in1=enbT[:, :], op0=ALU.mult, op1=ALU.mult),
                                 reads=[("fT", h), "omlv", "enbT"], writes=["ktT"])
                            p, k_ = pq()
                            S.op("pe", lambda e, p=p: e.matmul(p, ktT[:, :], qtT[:, :], start=True, stop=True), reads=["ktT", "qtT"], writes=[k_])
                            S.op("dve", lambda e, p=p: e.tensor_tensor(out=AT[:, :], in0=p, in1=C("triu"), op=ALU.mult), reads=[k_, "cst"], writes=["AT"])
                            po, ko = pfull()
                            po = po[:, 0:128]
                        for c in range(2):
                            cs = slice(c * 64, (c + 1) * 64)
                            if full:
                                S.op("pe", lambda e, po=po, cs=cs, h=h: e.matmul(po[cs, :], qtT[:, cs], Sh[h][:, :], start=True, stop=False),
                                     reads=["qtT", ("Sh", h)], writes=[ko])
                                S.op("pe", lambda e, po=po, cs=cs, hs=hs: e.matmul(po[cs, :], AT[cs, cs], vh[cs, hs], start=False, stop=True),
                                     reads=["AT", "vh"], writes=[ko])
                            p2_, k2_ = pq()
                            S.op("pe", lambda e, p2_=p2_, cs=cs, hs=hs: mm_c(e, p2_, ktail[cs, hs], vh[cs, hs], 8), reads=["ktail", "vh"], writes=[k2_])
                            S.op("act", lambda e, c=c, h=h: e.activation(out=Sh[h][:, :], in_=Sh[h][:, :], func=AF.Copy, scale=ebl[:, h, c:c + 1]),
                                 reads=[("ebl", h), ("Sh", h)], writes=[("Sh", h)])
                            S.op("dve", lambda e, p2_=p2_, h=h: e.tensor_tensor(out=Sh[h][:, :], in0=p2_, in1=Sh[h][:, :], op=ALU.add),
                                 reads=[k2_, ("Sh", h)], writes=[("Sh", h)])
                        if full:
                            S.op("act", lambda e, po=po, h=h: e.activation(out=otok[:, 512 + h * 128:512 + (h + 1) * 128], in_=po, func=AF.Copy), reads=[ko], writes=[("otok", 4 + h)])

                    if not full or stage < 6:
                        continue
                    oi = ti - OWN0
                    ok_all = [("otok", i) for i in range(8)]
                    dump("otok", otok[:, :], [NOWN * 128, D], ok_all, oi * 128, (oi + 1) * 128)
                    for h in range(4):
                        S.op("act", lambda e, h=h: e.activation(out=sq[:, 0:128], in_=otok[:, h * 128:(h + 1) * 128], func=AF.Square, accum_out=stat[:, 2 + h:3 + h]),
                             reads=[("otok", h)], writes=["sq", ("statA", h)])
                    S.op("dve", lambda e: e.tensor_scalar(out=stat[:, 2:6], in0=stat[:, 2:6], scalar1=1.0 / 128, scalar2=EPS, op0=ALU.mult, op1=ALU.add),
                         reads=[("statA", h) for h in range(4)], writes=["statA"])
                    S.op("act", lambda e: e.activation(out=stat[:, 2:6], in_=stat[:, 2:6], func=AF.Sqrt), reads=["statA"], writes=["statA"])
                    S.op("dve", lambda e: e.reciprocal(out=stat[:, 2:6], in_=stat[:, 2:6]), reads=["statA"], writes=["statA"])
                    for h in range(4):
                        S.op("dve", lambda e, h=h: e.scalar_tensor_tensor(out=otok[:, h * 128:(h + 1) * 128], in0=otok[:, h * 128:(h + 1) * 128], scalar=stat[:, 2 + h:3 + h],
                                                                          in1=gnw[:, :], op0=ALU.mult, op1=ALU.mult),
                             reads=[("otok", h), "statA", "gnw"], writes=[("otok", h)])
                    S.op("act", lambda e: e.activation(out=sq[:, 0:512], in_=otok[:, 512:1024], func=AF.Square, accum_out=stat[:, 6:7]),
                         reads=ok_all[4:], writes=["sq", "statB"])
                    S.op("dve", lambda e: e.tensor_scalar(out=stat[:, 6:7], in0=stat[:, 6:7], scalar1=1.0 / 512, scalar2=EPS, op0=ALU.mult, op1=ALU.add),
                         reads=["statB"], writes=["statB"])
                    S.op("act", lambda e: e.activation(out=stat[:, 6:7], in_=stat[:, 6:7], func=AF.Sqrt), reads=["statB"], writes=["statB"])
                    S.op("dve", lambda e: e.reciprocal(out=stat[:, 6:7], in_=stat[:, 6:7]), reads=["statB"], writes=["statB"])
                    S.op("dve", lambda e: e.scalar_tensor_tensor(out=otok[:, 512:1024], in0=otok[:, 512:1024], scalar=stat[:, 6:7], in1=hnw[:, :], op0=ALU.mult, op1=ALU.mult),
                         reads=ok_all[4:] + ["statB", "hnw"], writes=ok_all[4:])
                    S.op("dve", lambda e: e.tensor_tensor(out=ob[:, :], in0=otok[:, :], in1=gateS[:, :], op=ALU.mult),
                         reads=ok_all + ["gateA", "gateB"], writes=["ob"])
                    for kc in range(8):
                        S.op("pe", lambda e, kc=kc: e.transpose(out=tbank_bf[:, kc * 128:(kc + 1) * 128], in_=ob[:, kc * 128:(kc + 1) * 128], identity=cst_bf[:, :]),
                             reads=["ob", "cst_bf"], writes=[TB])
                    S.op("act", lambda e: e.activation(out=oT[:, :, :], in_=tbank_bf.rearrange("p (k t) -> p k t", k=8), func=AF.Copy), reads=[TB], writes=["oT"])
                    xb = xt[ti % 2]
                    xk = ("xt", ti % 2)
                    dma_load(xb[:, :], xseq[ti * 128:(ti + 1) * 128, :], [xk])
                    for half in range(2):
                        wsb, wk = wload(wout_bf, "wout_bf", half * 512, 512)
                        pb, pk = pfull()
                        for kc in range(8):
                            S.op("pe", lambda e, kc=kc, pb=pb, wsb=wsb: e.matmul(pb[:, :], oT[:, kc, :], wsb[:, kc, :], start=(kc == 0), stop=(kc == 7)),
                                 reads=["oT", wk], writes=[pk])
                        hsl = slice(half * 512, (half + 1) * 512)
                        S.op("dve", lambda e, pb=pb, hsl=hsl: e.tensor_tensor(out=x1[:, hsl], in0=pb[:, :], in1=G1[:, hsl], op=ALU.mult), reads=[pk, "modbc"], writes=[("x1", half)])
                        S.op("dve", lambda e, hsl=hsl, xb=xb: e.tensor_tensor(out=x1[:, hsl], in0=x1[:, hsl], in1=xb[:, hsl], op=ALU.add), reads=[("x1", half), xk], writes=[("x1", half)])
                    x1k = [("x1", 0), ("x1", 1)]
                    dump("x1all", x1[:, :], [NOWN * 128, D], x1k, oi * 128, (oi + 1) * 128)
                    S.op("sp", lambda e, oi=oi: e.dma_start(out=x1scr[oi * 128:(oi + 1) * 128, :], in_=x1[:, :]), reads=x1k, dma="x1st")
                    S.op("act", lambda e: e.activation(out=sq[:, :], in_=x1[:, :], func=AF.Square, accum_out=stat[:, 8:9]), reads=x1k, writes=["sq", "stat8"])
                    S.op("dve", lambda e: e.tensor_scalar(out=stat[:, 8:9], in0=stat[:, 8:9], scalar1=1.0 / D, scalar2=EPS, op0=ALU.mult, op1=ALU.add), reads=["stat8"], writes=["stat8"])
                    S.op("act", lambda e: e.activation(out=stat[:, 8:9], in_=stat[:, 8:9], func=AF.Sqrt), reads=["stat8"], writes=["stat8"])
                    S.op("dve", lambda e: e.reciprocal(out=stat[:, 8:9], in_=stat[:, 8:9]), reads=["stat8"], writes=["stat8"])
                    S.op("dve", lambda e: e.scalar_tensor_tensor(out=h2f[:, :], in0=x1[:, :], scalar=stat[:, 8:9], in1=SC2, op0=ALU.mult, op1=ALU.mult),
                         reads=x1k + ["stat8", "modbc"], writes=["h2f"])
                    S.op("dve", lambda e: e.tensor_tensor(out=h2f[:, :], in0=h2f[:, :], in1=SH2, op=ALU.add), reads=["h2f", "modbc"], writes=["h2f"])
                    S.op("act", lambda e: e.activation(out=h2b[:, :], in_=h2f[:, :], func=AF.Copy), reads=["h2f"], writes=["h2b"])
                    for kc in range(8):
                        S.op("pe", lambda e, kc=kc: e.transpose(out=tbank_bf[:, kc * 128:(kc + 1) * 128], in_=h2b[:, kc * 128:(kc + 1) * 128], identity=cst_bf[:, :]),
                             reads=["h2b", "cst_bf"], writes=[TB])
                    S.op("act", lambda e: e.activation(out=h2Tt[:, :], in_=tbank_bf, func=AF.Copy), reads=[TB], writes=["h2Tt"])
                    S.op("sp", lambda e, oi=oi: e.dma_start(out=h2scr[oi], in_=h2Tt[:, :]), reads=["h2Tt"], dma="h2st")
                    for kc in range(8):
                        p, k_ = pq()
                        S.op("pe", lambda e, p=p, kc=kc: trp(e, p, h2f[:, kc * 128:(kc + 1) * 128]), reads=["h2f", "cst"], writes=[k_])
                        S.op("act", lambda e, p=p, kc=kc: e.activation(out=h2Tf[:, kc, :], in_=p, func=AF.Copy), reads=[k_], writes=[("h2Tf", kc)])
                    p, k_ = pq()
                    for kc in range(8):
                        S.op("pe", lambda e, p=p, kc=kc: e.matmul(p[:, 0:NE], h2Tf[:, kc, :], wr[:, kc, :], start=(kc == 0), stop=(kc == 7)),
                             reads=[("h2Tf", kc), "wr"], writes=[k_])
                    if stage < 7:
                        continue
                    S.op("act", lambda e, p=p: e.activation(out=rsc[:, :], in_=p[:, 0:NE], func=AF.Sigmoid), reads=[k_], writes=["rsc"])
                    S.op("dve", lambda e: e.tensor_tensor(out=rsel[:, :], in0=rsc[:, :], in1=rbb[:, :], op=ALU.add), reads=["rsc", "rbb"], writes=["rsel"])
                    rs3 = rsel[:, :].rearrange("p (g k) -> p g k", g=8)
                    rt3 = rtmp[:, :].rearrange("p (g k) -> p g k", g=8)
                    S.op("dve", lambda e: e.tensor_reduce(out=rg8[:, :, 0], in_=rs3, axis=AX.X, op=ALU.max), reads=["rsel"], writes=["rg8a"])
                    S.op("dve", lambda e: e.tensor_tensor(out=rt3, in0=rs3, in1=rg8[:, :, 0:1].to_broadcast([128, 8, 8]), op=ALU.is_equal),
                         reads=["rsel", "rg8a"], writes=["rtmp"])
                    S.op("dve", lambda e: e.scalar_tensor_tensor(out=rtmp[:, :], in0=rtmp[:, :], scalar=-1.0e4, in1=rsel[:, :], op0=ALU.mult, op1=ALU.add),
                         reads=["rtmp", "rsel"], writes=["rtmp"])
                    S.op("dve", lambda e: e.tensor_reduce(out=rg8[:, :, 1], in_=rt3, axis=AX.X, op=ALU.max), reads=["rtmp"], writes=["rg8b"])
                    S.op("dve", lambda e: e.tensor_tensor(out=rg8[:, :, 2], in0=rg8[:, :, 0], in1=rg8[:, :, 1], op=ALU.add), reads=["rg8a", "rg8b"], writes=["rg8c"])
                    S.op("dve", lambda e: e.tensor_copy(out=stat[:, 8:16], in_=rg8[:, :, 2]), reads=["rg8c", "stat8"], writes=["stat8"])
                    S.op("dve", lambda e: e.max(out=rtmp[:, 0:8], in_=stat[:, 8:16]), reads=["stat8"], writes=["rtmp"])
                    S.op("dve", lambda e: e.tensor_scalar(out=rg8[:, :, 3], in0=rg8[:, :, 2], scalar1=rtmp[:, 3:4], scalar2=None, op0=ALU.is_ge), reads=["rg8c", "rtmp"], writes=["rg8d"])
                    S.op("dve", lambda e: e.tensor_tensor(out=rt3, in0=rs3, in1=rg8[:, :, 3:4].to_broadcast([128, 8, 8]), op=ALU.mult), reads=["rsel", "rg8d"], writes=["rtmp"])
                    S.op("dve", lambda e: e.tensor_scalar(out=rg8[:, :, 2], in0=rg8[:, :, 3], scalar1=1.0e4, scalar2=-1.0e4, op0=ALU.mult, op1=ALU.add), reads=["rg8d"], writes=["rg8c"])
                    S.op("dve", lambda e: e.tensor_tensor(out=rt3, in0=rt3, in1=rg8[:, :, 2:3].to_broadcast([128, 8, 8]), op=ALU.add), reads=["rtmp", "rg8c"], writes=["rtmp"])
                    S.op("dve", lambda e: e.max(out=stat[:, 8:16], in_=rtmp[:, :]), reads=["rtmp"], writes=["stat8"])
                    S.op("dve", lambda e: e.tensor_scalar(out=rsel[:, :], in0=rtmp[:, :], scalar1=stat[:, 15:16], scalar2=None, op0=ALU.is_ge), reads=["rtmp", "stat8"], writes=["rsel"])
                    S.op("dve", lambda e: e.tensor_tensor(out=rsel[:, :], in0=rsel[:, :], in1=rsc[:, :], op=ALU.mult), reads=["rsel", "rsc"], writes=["rsel"])
                    S.op("dve", lambda e: e.tensor_reduce(out=stat[:, 7:8], in_=rsel[:, :], axis=AX.X, op=ALU.add), reads=["rsel"], writes=["stat7"])
                    S.op("dve", lambda e: e.reciprocal(out=stat[:, 7:8], in_=stat[:, 7:8]), reads=["stat7"], writes=["stat7"])
                    S.op("dve", lambda e, oi=oi: e.tensor_scalar(out=gates[:, oi, 0:NE], in0=rsel[:, :], scalar1=stat[:, 7:8], scalar2=2.5, op0=ALU.mult, op1=ALU.mult),
                         reads=["rsel", "stat7"], writes=["gates"])
                    S.op("dve", lambda e, oi=oi: e.memset(gates[:, oi, NE:NE + 1], 1.0), writes=["gates"])
                    dump("gatesall", gates[:, oi, :], [NOWN * 128, NE + 1], "gates", oi * 128, (oi + 1) * 128)
            S.emit()

        pbk = ExitStack()
        with pbk:
            acc = sb(pbk, "acc", [128, NOWN, D])
            h2T = sb(pbk, "h2T", [128, 8, NOWN * 128], BF16)
            for tl in range(NOWN):
                dma_load(h2T[:, :, tl * 128:(tl + 1) * 128], h2scr[tl].rearrange("p (k t) -> p k t", k=8), ["h2T"], rkeys=[])
            for i in range(NOWN):
                S.op("pool", lambda e, i=i: e.memset(acc[:, i, :], 0.0), writes=[("acc", i)])
            NSLOT = 3
            wg = [sb(pbk, "wg%d" % i, [128, 8, 256], BF16) for i in range(NSLOT)]
            wu = [sb(pbk, "wu%d" % i, [128, 8, 256], BF16) for i in range(NSLOT)]
            wd = [sb(pbk, "wd%d" % i, [128, 2, D], BF16) for i in range(NSLOT)]
            actT = [sb(pbk, "actT%d" % i, [128, 2, NOWN * 128], BF16) for i in range(2)]
            sg = [sb(pbk, "sg%d" % i, [128, 512]) for i in range(2)]
            sgi = [0]
            pending = []
            for ex in range(nex):
                sl = ex % NSLOT
                dma_load(wg[sl][:, :, :], wg_bf[ex].rearrange("(kc p) n -> p kc n", p=128), [("wg", sl)], rkeys=["wg_bf"])
                dma_load(wu[sl][:, :, :], wu_bf[ex].rearrange("(kc p) n -> p kc n", p=128), [("wu", sl)], rkeys=["wu_bf"])
                dma_load(wd[sl][:, :, :], wd_bf[ex].rearrange("(fc p) n -> p fc n", p=128), [("wd", sl)], rkeys=["wd_bf"])
                aT = actT[ex % 2]
                aK = ("actT", ex % 2)
                for nb in range(NOWN // 4):
                    nsl = slice(nb * 512, (nb + 1) * 512)
                    for fm in range(2):
                        fsl = slice(fm * 128, (fm + 1) * 128)
                        b0 = (2 * (nb * 2 + fm)) % 4
                        pg_, kg_ = banks[b0], ("bank", b0)
                        pu_, ku_ = banks[b0 + 1], ("bank", b0 + 1)
                        for kc in range(8):
                            S.op("pe", lambda e, kc=kc, pg_=pg_, sl=sl, fsl=fsl, nsl=nsl: e.matmul(pg_[:, :], wg[sl][:, kc, fsl], h2T[:, kc, nsl], start=(kc == 0), stop=(kc == 7)),
                                 reads=[("wg", sl), "h2T"], writes=[kg_])
                        for kc in range(8):
                            S.op("pe", lambda e, kc=kc, pu_=pu_, sl=sl, fsl=fsl, nsl=nsl: e.matmul(pu_[:, :], wu[sl][:, kc, fsl], h2T[:, kc, nsl], start=(kc == 0), stop=(kc == 7)),
                                 reads=[("wu", sl), "h2T"], writes=[ku_])
                        sgb = sg[sgi[0] % 2]
                        sgk = ("sg", sgi[0] % 2)
                        sgi[0] += 1
                        S.op("act", lambda e, pg_=pg_, sgb=sgb: e.activation(out=sgb[:, :], in_=pg_[:, :], func=AF.Silu), reads=[kg_], writes=[sgk])
                        S.op("dve", lambda e, pu_=pu_, sgb=sgb, aT=aT, fm=fm, nsl=nsl: e.tensor_tensor(out=aT[:, fm, nsl], in0=pu_[:, :], in1=sgb[:, :], op=ALU.mult),
                             reads=[ku_, sgk], writes=[aK])
                        for _ in range(4):
                            if pending:
                                pending.pop(0)()
                def mk_step(tl, half, aT=aT, aK=aK, sl=sl, ex=ex):
                    def step():
                        tsl = slice(tl * 128, (tl + 1) * 128)
                        bi = 4 + (tl * 2 + half) % 4
                        po_, ko_ = banks[bi], ("bank", bi)
                        for fm in range(2):
                            S.op("pe", lambda e, po_=po_, aT=aT, fm=fm, tsl=tsl, sl=sl, half=half: e.matmul(po_[:, :], aT[:, fm, tsl], wd[sl][:, fm, half * 512:(half + 1) * 512],
                                                                                                              start=(fm == 0), stop=(fm == 1)),
                                 reads=[aK, ("wd", sl)], writes=[ko_])
                        hsl = slice(half * 512, (half + 1) * 512)
                        S.op("dve", lambda e, po_=po_, tl=tl, ex=ex, hsl=hsl: e.scalar_tensor_tensor(out=acc[:, tl, hsl], in0=po_[:, :], scalar=gates[:, tl, ex:ex + 1], in1=acc[:, tl, hsl],
                                                                                                      op0=ALU.mult, op1=ALU.add),
                             reads=[ko_, "gates", ("acc", tl)], writes=[("acc", tl)])
                    return step
                while pending:
                    pending.pop(0)()
                pending.extend(mk_step(tl, half) for tl in range(NOWN) for half in range(2))
            while pending:
                pending.pop(0)()
            for tl in range(NOWN):
                dump("moe", acc[:, tl, :], [NOWN * 128, D], ("acc", tl), tl * 128, (tl + 1) * 128)
            xr = [sb(pbk, "xr%d" % i, [128, D]) for i in range(2)]
            fsq = sb(pbk, "fsq", [128, D])
            fst = sb(pbk, "fst", [128, 2])
            for tl in range(NOWN):
                xb = xr[tl % 2]
                xk = ("xr", tl % 2)
                S.op("sp", lambda e, xb=xb, tl=tl: e.dma_start(out=xb[:, :], in_=x1scr[tl * 128:(tl + 1) * 128, :]), reads=[], writes=[xk], dma="xr%d" % (tl % 2))
                S.op("dve", lambda e, tl=tl: e.tensor_tensor(out=acc[:, tl, :], in0=acc[:, tl, :], in1=G2, op=ALU.mult), reads=[("acc", tl), "modbc"], writes=[("acc", tl)])
                S.op("dve", lambda e, tl=tl, xb=xb: e.tensor_tensor(out=acc[:, tl, :], in0=acc[:, tl, :], in1=xb[:, :], op=ALU.add), reads=[("acc", tl), xk], writes=[("acc", tl)])
                S.op("act", lambda e, tl=tl: e.activation(out=fsq[:, :], in_=acc[:, tl, :], func=AF.Square, accum_out=fst[:, 0:1]), reads=[("acc", tl)], writes=["fsq", "fst"])
                S.op("dve", lambda e: e.tensor_scalar(out=fst[:, 1:2], in0=fst[:, 0:1], scalar1=1.0 / D, scalar2=EPS, op0=ALU.mult, op1=ALU.add), reads=["fst"], writes=["fst1"])
                S.op("act", lambda e: e.activation(out=fst[:, 1:2], in_=fst[:, 1:2], func=AF.Sqrt), reads=["fst1"], writes=["fst1"])
                S.op("dve", lambda e: e.reciprocal(out=fst[:, 1:2], in_=fst[:, 1:2]), reads=["fst1"], writes=["fst1"])
                S.op("dve", lambda e, tl=tl: e.scalar_tensor_tensor(out=acc[:, tl, :], in0=acc[:, tl, :], scalar=fst[:, 1:2], in1=fwbc[:, :], op0=ALU.mult, op1=ALU.mult),
                     reads=[("acc", tl), "fst1", "fwbc"], writes=[("acc", tl)])
                S.op("sp", lambda e, tl=tl: e.dma_start(out=y[tl * 128:(tl + 1) * 128, :], in_=acc[:, tl, :]), reads=[("acc", tl)], dma="yst")
            fin = [("d", nm, v) for nm, v in S.dma_cnt.items()]
            S.ins["sp"].append(dict(fn=None, waits=set(fin), tok=None, dma=None, idx=S.done["sp"] + len(S.ins["sp"])))
            S.emit()
    if os.environ.get('SCHED_DEBUG'):
        print('cbase', S.cbase, 'dma', S.dma_cnt, 'done', S.done)
    return nc, dbg_outs


_CACHE = {}


def prep_inputs(inp, dne=NE):
    f = lambda a: np.ascontiguousarray(np.asarray(a, dtype=np.float32))
    x = f(inp["x"])
    c = f(inp["c"])
    conv_w = f(inp["conv_w"])[0]
    convw = conv_w.reshape(4, 12, 128).transpose(2, 1, 0).reshape(128, 48)
    hg_lb = f(inp["hg_lb"])
    lbcol = np.concatenate([hg_lb[0].reshape(4, 128).T, hg_lb[1].reshape(4, 128).T], axis=1)
    shared = {
        "w_ada": f(inp["w_ada"])[0], "b_ada": f(inp["b_ada"])[0], "norm1_w": f(inp["norm1_w"])[0],
        "w_in": f(inp["w_in"])[0], "convw": f(convw), "a_log": f(inp["gdn_a_log"])[0], "dt_bias": f(inp["gdn_dt_bias"])[0],
        "gdn_nw": f(inp["gdn_norm_w"])[0], "lbcol": f(lbcol), "hg_lb": hg_lb, "hg_nw": f(inp["hg_norm_w"])[0],
        "w_out": f(inp["w_out"])[0], "norm2_w": f(inp["norm2_w"])[0], "w_router": f(inp["w_router"])[0],
        "router_bias": f(inp["router_bias"])[0], "w_gate": f(inp["w_gate"])[0][:dne], "w_up": f(inp["w_up"])[0][:dne],
        "w_down": f(inp["w_down"])[0][:dne], "ws_gate": f(inp["ws_gate"])[0], "ws_up": f(inp["ws_up"])[0],
        "ws_down": f(inp["ws_down"])[0], "final_w": f(inp["final_norm_w"]), "consts": CONST_ARR,
    }
    maps = []
    for core in range(8):
        b, j = core // 4, core % 4
        nreal = (j + 1) * 2048
        xs = np.zeros((T, D), np.float32)
        xs[T - nreal:] = x[b, :nreal]
        m = np.zeros(T, np.float32)
        m[T - nreal:] = 1.0
        d = dict(shared)
        d["xseq"] = xs
        d["tmask"] = np.ascontiguousarray(m.reshape(NT, 128).T)
        d["cvec"] = np.ascontiguousarray(c[b].reshape(8, 128).T)
        maps.append(d)
    return maps


def kernel(**inputs):
    if "nc" not in _CACHE:
        _CACHE["nc"] = build()[0]
    nc = _CACHE["nc"]
    maps = prep_inputs(inputs)
    res = run_bass_kernel_spmd(nc, maps, core_ids=list(range(8)))
    out = np.zeros((2, T, D), np.float32)
    for core in range(8):
        b, j = core // 4, core % 4
        out[b, j * 2048:(j + 1) * 2048] = res.results[core]["y"]
    return out
```

```python
from contextlib import ExitStack
import types
import numpy as np
import concourse.bass as bass
import concourse.mybir as mybir
from concourse.bass_utils import run_bass_kernel_spmd

F32 = mybir.dt.float32
BF16 = mybir.dt.bfloat16
AF = mybir.ActivationFunctionType
ALU = mybir.AluOpType
AX = mybir.AxisListType

D = 1024
T = 8192
TS = 2
NW = TS * 128
NT = 64
OWN0 = 48
NOWN = 16
DIN = 4104
OQ, OK_, OV, OGA, OBETA, OA, OF, OI, OQB, OGB = 0, 512, 1024, 1536, 2048, 2052, 2056, 2568, 3080, 3592
EPS = 1e-6
NE = 64
SAME_ENGINE_SYNC = True
import os
SUB = float(os.environ.get("SUB", "9"))


def _snap(fn):
    if fn is None or not getattr(fn, "__closure__", None):
        return fn
    cells = []
    for c in fn.__closure__:
        try:
            cells.append(types.CellType(c.cell_contents))
        except ValueError:
            cells.append(c)
    return types.FunctionType(fn.__code__, fn.__globals__, fn.__name__, fn.__defaults__, tuple(cells))


class Sched:
    ENGS = ("pe", "dve", "act", "pool", "sp")
    BLK = {"pe": "tensor", "dve": "vector", "act": "scalar", "pool": "gpsimd", "sp": "sync"}

    def __init__(self, nc, stack):
        self.nc = nc
        self.ins = {e: [] for e in self.ENGS}
        self.done = {e: 0 for e in self.ENGS}
        self.lastw = {}
        self.readers = {}
        self.dma_cnt = {}
        self.esem = {e: stack.enter_context(nc.semaphore("c_" + e)) for e in self.ENGS}
        self.dsem = {}
        self.stack = stack
        self.cbase = {e: 0 for e in self.ENGS}
        self.cval = {e: {} for e in self.ENGS}
        self.seen = {e: {} for e in self.ENGS}
        self.bar_waits = set()
        self.bar_pending = set()

    def op(self, eng, fn, reads=(), writes=(), dma=None):
        ex = [k for k in reads if isinstance(k, tuple) and k[0] == "bank"]
        if ex:
            reads = [k for k in reads if k not in ex]
            writes = list(writes) + ex
        waits = set()
        for k in reads:
            t = self.lastw.get(k)
            if t is not None:
                waits.add(t)
        for k in writes:
            t = self.lastw.get(k)
            if t is not None:
                waits.add(t)
            for r in self.readers.get(k, ()):
                waits.add(r)
        idx = self.done[eng] + len(self.ins[eng])
        if dma is not None:
            if dma not in self.dsem:
                self.dsem[dma] = self.stack.enter_context(self.nc.semaphore("d_" + dma))
            self.dma_cnt[dma] = self.dma_cnt.get(dma, 0) + 16
            tok = ("d", dma, self.dma_cnt[dma])
        else:
            tok = ("e", eng, idx)
        w2 = set()
        for w in waits:
            if w[0] == "e" and w[2] < self.done[w[1]]:
                continue
            if w[0] == "e" and w[1] == eng and (eng == "pe" or not SAME_ENGINE_SYNC):
                continue
            w2.add(w)
        if eng in self.bar_pending:
            self.bar_pending.discard(eng)
            w2 |= self.bar_waits
        self.ins[eng].append(dict(fn=_snap(fn), waits=w2, tok=tok, dma=dma, idx=idx))
        for k in reads:
            self.readers.setdefault(k, []).append(tok)
        for k in writes:
            self.lastw[k] = tok
            self.readers[k] = []
        return tok

    def emit(self):
        nc = self.nc
        flagged = {e: set() for e in self.ENGS}
        for e in self.ENGS:
            for ins in self.ins[e]:
                for w in ins["waits"]:
                    if w[0] == "e" and w[2] >= self.done[w[1]]:
                        flagged[w[1]].add(w[2])
        lastc = {}
        for e in self.ENGS:
            for ins in self.ins[e]:
                if ins["dma"] is None and ins["fn"] is not None:
                    lastc[e] = ins["idx"]
            if e in lastc:
                flagged[e].add(lastc[e])
        for e in self.ENGS:
            c = self.cbase[e]
            for ins in self.ins[e]:
                if ins["idx"] in flagged[e]:
                    c += 1
                    self.cval[e][ins["idx"]] = c
            self.cbase[e] = c
        with nc.Block() as block:
            for e in self.ENGS:
                lst = self.ins[e]

                def body(engine, e=e, lst=lst):
                    seen = self.seen[e]
                    for ins in lst:
                        need = {}
                        for w in ins["waits"]:
                            if w[0] == "e":
                                key = ("e", w[1])
                                val = self.cval[w[1]][w[2]]
                            else:
                                key = ("d", w[1])
                                val = w[2]
                            need[key] = max(need.get(key, 0), val)
                        for key, val in need.items():
                            if seen.get(key, 0) >= val:
                                continue
                            seen[key] = val
                            sem = self.esem[key[1]] if key[0] == "e" else self.dsem[key[1]]
                            engine.wait_ge(sem, val)
                        if ins["fn"] is None:
                            continue
                        r = ins["fn"](engine)
                        if ins["dma"] is not None:
                            r.then_inc(self.dsem[ins["dma"]], 16)
                        elif ins["idx"] in flagged[e]:
                            r.then_inc(self.esem[e], 1)

                getattr(block, self.BLK[e])(body)
        bw = set()
        for e in self.ENGS:
            self.last_idx = getattr(self, "last_idx", {})
            if e in lastc:
                self.last_idx[e] = lastc[e]
            self.done[e] += len(self.ins[e])
            self.ins[e] = []
        for e, i in getattr(self, "last_idx", {}).items():
            bw.add(("e", e, i))
        for nm, v in self.dma_cnt.items():
            bw.add(("d", nm, v))
        self.bar_waits = bw
        self.bar_pending = set(self.ENGS)


F32R = mybir.dt.float32r
USE_F32R = os.environ.get("NO_F32R") is None
USE_TRP = os.environ.get("NO_TRP") is None


def mmr(e, out, lhsT, rhs, start=True, stop=True):
    if USE_F32R:
        return e.matmul(out, lhsT.bitcast(F32R), rhs.bitcast(F32R), start=start, stop=stop)
    return e.matmul(out, lhsT, rhs, start=start, stop=stop)


def rr(ap):
    return ap.bitcast(F32R) if USE_F32R else ap


R_CHAIN = USE_F32R and os.environ.get("NO_F32R_CHAIN") is None
R_CUM = USE_F32R and os.environ.get("NO_F32R_CUM") is None


def rc(ap):
    return ap.bitcast(F32R) if R_CHAIN else ap


def ru(ap):
    return ap.bitcast(F32R) if R_CUM else ap


CH_MASK = int(os.environ.get("CH_MASK", "14"))


def mm_c(e, out, lhsT, rhs, bit=0):
    if R_CHAIN and (CH_MASK & bit):
        return e.matmul(out, lhsT.bitcast(F32R), rhs.bitcast(F32R), start=True, stop=True)
    return e.matmul(out, lhsT, rhs, start=True, stop=True)


def mm_u(e, out, lhsT, rhs):
    if R_CUM:
        return e.matmul(out, lhsT.bitcast(F32R), rhs.bitcast(F32R), start=True, stop=True)
    return e.matmul(out, lhsT, rhs, start=True, stop=True)


def make_consts():
    c = {}
    p = np.arange(128)
    same = (p[:, None] // 64) == (p[None, :] // 64)
    c["ident"] = np.eye(128, dtype=np.float32)
    c["triu"] = (same & (p[:, None] <= p[None, :])).astype(np.float32)
    c["rev"] = (same & (p[:, None] > p[None, :])).astype(np.float32)
    c["blk"] = same.astype(np.float32)
    c["strictl"] = (same & (p[:, None] > p[None, :])).astype(np.float32)
    c["ones"] = np.ones((128, 128), np.float32)
    ci = np.zeros((128, 128), np.float32)
    ci[:64, 0] = 1.0
    ci[64:, 1] = 1.0
    c["cind"] = ci
    names = ["ident", "triu", "rev", "blk", "strictl", "ones", "cind"]
    arr = np.concatenate([c[n] for n in names], axis=1)
    offs = {n: i * 128 for i, n in enumerate(names)}
    return arr, offs


CONST_ARR, COFF = make_consts()
NCONST = CONST_ARR.shape[1]


def build(debug=(), stage=9, nsi=NT // TS, nex=NE + 1, dne=NE):
    nc = bass.Bass("TRN2", target_bir_lowering=False)
    dbg_outs = {}

    def din(name, shape):
        return nc.dram_tensor(name, list(shape), F32, kind="ExternalInput").ap()

    xseq = din("xseq", [T, D])
    tmask = din("tmask", [128, NT])
    cvec = din("cvec", [128, 8])
    w_ada = din("w_ada", [D, 6 * D])
    b_ada = din("b_ada", [6 * D])
    norm1_w = din("norm1_w", [D])
    w_in = din("w_in", [D, DIN])
    convw = din("convw", [128, 48])
    a_log = din("a_log", [4])
    dt_bias = din("dt_bias", [4])
    gdn_nw = din("gdn_nw", [128])
    lbcol = din("lbcol", [128, 8])
    hg_lb = din("hg_lb", [2, 512])
    hg_nw = din("hg_nw", [512])
    w_out = din("w_out", [D, D])
    norm2_w = din("norm2_w", [D])
    w_router = din("w_router", [D, NE])
    router_bias = din("router_bias", [NE])
    w_gate = din("w_gate", [dne, D, 256])
    w_up = din("w_up", [dne, D, 256])
    w_down = din("w_down", [dne, 256, D])
    ws_gate = din("ws_gate", [D, 256])
    ws_up = din("ws_up", [D, 256])
    ws_down = din("ws_down", [256, D])
    final_w = din("final_w", [D])
    consts = din("consts", [128, NCONST])
    y = nc.dram_tensor("y", [NOWN * 128, D], F32, kind="ExternalOutput").ap()
    x1scr = nc.dram_tensor("x1scr", [NOWN * 128, D], F32)
    win_bf = nc.dram_tensor("win_bf", [D, DIN], BF16)
    wout_bf = nc.dram_tensor("wout_bf", [D, D], BF16)
    wg_bf = nc.dram_tensor("wg_bf", [NE + 1, D, 256], BF16)
    wu_bf = nc.dram_tensor("wu_bf", [NE + 1, D, 256], BF16)
    wd_bf = nc.dram_tensor("wd_bf", [NE + 1, 256, D], BF16)
    h2scr = nc.dram_tensor("h2scr", [NOWN, 128, 8 * 128], BF16)

    top = ExitStack()
    with top:
        S = Sched(nc, top)

        def sb(stack, name, shape, dt=F32):
            return stack.enter_context(nc.sbuf_tensor(name, list(shape), dt))

        banks = [top.enter_context(nc.psum_tensor("bank%d" % i, [128, 512], F32)) for i in range(8)]
        full_rr = [0]
        q_rr = [0]

        def pfull():
            i = full_rr[0] % 3
            full_rr[0] += 1
            return banks[i], ("bank", i)

        def pq():
            i = q_rr[0] % 4
            q_rr[0] += 1
            return banks[4 + i][:, 0:128], ("bank", 4 + i)

        tbank_bf = banks[3][:, :].bitcast(BF16)
        TB = ("bank", 3)

        cst = sb(top, "cst", [128, NCONST])
        cst_bf = sb(top, "cst_bf", [128, 128], BF16)
        modbc = sb(top, "modbc", [128, 6 * D])
        fwbc = sb(top, "fwbc", [128, D])
        gates = sb(top, "gates", [128, NOWN, NE + 1])
        tmk = sb(top, "tmk", [128, NT])
        one_col = sb(top, "one_col", [128, 1])

        def C(n):
            return cst[:, COFF[n]:COFF[n] + 128]

        cstr = sb(top, "cstr", [128, NCONST])

        def CR(n):
            return cstr[:, COFF[n]:COFF[n] + 128]

        def trp(e, out, in_):
            if USE_TRP:
                return e.transpose(out=out, in_=in_, identity=C("ident"))
            return e.matmul(out, in_, C("ident"), start=True, stop=True)

        dma_i = [0]

        def dma_load(out_ap, in_ap, wkeys, eng="sp", rkeys=()):
            k0 = wkeys[0]
            nm = "ld_" + (k0 if isinstance(k0, str) else "_".join(str(z) for z in k0))
            S.op(eng, lambda e: e.dma_start(out=out_ap, in_=in_ap), reads=rkeys, writes=wkeys, dma=nm)

        def dump(name, ap, shape, key, r0=None, r1=None):
            if name not in debug:
                return
            if name not in dbg_outs:
                dbg_outs[name] = nc.dram_tensor("dbg_" + name, list(shape), F32, kind="ExternalOutput").ap()
            t = dbg_outs[name]
            tt = t if r0 is None else t[r0:r1]
            S.op("sp", lambda e: e.dma_start(out=tt, in_=ap), reads=[key] if not isinstance(key, list) else key, dma="dbg_" + name)

        fin_tokens = []

        dma_load(cst[:, :], consts, ["cst"])
        dma_load(tmk[:, :], tmask, ["tmk"])
        S.op("dve", lambda e: e.tensor_copy(out=cst_bf[:, :], in_=C("ident")), reads=["cst"], writes=["cst_bf"])
        S.op("dve", lambda e: e.memset(one_col[:, :], 1.0), writes=["one_col"])
        S.op("dve", lambda e: e.tensor_copy(out=ru(cstr[:, :]), in_=cst[:, :]), reads=["cst"], writes=["cstr"])

        pa = ExitStack()
        with pa:
            wr = sb(pa, "wr", [128, 8, NE])
            wsl = [sb(pa, "wsl%d" % i, [128, 8, 512], BF16) for i in range(3)]
            wsl_rr = [0]

            def flat2(ap_, total):
                return ap_.rearrange("(a b) -> a b", b=2048) if False else ap_

            def cast_flat(dst, src, total, key):
                rows = total // 2048
                r0 = 0
                while r0 < rows:
                    n = min(1024, rows - r0)
                    dma_load(dst[r0:r0 + n, :], src[r0:r0 + n, :], [key], eng="pool")
                    r0 += n

            cast_flat(win_bf[:, :].rearrange("a b -> (a b)").rearrange("(r c) -> r c", c=2048),
                      w_in.rearrange("a b -> (a b)").rearrange("(r c) -> r c", c=2048), D * DIN, "win_bf")
            cast_flat(wout_bf[:, :].rearrange("a b -> (a b)").rearrange("(r c) -> r c", c=2048),
                      w_out.rearrange("a b -> (a b)").rearrange("(r c) -> r c", c=2048), D * D, "wout_bf")

            def flatv(ap_, pat):
                return ap_.rearrange(pat).rearrange("(r c) -> r c", c=2048)

            def cast_experts():
                cast_flat(flatv(wg_bf[0:dne], "e a b -> (e a b)"), flatv(w_gate, "e a b -> (e a b)"), dne * D * 256, "wg_bf")
                cast_flat(flatv(wu_bf[0:dne], "e a b -> (e a b)"), flatv(w_up, "e a b -> (e a b)"), dne * D * 256, "wu_bf")
                cast_flat(flatv(wd_bf[0:dne], "e a b -> (e a b)"), flatv(w_down, "e a b -> (e a b)"), dne * D * 256, "wd_bf")
                cast_flat(flatv(wg_bf[NE], "a b -> (a b)"), flatv(ws_gate, "a b -> (a b)"), D * 256, "wg_bf")
                cast_flat(flatv(wu_bf[NE], "a b -> (a b)"), flatv(ws_up, "a b -> (a b)"), D * 256, "wu_bf")
                cast_flat(flatv(wd_bf[NE], "a b -> (a b)"), flatv(ws_down, "a b -> (a b)"), D * 256, "wd_bf")

            def wload(src_bf, key, col0, n):
                i = wsl_rr[0] % 3
                wsl_rr[0] += 1
                dma_load(wsl[i][:, :, 0:n], src_bf[:, col0:col0 + n].rearrange("(kc p) n -> p kc n", p=128), [("wsl", i)], rkeys=[key])
                return wsl[i], ("wsl", i)

            dma_load(wr[:, :, :], w_router.rearrange("(kc p) n -> p kc n", p=128), ["wr"])

            cw = sb(pa, "cw", [128, 48])
            gnw = sb(pa, "gnw", [128, 128])
            hnw = sb(pa, "hnw", [128, 512])
            rbb = sb(pa, "rbb", [128, NE])
            adt = sb(pa, "adt", [128, 8])
            negA = sb(pa, "negA", [128, 4])
            lbv = sb(pa, "lbv", [128, 4])
            omlv = sb(pa, "omlv", [128, 4])
            lbB = sb(pa, "lbB", [128, 512])
            omlB = sb(pa, "omlB", [128, 512])
            p0 = ExitStack()
            p0.__enter__()
            lbc = sb(p0, "lbc", [128, 8])
            lbb = sb(p0, "lbb", [128, 2, 512])
            n1bc = sb(p0, "n1bc", [128, D])
            n2bc = sb(p0, "n2bc", [128, D])
            cv = sb(p0, "cv", [128, 8])
            cB = sb(p0, "cB", [128, 8, 128])
            wada = [sb(p0, "wada%d" % i, [128, 8, 512]) for i in range(2)]
            dma_load(cw[:, :], convw, ["cw"])
            dma_load(lbc[:, :], lbcol, ["lbc"])
            dma_load(lbb[:, 0, :], hg_lb[0, :].partition_broadcast(128), ["lbb"])
            dma_load(lbb[:, 1, :], hg_lb[1, :].partition_broadcast(128), ["lbb"])
            dma_load(n1bc[:, :], norm1_w.partition_broadcast(128), ["n1bc"])
            dma_load(n2bc[:, :], norm2_w.partition_broadcast(128), ["n2bc"])
            dma_load(fwbc[:, :], final_w.partition_broadcast(128), ["fwbc"])
            dma_load(gnw[:, :], gdn_nw.partition_broadcast(128), ["gnw"])
            dma_load(hnw[:, :], hg_nw.partition_broadcast(128), ["hnw"])
            dma_load(rbb[:, :], router_bias.partition_broadcast(128), ["rbb"])
            dma_load(adt[:, 0:4], a_log.partition_broadcast(128), ["adt"])
            dma_load(adt[:, 4:8], dt_bias.partition_broadcast(128), ["adt"])
            S.op("act", lambda e: e.activation(out=negA[:, :], in_=adt[:, 0:4], func=AF.Exp), reads=["adt"], writes=["negA"])
            S.op("dve", lambda e: e.tensor_scalar(out=negA[:, :], in0=negA[:, :], scalar1=-1.0, scalar2=None, op0=ALU.mult),
                 reads=["negA"], writes=["negA"])
            S.op("dve", lambda e: e.tensor_tensor(out=lbv[:, :], in0=lbc[:, 0:4], in1=lbc[:, 4:8], op=ALU.subtract),
                 reads=["lbc"], writes=["lbv"])
            S.op("act", lambda e: e.activation(out=lbv[:, :], in_=lbv[:, :], func=AF.Sigmoid), reads=["lbv"], writes=["lbv"])
            S.op("dve", lambda e: e.tensor_scalar(out=omlv[:, :], in0=lbv[:, :], scalar1=-1.0, scalar2=1.0, op0=ALU.mult, op1=ALU.add),
                 reads=["lbv"], writes=["omlv"])
            S.op("dve", lambda e: e.tensor_tensor(out=lbB[:, :], in0=lbb[:, 0, :], in1=lbb[:, 1, :], op=ALU.subtract),
                 reads=["lbb"], writes=["lbB"])
            S.op("act", lambda e: e.activation(out=lbB[:, :], in_=lbB[:, :], func=AF.Sigmoid), reads=["lbB"], writes=["lbB"])
            S.op("dve", lambda e: e.tensor_scalar(out=omlB[:, :], in0=lbB[:, :], scalar1=-1.0, scalar2=1.0, op0=ALU.mult, op1=ALU.add),
                 reads=["lbB"], writes=["omlB"])

            dma_load(cv[:, :], cvec, ["cv"])
            S.op("act", lambda e: e.activation(out=cv[:, :], in_=cv[:, :], func=AF.Silu), reads=["cv"], writes=["cv"])
            for kc in range(8):
                S.op("dve", lambda e, kc=kc: e.tensor_scalar(out=cB[:, kc, :], in0=C("ones"), scalar1=cv[:, kc:kc + 1], scalar2=None, op0=ALU.mult),
                     reads=["cst", "cv"], writes=[("cB", kc)])
            dma_load(modbc[:, :], b_ada.partition_broadcast(128), ["modbc"])
            for g in range(12):
                wa = wada[g % 2]
                wk = ("wada", g % 2)
                dma_load(wa[:, :, :], w_ada[:, g * 512:(g + 1) * 512].rearrange("(kc p) n -> p kc n", p=128), [wk])
                pb, pk = pfull()
                for kc in range(8):
                    S.op("pe", lambda e, kc=kc, wa=wa, pb=pb: e.matmul(pb[:, :], cB[:, kc, :], wa[:, kc, :], start=(kc == 0), stop=(kc == 7)),
                         reads=[("cB", kc), wk], writes=[pk])
                S.op("dve", lambda e, g=g, pb=pb: e.tensor_tensor(out=modbc[:, g * 512:(g + 1) * 512], in0=pb[:, :], in1=modbc[:, g * 512:(g + 1) * 512], op=ALU.add),
                     reads=[pk, "modbc"], writes=["modbc"])
            SH1, SC1, G1, SH2, SC2, G2 = [modbc[:, i * D:(i + 1) * D] for i in range(6)]
            S.op("dve", lambda e: e.scalar_tensor_tensor(out=SC1, in0=SC1, scalar=1.0, in1=n1bc[:, :], op0=ALU.add, op1=ALU.mult),
                 reads=["modbc", "n1bc"], writes=["modbc"])
            S.op("dve", lambda e: e.scalar_tensor_tensor(out=SC2, in0=SC2, scalar=1.0, in1=n2bc[:, :], op0=ALU.add, op1=ALU.mult),
                 reads=["modbc", "n2bc"], writes=["modbc"])

            S.emit()
            p0.close()
            cast_experts()

            xt = [sb(pa, "xt%d" % i, [128, D]) for i in range(2)]
            sq = sb(pa, "sq", [128, D])
            hb = sb(pa, "hb", [128, D], BF16)
            hT = sb(pa, "hT", [128, 8, NW], BF16)
            pj = sb(pa, "pj", [128, 12, NW + 3])
            fT = sb(pa, "fT", [128, 4, NW])
            qbT = sb(pa, "qbT", [128, 4, NW])
            cvb = sb(pa, "cvb", [128, 12, NW])
            for g in range(12):
                S.op("pool", lambda e, g=g: e.memset(pj[:, g, 0:3], 0.0), writes=[("pj", g)])
            tmpf = sb(pa, "tmpf", [128, NW])
            tmpg = sb(pa, "tmpg", [128, NW])
            tsq = sb(pa, "tsq", [128, NW])
            Sg = [sb(pa, "Sg%d" % h, [128, 128]) for h in range(4)]
            Sh = [sb(pa, "Sh%d" % h, [128, 128]) for h in range(4)]
            for h in range(4):
                S.op("dve", lambda e, h=h: e.tensor_scalar(out=rc(Sg[h][:, :]), in0=C("ones"), scalar1=0.0, scalar2=None, op0=ALU.mult), reads=["cst"], writes=[("Sg", h)])
                S.op("pool", lambda e, h=h: e.memset(Sh[h][:, :], 0.0), writes=[("Sh", h)])
            ba = sb(pa, "ba", [128, 8])
            beta = sb(pa, "beta", [128, 4])
            gg = sb(pa, "gg", [128, 4])
            gc = sb(pa, "gc", [128, 4])
            gl = sb(pa, "gl", [128, 4])
            egc = sb(pa, "egc", [128, 4])
            ekt = sb(pa, "ekt", [128, 4])
            bek = sb(pa, "bek", [128, 4])
            Gb = sb(pa, "Gb", [128, 128])
            gcb = sb(pa, "gcb", [128, 128])
            glb = sb(pa, "glb", [128, 2])
            dec = sb(pa, "dec", [128, 128])
            decT = sb(pa, "decT", [128, 128])
            ktok = sb(pa, "ktok", [128, 128])
            kT2 = sb(pa, "kT2", [128, 128])
            vtok = sb(pa, "vtok", [128, 128])
            kbg = sb(pa, "kbg", [128, 128])
            bv = sb(pa, "bv", [128, 128])
            ktl = sb(pa, "ktl", [128, 128])
            Pm = [sb(pa, "Pm%d" % i, [128, 128]) for i in range(2)]
            Qm = [sb(pa, "Qm%d" % i, [128, 128]) for i in range(2)]
            Rm = [sb(pa, "Rm%d" % i, [128, 128]) for i in range(2)]
            ut = sb(pa, "ut", [128, 128])
            wT = sb(pa, "wT", [128, 128])
            vnew = sb(pa, "vnew", [128, 128])
            qdT = sb(pa, "qdT", [128, 128])
            atT = sb(pa, "atT", [128, 128])
            otok = sb(pa, "otok", [128, D])
            ob = sb(pa, "ob", [128, D], BF16)
            oT = sb(pa, "oT", [128, 8, 128], BF16)
            gateS = sb(pa, "gateS", [128, D])
            fv = sb(pa, "fv", [128, 512])
            logf = sb(pa, "logf", [128, 512])
            kh = sb(pa, "kh", [128, 512])
            ktail = sb(pa, "ktail", [128, 512])
            vh = sb(pa, "vh", [128, 512])
            ebl = sb(pa, "ebl", [128, 4, 2])
            ebT = sb(pa, "ebT", [128, 128])
            enbT = sb(pa, "enbT", [128, 128])
            qtT = sb(pa, "qtT", [128, 128])
            ktT = sb(pa, "ktT", [128, 128])
            AT = sb(pa, "AT", [128, 128])
            stat = sb(pa, "stat", [128, 16])
            x1 = sb(pa, "x1", [128, D])
            h2f = sb(pa, "h2f", [128, D])
            h2b = sb(pa, "h2b", [128, D], BF16)
            h2Tf = sb(pa, "h2Tf", [128, 8, 128])
            h2Tt = sb(pa, "h2Tt", [128, 8 * 128], BF16)
            rsc = sb(pa, "rsc", [128, NE])
            rsel = sb(pa, "rsel", [128, NE])
            rtmp = sb(pa, "rtmp", [128, NE])
            rg8 = sb(pa, "rg8", [128, 8, 4])

            def _two(name, first):
                return [first, sb(pa, name + "_b", [128, 128])]
            ktok_s = _two("ktok", ktok); bv_s = _two("bv", bv); kbg_s = _two("kbg", kbg); ktl_s = _two("ktl", ktl)
            Gb_s = _two("Gb", Gb); dec_s = _two("dec", dec); decT_s = _two("decT", decT); kT2_s = _two("kT2", kT2)
            ut_s = _two("ut", ut); wT_s = _two("wT", wT); vnew_s = _two("vnew", vnew); qdT_s = _two("qdT", qdT); atT_s = _two("atT", atT)
            glb_s = [glb, sb(pa, "glb_b", [128, 2])]
            Pm_s = [Pm, [sb(pa, "Pm_b%d" % i, [128, 128]) for i in range(2)]]
            Qm_s = [Qm, [sb(pa, "Qm_b%d" % i, [128, 128]) for i in range(2)]]
            Rm_s = [Rm, [sb(pa, "Rm_b%d" % i, [128, 128]) for i in range(2)]]
            ebT_s = _two("ebT", ebT); enbT_s = _two("enbT", enbT); qtT_s = _two("qtT", qtT); ktT_s = _two("ktT", ktT); AT_s = _two("AT", AT)
            mulmask = C("blk")
            if os.environ.get("SBUF_DEBUG"):
                print("SBUF remaining", nc.sbuf_bytes_remaining)

            for si in range(NT // TS - nsi, NT // TS):
                if stage < 1:
                    break
                full_s = (si * TS >= OWN0)
                for j in range(TS):
                    ti = si * TS + j
                    xb = xt[ti % 2]
                    xk = ("xt", ti % 2)
                    dma_load(xb[:, :], xseq[ti * 128:(ti + 1) * 128, :], [xk])
                    S.op("act", lambda e, xb=xb: e.activation(out=sq[:, :], in_=xb[:, :], func=AF.Square, accum_out=stat[:, 0:1]),
                         reads=[xk], writes=["sq", "stat0"])
                    S.op("dve", lambda e: e.tensor_scalar(out=stat[:, 1:2], in0=stat[:, 0:1], scalar1=1.0 / D, scalar2=EPS, op0=ALU.mult, op1=ALU.add),
                         reads=["stat0"], writes=["stat1"])
                    S.op("act", lambda e: e.activation(out=stat[:, 1:2], in_=stat[:, 1:2], func=AF.Sqrt), reads=["stat1"], writes=["stat1"])
                    S.op("dve", lambda e: e.reciprocal(out=stat[:, 1:2], in_=stat[:, 1:2]), reads=["stat1"], writes=["stat1"])
                    S.op("dve", lambda e, xb=xb: e.scalar_tensor_tensor(out=sq[:, :], in0=xb[:, :], scalar=stat[:, 1:2], in1=SC1, op0=ALU.mult, op1=ALU.mult),
                         reads=[xk, "stat1", "modbc"], writes=["sq"])
                    S.op("dve", lambda e: e.tensor_tensor(out=sq[:, :], in0=sq[:, :], in1=SH1, op=ALU.add), reads=["sq", "modbc"], writes=["sq"])
                    S.op("dve", lambda e, ti=ti: e.tensor_scalar(out=hb[:, :], in0=sq[:, :], scalar1=tmk[:, ti:ti + 1], scalar2=None, op0=ALU.mult),
                         reads=["sq", "tmk"], writes=["hb"])
                    if OWN0 <= ti < OWN0 + 2:
                        dump("hchk", sq[:, :], [256, D], "sq", (ti - OWN0) * 128, (ti - OWN0 + 1) * 128)
                    for kc in range(8):
                        S.op("pe", lambda e, kc=kc: e.transpose(out=tbank_bf[:, kc * 128:(kc + 1) * 128], in_=hb[:, kc * 128:(kc + 1) * 128], identity=cst_bf[:, :]),
                             reads=["hb", "cst_bf"], writes=[TB])
                    S.op("act", lambda e, j=j: e.activation(out=hT[:, :, j * 128:(j + 1) * 128],
                                                            in_=tbank_bf.rearrange("p (k t) -> p k t", k=8), func=AF.Copy),
                         reads=[TB], writes=[("hT", j)])
                hTk = [("hT", j) for j in range(TS)]

                if stage < 2:
                    continue
                def proj_fm(wsb, wk, c0, evac):
                    pb, pk = pfull()
                    for kc in range(8):
                        S.op("pe", lambda e, kc=kc, pb=pb: e.matmul(pb[:, 0:NW], wsb[:, kc, c0:c0 + 128], hT[:, kc, :], start=(kc == 0), stop=(kc == 7)),
                             reads=[wk] + hTk, writes=[pk])
                    evac(pb, pk)

                groups = list(range(4, 12)) if (si + 1) * TS < OWN0 else list(range(12))
                for g in groups:
                    if g % 4 == 0:
                        wsb, wk = wload(win_bf, "win_bf", g * 128, 512)
                    def ev(pb, pk, g=g):
                        S.op("act", lambda e: e.activation(out=pj[:, g, 3:NW + 3], in_=pb[:, 0:NW], func=AF.Copy),
                             reads=[pk], writes=[("pj", g)])
                    proj_fm(wsb, wk, (g % 4) * 128, ev)
                    S.op("dve", lambda e, g=g: e.tensor_scalar(out=tmpf[:, :], in0=pj[:, g, 3:NW + 3], scalar1=cw[:, g * 4 + 3:g * 4 + 4], scalar2=None, op0=ALU.mult),
                         reads=[("pj", g), "cw"], writes=["tmpf"])
                    for tap in (2, 1, 0):
                        S.op("dve", lambda e, g=g, tap=tap: e.scalar_tensor_tensor(out=tmpf[:, :], in0=pj[:, g, tap:tap + NW], scalar=cw[:, g * 4 + tap:g * 4 + tap + 1],
                                                                                   in1=tmpf[:, :], op0=ALU.mult, op1=ALU.add),
                             reads=[("pj", g), "cw", "tmpf"], writes=["tmpf"])
                    S.op("pool", lambda e, g=g: e.tensor_copy(out=pj[:, g, 0:3], in_=pj[:, g, NW:NW + 3]), reads=[("pj", g)], writes=[("pj", g)])
                    S.op("act", lambda e, g=g: e.activation(out=cvb[:, g, :], in_=tmpf[:, :], func=AF.Silu), reads=["tmpf"], writes=[("cvb", g)])
                    if g < 8:
                        S.op("act", lambda e, g=g: e.activation(out=ru(tsq[:, :]), in_=cvb[:, g, :], func=AF.Square), reads=[("cvb", g)], writes=["tsq"])
                        pb, pk = pfull()
                        S.op("pe", lambda e, pb=pb: mm_u(e, pb[:, 0:NW], CR("ones"), tsq[:, :]), reads=["cstr", "tsq"], writes=[pk])
                        sc = EPS if g >= 4 else EPS
                        S.op("dve", lambda e, pb=pb: e.tensor_scalar(out=tmpg[:, :], in0=pb[:, 0:NW], scalar1=EPS, scalar2=None, op0=ALU.add), reads=[pk], writes=["tmpg"])
                        S.op("act", lambda e: e.activation(out=tmpg[:, :], in_=tmpg[:, :], func=AF.Sqrt), reads=["tmpg"], writes=["tmpg"])
                        S.op("dve", lambda e: e.reciprocal(out=tmpg[:, :], in_=tmpg[:, :]), reads=["tmpg"], writes=["tmpg"])
                        if g < 4:
                            S.op("dve", lambda e, g=g: e.scalar_tensor_tensor(out=cvb[:, g, :], in0=cvb[:, g, :], scalar=128.0 ** -0.5, in1=tmpg[:, :], op0=ALU.mult, op1=ALU.mult),
                                 reads=[("cvb", g), "tmpg"], writes=[("cvb", g)])
                        else:
                            S.op("dve", lambda e, g=g: e.tensor_tensor(out=rc(cvb[:, g, :]), in0=cvb[:, g, :], in1=tmpg[:, :], op=ALU.mult),
                                 reads=[("cvb", g), "tmpg"], writes=[("cvb", g)])
                if full_s:
                    wsb, wk = wload(win_bf, "win_bf", OF, 512)
                    for h in range(4):
                        def evf(pb, pk, h=h):
                            S.op("act", lambda e: e.activation(out=fT[:, h, :], in_=pb[:, 0:NW], func=AF.Sigmoid, scale=-1.0), reads=[pk], writes=[("fT", h)])
                        proj_fm(wsb, wk, h * 128, evf)
                    wsb, wk = wload(win_bf, "win_bf", OQB, 512)
                    for h in range(4):
                        def evq(pb, pk, h=h):
                            S.op("act", lambda e: e.activation(out=qbT[:, h, :], in_=pb[:, 0:NW], func=AF.Silu), reads=[pk], writes=[("qbT", h)])
                        proj_fm(wsb, wk, h * 128, evq)

                if si * TS == OWN0:
                    dump("cvb", cvb[:, :, :].rearrange("p g t -> p (g t)"), [128, 12 * NW], [("cvb", g) for g in range(12)])
                if stage < 3:
                    continue
                for j in range(TS):
                    ti = si * TS + j
                    full = ti >= OWN0
                    tsl = slice(j * 128, (j + 1) * 128)

                    def proj_tm(col0, n):
                        wsb, wk = wload(win_bf, "win_bf", col0, n)
                        pb, pk = pfull()
                        for kc in range(8):
                            S.op("pe", lambda e, kc=kc, pb=pb: e.matmul(pb[:, 0:n], hT[:, kc, tsl], wsb[:, kc, 0:n], start=(kc == 0), stop=(kc == 7)),
                                 reads=[wk, ("hT", j)], writes=[pk])
                        return pb, pk

                    pb, pk = proj_tm(OBETA, 8)
                    S.op("act", lambda e, pb=pb: e.activation(out=beta[:, :], in_=pb[:, 0:4], func=AF.Sigmoid), reads=[pk], writes=["beta"])
                    S.op("dve", lambda e, pb=pb: e.tensor_tensor(out=ba[:, 4:8], in0=pb[:, 4:8], in1=adt[:, 4:8], op=ALU.add), reads=[pk, "adt"], writes=["ba"])
                    S.op("act", lambda e: e.activation(out=ba[:, 4:8], in_=ba[:, 4:8], func=AF.Exp), reads=["ba"], writes=["ba"])
                    S.op("act", lambda e: e.activation(out=ba[:, 4:8], in_=ba[:, 4:8], func=AF.Ln, bias=one_col[:, 0:1]), reads=["ba", "one_col"], writes=["ba"])
                    S.op("dve", lambda e: e.tensor_tensor(out=ru(gg[:, :]), in0=ba[:, 4:8], in1=negA[:, :], op=ALU.mult), reads=["ba", "negA"], writes=["gg"])
                    if stage == 3 and SUB < 1:
                        continue
                    p1, k1 = pq()
                    S.op("pe", lambda e, p1=p1: mm_u(e, p1[:, 0:4], CR("triu"), gg[:, :]), reads=["cstr", "gg"], writes=[k1])
                    S.op("dve", lambda e, p1=p1: e.tensor_copy(out=gc[:, :], in_=p1[:, 0:4]), reads=[k1], writes=["gc"])
                    p2, k2 = pq()
                    S.op("pe", lambda e, p2=p2: mm_u(e, p2[:, 0:4], CR("blk"), gg[:, :]), reads=["cstr", "gg"], writes=[k2])
                    S.op("dve", lambda e, p2=p2: e.tensor_tensor(out=gl[:, :], in0=p2[:, 0:4], in1=gc[:, :], op=ALU.subtract), reads=[k2, "gc"], writes=["gl"])
                    S.op("act", lambda e: e.activation(out=ekt[:, :], in_=gl[:, :], func=AF.Exp), reads=["gl"], writes=["ekt"])
                    S.op("act", lambda e: e.activation(out=egc[:, :], in_=gc[:, :], func=AF.Exp), reads=["gc"], writes=["egc"])
                    S.op("dve", lambda e: e.tensor_tensor(out=bek[:, :], in0=egc[:, :], in1=beta[:, :], op=ALU.mult), reads=["egc", "beta"], writes=["bek"])

                    if OWN0 <= ti < OWN0 + 2:
                        r0 = (ti - OWN0) * 128
                        dump("beta", beta[:, :], [256, 4], "beta", r0, r0 + 128)
                        dump("gg", gg[:, :], [256, 4], "gg", r0, r0 + 128)
                        dump("gc", gc[:, :], [256, 4], "gc", r0, r0 + 128)
                    if stage == 3 and SUB < 2:
                        continue
                    if full:
                        pbA, pkA = proj_tm(OGA, 512)
                        S.op("act", lambda e, pbA=pbA: e.activation(out=gateS[:, 0:512], in_=pbA[:, :], func=AF.Silu), reads=[pkA], writes=["gateA"])
                        pbB, pkB = proj_tm(OGB, 512)
                        S.op("act", lambda e, pbB=pbB: e.activation(out=gateS[:, 512:1024], in_=pbB[:, :], func=AF.Silu), reads=[pkB], writes=["gateB"])

                    def gdn_gen(h, bs):
                        gq, gk, gv = h, 4 + h, 8 + h
                        kTt = cvb[:, gk, tsl]
                        qTt = cvb[:, gq, tsl]
                        yield
                        p, k_ = pq()
                        S.op("pe", lambda e, p=p, kTt=kTt: trp(e, p, kTt), reads=[("cvb", gk), "cst"], writes=[k_])
                        S.op("act", lambda e, p=p: e.activation(out=ktok_s[bs][:, :], in_=p, func=AF.Copy), reads=[k_], writes=[("ktok", bs)])
                        yield
                        p, k_ = pq()
                        S.op("pe", lambda e, p=p, gv=gv: trp(e, p, cvb[:, gv, tsl]), reads=[("cvb", gv), "cst"], writes=[k_])
                        S.op("dve", lambda e, p=p, h=h: e.tensor_scalar(out=rr(bv_s[bs][:, :]), in0=p, scalar1=beta[:, h:h + 1], scalar2=None, op0=ALU.mult),
                             reads=[k_, "beta"], writes=[("bv", bs)])
                        S.op("dve", lambda e, h=h: e.tensor_scalar(out=rr(kbg_s[bs][:, :]), in0=ktok_s[bs][:, :], scalar1=bek[:, h:h + 1], scalar2=None, op0=ALU.mult),
                             reads=[("ktok", bs), "bek"], writes=[("kbg", bs)])
                        S.op("dve", lambda e, h=h: e.tensor_scalar(out=rc(ktl_s[bs][:, :]), in0=ktok_s[bs][:, :], scalar1=ekt[:, h:h + 1], scalar2=None, op0=ALU.mult),
                             reads=[("ktok", bs), "ekt"], writes=[("ktl", bs)])
                        if stage == 4 and SUB < 1:
                            return
                        S.op("dve", lambda e, h=h: e.tensor_scalar(out=ru(Gb_s[bs][:, :]), in0=C("ones"), scalar1=gg[:, h:h + 1], scalar2=None, op0=ALU.mult),
                             reads=["cst", "gg"], writes=[("Gb", bs)])
                        yield
                        pg, kg = pq()
                        S.op("pe", lambda e, pg=pg: mm_u(e, pg, Gb_s[bs][:, :], CR("triu")), reads=[("Gb", bs), "cstr"], writes=[kg])
                        yield
                        pl, kl = pq()
                        S.op("pe", lambda e, pl=pl: mm_u(e, pl[:, 0:2], Gb_s[bs][:, :], CR("cind")[:, 0:2]), reads=[("Gb", bs), "cstr"], writes=[kl])
                        S.op("act", lambda e, pl=pl: e.activation(out=glb_s[bs][:, :], in_=pl[:, 0:2], func=AF.Exp), reads=[kl], writes=[("glb", bs)])
                        S.op("dve", lambda e, pg=pg, h=h: e.tensor_scalar(out=dec_s[bs][:, :], in0=pg, scalar1=gc[:, h:h + 1], scalar2=0.0, op0=ALU.subtract, op1=ALU.max),
                             reads=[kg, "gc"], writes=[("dec", bs)])
                        S.op("act", lambda e: e.activation(out=dec_s[bs][:, :], in_=dec_s[bs][:, :], func=AF.Exp, scale=-1.0), reads=[("dec", bs)], writes=[("dec", bs)])
                        if full:
                            S.op("dve", lambda e, pg=pg, h=h: e.tensor_scalar(out=decT_s[bs][:, :], in0=pg, scalar1=gc[:, h:h + 1], scalar2=0.0, op0=ALU.subtract, op1=ALU.min),
                                 reads=[kg, "gc"], writes=[("decT", bs)])
                            S.op("act", lambda e: e.activation(out=decT_s[bs][:, :], in_=decT_s[bs][:, :], func=AF.Exp), reads=[("decT", bs)], writes=[("decT", bs)])
                            S.op("act", lambda e, pg=pg: e.activation(out=qdT_s[bs][:, :], in_=pg, func=AF.Exp), reads=[kg], writes=[("qdT", bs)])
                            S.op("dve", lambda e, qTt=qTt: e.tensor_tensor(out=qdT_s[bs][:, :], in0=qdT_s[bs][:, :], in1=qTt, op=ALU.mult), reads=[("qdT", bs), ("cvb", gq)], writes=[("qdT", bs)])
                        if stage == 4 and SUB < 2:
                            return
                        yield
                        pkk, kkk = pq()
                        S.op("act", lambda e, kTt=kTt: e.activation(out=rc(kT2_s[bs][:, :]), in_=kTt, func=AF.Copy), reads=[("cvb", gk)], writes=[("kT2", bs)])
                        S.op("pe", lambda e, pkk=pkk, kTt=kTt: mm_c(e, pkk, kTt, kT2_s[bs][:, :], 1), reads=[("cvb", gk), ("kT2", bs)], writes=[kkk])
                        S.op("dve", lambda e, pkk=pkk, h=h: e.scalar_tensor_tensor(out=rr(Pm_s[bs][0][:, :]), in0=pkk, scalar=beta[:, h:h + 1], in1=dec_s[bs][:, :], op0=ALU.mult, op1=ALU.mult),
                             reads=[kkk, "beta", ("dec", bs)], writes=[("Pm", bs, 0)])
                        S.op("dve", lambda e: e.tensor_tensor(out=rr(Pm_s[bs][0][:, :]), in0=Pm_s[bs][0][:, :], in1=C("strictl"), op=ALU.mult), reads=[("Pm", bs, 0), "cst"], writes=[("Pm", bs, 0)])
                        if stage == 4 and SUB < 2.2:
                            return
                        if full:
                            yield
                            pa_, ka_ = pq()
                            S.op("pe", lambda e, pa_=pa_, kTt=kTt, qTt=qTt: e.matmul(pa_, kTt, qTt, start=True, stop=True), reads=[("cvb", gk), ("cvb", gq)], writes=[ka_])
                            S.op("dve", lambda e, pa_=pa_: e.tensor_tensor(out=atT_s[bs][:, :], in0=pa_, in1=decT_s[bs][:, :], op=ALU.mult), reads=[ka_, ("decT", bs)], writes=[("atT", bs)])
                            S.op("dve", lambda e: e.tensor_tensor(out=atT_s[bs][:, :], in0=atT_s[bs][:, :], in1=C("triu"), op=ALU.mult), reads=[("atT", bs), "cst"], writes=[("atT", bs)])
                        if stage == 4 and SUB < 2.4:
                            return
                        yield
                        p, k_ = pq()
                        S.op("pe", lambda e, p=p: trp(e, p, Pm_s[bs][0][:, :]), reads=[("Pm", bs, 0), "cst"], writes=[k_])
                        S.op("act", lambda e, p=p: e.activation(out=rr(Qm_s[bs][0][:, :]), in_=p, func=AF.Copy), reads=[k_], writes=[("Qm", bs, 0)])
                        S.op("dve", lambda e, p=p: e.scalar_tensor_tensor(out=rr(Rm_s[bs][0][:, :]), in0=p, scalar=-1.0, in1=C("ident"), op0=ALU.mult, op1=ALU.add), reads=[k_, "cst"], writes=[("Rm", bs, 0)])
                        cur = 0
                        if stage == 4 and SUB < 2.6:
                            return
                        for lev in range(1, 6):
                            nxt = 1 - cur
                            if lev < 5:
                                yield
                                p, k_ = pq()
                                S.op("pe", lambda e, p=p, cur=cur: mmr(e, p, Pm_s[bs][cur][:, :], Qm_s[bs][cur][:, :], start=True, stop=True),
                                     reads=[("Pm", bs, cur), ("Qm", bs, cur)], writes=[k_])
                                S.op("act", lambda e, p=p, nxt=nxt: e.activation(out=rr(Qm_s[bs][nxt][:, :]), in_=p, func=AF.Copy), reads=[k_], writes=[("Qm", bs, nxt)])
                            yield
                            p, k_ = pq()
                            S.op("pe", lambda e, p=p, cur=cur: mmr(e, p, Qm_s[bs][cur][:, :], Pm_s[bs][cur][:, :], start=True, stop=True),
                                 reads=[("Pm", bs, cur), ("Qm", bs, cur)], writes=[k_])
                            S.op("dve", lambda e, p=p, nxt=nxt: e.tensor_copy(out=rr(Pm_s[bs][nxt][:, :]), in_=p), reads=[k_], writes=[("Pm", bs, nxt)])
                            yield
                            p, k_ = pq()
                            S.op("pe", lambda e, p=p, cur=cur, nxt=nxt: mmr(e, p, Pm_s[bs][nxt][:, :], Rm_s[bs][cur][:, :], start=True, stop=True),
                                 reads=[("Pm", bs, nxt), ("Rm", bs, cur)], writes=[k_])
                            S.op("dve", lambda e, p=p, cur=cur, nxt=nxt: e.tensor_tensor(out=rr(Rm_s[bs][nxt][:, :]), in0=p, in1=Rm_s[bs][cur][:, :], op=ALU.add),
                                 reads=[k_, ("Rm", bs, cur)], writes=[("Rm", bs, nxt)])
                            cur = nxt
                        if stage == 4 and SUB < 2.8:
                            return
                        TT = Rm_s[bs][cur]
                        TTk = ("Rm", bs, cur)
                        yield
                        p, k_ = pq()
                        S.op("pe", lambda e, p=p, TT=TT: mmr(e, p, TT[:, :], bv_s[bs][:, :], start=True, stop=True), reads=[TTk, ("bv", bs)], writes=[k_])
                        S.op("act", lambda e, p=p: e.activation(out=ut_s[bs][:, :], in_=p, func=AF.Copy), reads=[k_], writes=[("ut", bs)])
                        yield
                        p, k_ = pq()
                        S.op("pe", lambda e, p=p, TT=TT: mmr(e, p, kbg_s[bs][:, :], TT[:, :], start=True, stop=True), reads=[TTk, ("kbg", bs)], writes=[k_])
                        S.op("act", lambda e, p=p: e.activation(out=rc(wT_s[bs][:, :]), in_=p, func=AF.Copy), reads=[k_], writes=[("wT", bs)])
                        if stage == 4 and SUB < 3:
                            return
                        if full:
                            po, ko = pfull()
                            po = po[:, 0:128]
                        for c in range(2):
                            cs = slice(c * 64, (c + 1) * 64)
                            yield
                            p, k_ = pq()
                            S.op("pe", lambda e, p=p, cs=cs, h=h: (mm_c(e, p, wT_s[bs][:, :], Sg[h][:, :], 2) if (R_CHAIN and (CH_MASK & 2)) else e.matmul(p[cs, :], wT_s[bs][:, cs], Sg[h][:, :], start=True, stop=True)), reads=[("wT", bs), ("Sg", h)], writes=[k_])
                            S.op("dve", lambda e, p=p, cs=cs: e.scalar_tensor_tensor(out=rc(vnew_s[bs][cs, :]), in0=p[cs, :], scalar=-1.0, in1=ut_s[bs][cs, :], op0=ALU.mult, op1=ALU.add),
                                 reads=[k_, ("ut", bs)], writes=[(("vnew", bs), c)])
                            if full:
                                S.op("pe", lambda e, po=po, cs=cs, h=h: e.matmul(po[cs, :], qdT_s[bs][:, cs], Sg[h][:, :], start=True, stop=False),
                                     reads=[("qdT", bs), ("Sg", h)], writes=[ko])
                                S.op("pe", lambda e, po=po, cs=cs: e.matmul(po[cs, :], atT_s[bs][cs, cs], vnew_s[bs][cs, :], start=False, stop=True),
                                     reads=[("atT", bs), (("vnew", bs), c)], writes=[ko])
                            yield
                            p2_, k2_ = pq()
                            S.op("pe", lambda e, p2_=p2_, cs=cs: mm_c(e, p2_, ktl_s[bs][cs, :], vnew_s[bs][cs, :], 4), reads=[("ktl", bs), (("vnew", bs), c)], writes=[k2_])
                            S.op("act", lambda e, c=c, h=h: e.activation(out=rc(Sg[h][:, :]), in_=Sg[h][:, :], func=AF.Copy, scale=glb_s[bs][:, c:c + 1]),
                                 reads=[("glb", bs), ("Sg", h)], writes=[("Sg", h)])
                            S.op("dve", lambda e, p2_=p2_, h=h: e.tensor_tensor(out=rc(Sg[h][:, :]), in0=p2_, in1=Sg[h][:, :], op=ALU.add),
                                 reads=[k2_, ("Sg", h)], writes=[("Sg", h)])
                        if full:
                            S.op("act", lambda e, po=po, h=h: e.activation(out=otok[:, h * 128:(h + 1) * 128], in_=po, func=AF.Copy), reads=[ko], writes=[("otok", h)])

                    for pair in (((0, 1), (2, 3)) if stage >= 4 else ()):
                        gens = [gdn_gen(hh, ii) for ii, hh in enumerate(pair)]
                        while gens:
                            for g_ in list(gens):
                                try:
                                    next(g_)
                                except StopIteration:
                                    gens.remove(g_)
                    if stage < 5:
                        continue
                    pbf, pkf = proj_tm(OF, 512)
                    S.op("act", lambda e, pbf=pbf: e.activation(out=fv[:, :], in_=pbf[:, :], func=AF.Sigmoid), reads=[pkf], writes=["fv"])
                    S.op("dve", lambda e: e.tensor_tensor(out=fv[:, :], in0=fv[:, :], in1=omlB[:, :], op=ALU.mult), reads=["fv", "omlB"], writes=["fv"])
                    S.op("dve", lambda e: e.tensor_tensor(out=fv[:, :], in0=fv[:, :], in1=lbB[:, :], op=ALU.add), reads=["fv", "lbB"], writes=["fv"])
                    S.op("act", lambda e: e.activation(out=ru(logf[:, :]), in_=fv[:, :], func=AF.Ln), reads=["fv"], writes=["logf"])
                    S.op("dve", lambda e: e.tensor_scalar(out=kh[:, :], in0=fv[:, :], scalar1=-1.0, scalar2=1.0, op0=ALU.mult, op1=ALU.add), reads=["fv"], writes=["kh"])
                    pbi, pki = proj_tm(OI, 512)
                    S.op("act", lambda e, pbi=pbi: e.activation(out=rc(vh[:, :]), in_=pbi[:, :], func=AF.Copy), reads=[pki], writes=["vh"])
                    pbr, pkr = pfull()
                    S.op("pe", lambda e, pbr=pbr: mm_u(e, pbr[:, :], CR("rev"), logf[:, :]), reads=["cstr", "logf"], writes=[pkr])
                    S.op("act", lambda e, pbr=pbr: e.activation(out=rc(ktail[:, :]), in_=pbr[:, :], func=AF.Exp), reads=[pkr], writes=["ktail"])
                    S.op("dve", lambda e: e.tensor_tensor(out=rc(ktail[:, :]), in0=ktail[:, :], in1=kh[:, :], op=ALU.mult), reads=["ktail", "kh"], writes=["ktail"])
                    if OWN0 <= ti < OWN0 + 2:
                        r0 = (ti - OWN0) * 128
                        dump("logf", logf[:, :], [256, 512], "logf", r0, r0 + 128)
                        dump("kh", kh[:, :], [256, 512], "kh", r0, r0 + 128)
                        dump("vh", vh[:, :], [256, 512], "vh", r0, r0 + 128)
                    def hg_gen(h, bs):
                        hs = slice(h * 128, (h + 1) * 128)
                        yield
                        p, k_ = pq()
                        S.op("pe", lambda e, p=p, hs=hs: mm_u(e, p[:, 0:2], logf[:, hs], CR("cind")[:, 0:2]), reads=["logf", "cstr"], writes=[k_])
                        S.op("act", lambda e, p=p, h=h: e.activation(out=ebl[:, h, :], in_=p[:, 0:2], func=AF.Exp), reads=[k_], writes=[("ebl", h)])
                        if full:
                            yield
                            p, k_ = pq()
                            S.op("pe", lambda e, p=p, hs=hs: mm_u(e, p, logf[:, hs], CR("triu")), reads=["logf", "cstr"], writes=[k_])
                            S.op("act", lambda e, p=p: e.activation(out=ebT_s[bs][:, :], in_=p, func=AF.Exp), reads=[k_], writes=[("ebT", bs)])
                            S.op("act", lambda e, p=p: e.activation(out=enbT_s[bs][:, :], in_=p, func=AF.Exp, scale=-1.0), reads=[k_], writes=[("enbT", bs)])
                            S.op("dve", lambda e, h=h: e.tensor_tensor(out=qtT_s[bs][:, :], in0=qbT[:, h, tsl], in1=ebT_s[bs][:, :], op=ALU.mult), reads=[("qbT", h), ("ebT", bs)], writes=[("qtT", bs)])
                            S.op("dve", lambda e, h=h: e.scalar_tensor_tensor(out=ktT_s[bs][:, :], in0=fT[:, h, tsl], scalar=omlv[:, h:h + 1], in1=enbT_s[bs][:, :], op0=ALU.mult, op1=ALU.mult),
                                 reads=[("fT", h), "omlv", ("enbT", bs)], writes=[("ktT", bs)])
                            yield
                            p, k_ = pq()
                            S.op("pe", lambda e, p=p: e.matmul(p, ktT_s[bs][:, :], qtT_s[bs][:, :], start=True, stop=True), reads=[("ktT", bs), ("qtT", bs)], writes=[k_])
                            S.op("dve", lambda e, p=p: e.tensor_tensor(out=AT_s[bs][:, :], in0=p, in1=C("triu"), op=ALU.mult), reads=[k_, "cst"], writes=[("AT", bs)])
                            po, ko = pfull()
                            po = po[:, 0:128]
                        for c in range(2):
                            cs = slice(c * 64, (c + 1) * 64)
                            if full:
                                S.op("pe", lambda e, po=po, cs=cs, h=h: e.matmul(po[cs, :], qtT_s[bs][:, cs], Sh[h][:, :], start=True, stop=False),
                                     reads=[("qtT", bs), ("Sh", h)], writes=[ko])
                                S.op("pe", lambda e, po=po, cs=cs, hs=hs: e.matmul(po[cs, :], AT_s[bs][cs, cs], vh[cs, hs], start=False, stop=True),
                                     reads=[("AT", bs), "vh"], writes=[ko])
                            yield
                            p2_, k2_ = pq()
                            S.op("pe", lambda e, p2_=p2_, cs=cs, hs=hs: mm_c(e, p2_, ktail[cs, hs], vh[cs, hs], 8), reads=["ktail", "vh"], writes=[k2_])
                            S.op("act", lambda e, c=c, h=h: e.activation(out=Sh[h][:, :], in_=Sh[h][:, :], func=AF.Copy, scale=ebl[:, h, c:c + 1]),
                                 reads=[("ebl", h), ("Sh", h)], writes=[("Sh", h)])
                            S.op("dve", lambda e, p2_=p2_, h=h: e.tensor_tensor(out=Sh[h][:, :], in0=p2_, in1=Sh[h][:, :], op=ALU.add),
                                 reads=[k2_, ("Sh", h)], writes=[("Sh", h)])
                        if full:
                            S.op("act", lambda e, po=po, h=h: e.activation(out=otok[:, 512 + h * 128:512 + (h + 1) * 128], in_=po, func=AF.Copy), reads=[ko], writes=[("otok", 4 + h)])
                    for pair in ((0, 1), (2, 3)):
                        gens = [hg_gen(hh, ii) for ii, hh in enumerate(pair)]
                        while gens:
                            for g_ in list(gens):
                                try:
                                    next(g_)
                                except StopIteration:
                                    gens.remove(g_)

                    if not full or stage < 6:
                        continue
                    oi = ti - OWN0
                    ok_all = [("otok", i) for i in range(8)]
                    dump("otok", otok[:, :], [NOWN * 128, D], ok_all, oi * 128, (oi + 1) * 128)
                    for h in range(4):
                        S.op("act", lambda e, h=h: e.activation(out=sq[:, 0:128], in_=otok[:, h * 128:(h + 1) * 128], func=AF.Square, accum_out=stat[:, 2 + h:3 + h]),
                             reads=[("otok", h)], writes=["sq", ("statA", h)])
                    S.op("dve", lambda e: e.tensor_scalar(out=stat[:, 2:6], in0=stat[:, 2:6], scalar1=1.0 / 128, scalar2=EPS, op0=ALU.mult, op1=ALU.add),
                         reads=[("statA", h) for h in range(4)], writes=["statA"])
                    S.op("act", lambda e: e.activation(out=stat[:, 2:6], in_=stat[:, 2:6], func=AF.Sqrt), reads=["statA"], writes=["statA"])
                    S.op("dve", lambda e: e.reciprocal(out=stat[:, 2:6], in_=stat[:, 2:6]), reads=["statA"], writes=["statA"])
                    for h in range(4):
                        S.op("dve", lambda e, h=h: e.scalar_tensor_tensor(out=otok[:, h * 128:(h + 1) * 128], in0=otok[:, h * 128:(h + 1) * 128], scalar=stat[:, 2 + h:3 + h],
                                                                          in1=gnw[:, :], op0=ALU.mult, op1=ALU.mult),
                             reads=[("otok", h), "statA", "gnw"], writes=[("otok", h)])
                    S.op("act", lambda e: e.activation(out=sq[:, 0:512], in_=otok[:, 512:1024], func=AF.Square, accum_out=stat[:, 6:7]),
                         reads=ok_all[4:], writes=["sq", "statB"])
                    S.op("dve", lambda e: e.tensor_scalar(out=stat[:, 6:7], in0=stat[:, 6:7], scalar1=1.0 / 512, scalar2=EPS, op0=ALU.mult, op1=ALU.add),
                         reads=["statB"], writes=["statB"])
                    S.op("act", lambda e: e.activation(out=stat[:, 6:7], in_=stat[:, 6:7], func=AF.Sqrt), reads=["statB"], writes=["statB"])
                    S.op("dve", lambda e: e.reciprocal(out=stat[:, 6:7], in_=stat[:, 6:7]), reads=["statB"], writes=["statB"])
                    S.op("dve", lambda e: e.scalar_tensor_tensor(out=otok[:, 512:1024], in0=otok[:, 512:1024], scalar=stat[:, 6:7], in1=hnw[:, :], op0=ALU.mult, op1=ALU.mult),
                         reads=ok_all[4:] + ["statB", "hnw"], writes=ok_all[4:])
                    S.op("dve", lambda e: e.tensor_tensor(out=ob[:, :], in0=otok[:, :], in1=gateS[:, :], op=ALU.mult),
                         reads=ok_all + ["gateA", "gateB"], writes=["ob"])
                    for kc in range(8):
                        S.op("pe", lambda e, kc=kc: e.transpose(out=tbank_bf[:, kc * 128:(kc + 1) * 128], in_=ob[:, kc * 128:(kc + 1) * 128], identity=cst_bf[:, :]),
                             reads=["ob", "cst_bf"], writes=[TB])
                    S.op("act", lambda e: e.activation(out=oT[:, :, :], in_=tbank_bf.rearrange("p (k t) -> p k t", k=8), func=AF.Copy), reads=[TB], writes=["oT"])
                    xb = xt[ti % 2]
                    xk = ("xt", ti % 2)
                    dma_load(xb[:, :], xseq[ti * 128:(ti + 1) * 128, :], [xk])
                    for half in range(2):
                        wsb, wk = wload(wout_bf, "wout_bf", half * 512, 512)
                        pb, pk = pfull()
                        for kc in range(8):
                            S.op("pe", lambda e, kc=kc, pb=pb, wsb=wsb: e.matmul(pb[:, :], oT[:, kc, :], wsb[:, kc, :], start=(kc == 0), stop=(kc == 7)),
                                 reads=["oT", wk], writes=[pk])
                        hsl = slice(half * 512, (half + 1) * 512)
                        S.op("dve", lambda e, pb=pb, hsl=hsl: e.tensor_tensor(out=x1[:, hsl], in0=pb[:, :], in1=G1[:, hsl], op=ALU.mult), reads=[pk, "modbc"], writes=[("x1", half)])
                        S.op("dve", lambda e, hsl=hsl, xb=xb: e.tensor_tensor(out=x1[:, hsl], in0=x1[:, hsl], in1=xb[:, hsl], op=ALU.add), reads=[("x1", half), xk], writes=[("x1", half)])
                    x1k = [("x1", 0), ("x1", 1)]
                    dump("x1all", x1[:, :], [NOWN * 128, D], x1k, oi * 128, (oi + 1) * 128)
                    S.op("sp", lambda e, oi=oi: e.dma_start(out=x1scr[oi * 128:(oi + 1) * 128, :], in_=x1[:, :]), reads=x1k, dma="x1st")
                    S.op("act", lambda e: e.activation(out=sq[:, :], in_=x1[:, :], func=AF.Square, accum_out=stat[:, 8:9]), reads=x1k, writes=["sq", "stat8"])
                    S.op("dve", lambda e: e.tensor_scalar(out=stat[:, 8:9], in0=stat[:, 8:9], scalar1=1.0 / D, scalar2=EPS, op0=ALU.mult, op1=ALU.add), reads=["stat8"], writes=["stat8"])
                    S.op("act", lambda e: e.activation(out=stat[:, 8:9], in_=stat[:, 8:9], func=AF.Sqrt), reads=["stat8"], writes=["stat8"])
                    S.op("dve", lambda e: e.reciprocal(out=stat[:, 8:9], in_=stat[:, 8:9]), reads=["stat8"], writes=["stat8"])
                    S.op("dve", lambda e: e.scalar_tensor_tensor(out=h2f[:, :], in0=x1[:, :], scalar=stat[:, 8:9], in1=SC2, op0=ALU.mult, op1=ALU.mult),
                         reads=x1k + ["stat8", "modbc"], writes=["h2f"])
                    S.op("dve", lambda e: e.tensor_tensor(out=h2f[:, :], in0=h2f[:, :], in1=SH2, op=ALU.add), reads=["h2f", "modbc"], writes=["h2f"])
                    S.op("act", lambda e: e.activation(out=h2b[:, :], in_=h2f[:, :], func=AF.Copy), reads=["h2f"], writes=["h2b"])
                    for kc in range(8):
                        S.op("pe", lambda e, kc=kc: e.transpose(out=tbank_bf[:, kc * 128:(kc + 1) * 128], in_=h2b[:, kc * 128:(kc + 1) * 128], identity=cst_bf[:, :]),
                             reads=["h2b", "cst_bf"], writes=[TB])
                    S.op("act", lambda e: e.activation(out=h2Tt[:, :], in_=tbank_bf, func=AF.Copy), reads=[TB], writes=["h2Tt"])
                    S.op("sp", lambda e, oi=oi: e.dma_start(out=h2scr[oi], in_=h2Tt[:, :]), reads=["h2Tt"], dma="h2st")
                    for kc in range(8):
                        p, k_ = pq()
                        S.op("pe", lambda e, p=p, kc=kc: trp(e, p, h2f[:, kc * 128:(kc + 1) * 128]), reads=["h2f", "cst"], writes=[k_])
                        S.op("act", lambda e, p=p, kc=kc: e.activation(out=h2Tf[:, kc, :], in_=p, func=AF.Copy), reads=[k_], writes=[("h2Tf", kc)])
                    p, k_ = pq()
                    for kc in range(8):
                        S.op("pe", lambda e, p=p, kc=kc: e.matmul(p[:, 0:NE], h2Tf[:, kc, :], wr[:, kc, :], start=(kc == 0), stop=(kc == 7)),
                             reads=[("h2Tf", kc), "wr"], writes=[k_])
                    if stage < 7:
                        continue
                    S.op("act", lambda e, p=p: e.activation(out=rsc[:, :], in_=p[:, 0:NE], func=AF.Sigmoid), reads=[k_], writes=["rsc"])
                    S.op("dve", lambda e: e.tensor_tensor(out=rsel[:, :], in0=rsc[:, :], in1=rbb[:, :], op=ALU.add), reads=["rsc", "rbb"], writes=["rsel"])
                    rs3 = rsel[:, :].rearrange("p (g k) -> p g k", g=8)
                    rt3 = rtmp[:, :].rearrange("p (g k) -> p g k", g=8)
                    S.op("dve", lambda e: e.tensor_reduce(out=rg8[:, :, 0], in_=rs3, axis=AX.X, op=ALU.max), reads=["rsel"], writes=["rg8a"])
                    S.op("dve", lambda e: e.tensor_tensor(out=rt3, in0=rs3, in1=rg8[:, :, 0:1].to_broadcast([128, 8, 8]), op=ALU.is_equal),
                         reads=["rsel", "rg8a"], writes=["rtmp"])
                    S.op("dve", lambda e: e.scalar_tensor_tensor(out=rtmp[:, :], in0=rtmp[:, :], scalar=-1.0e4, in1=rsel[:, :], op0=ALU.mult, op1=ALU.add),
                         reads=["rtmp", "rsel"], writes=["rtmp"])
                    S.op("dve", lambda e: e.tensor_reduce(out=rg8[:, :, 1], in_=rt3, axis=AX.X, op=ALU.max), reads=["rtmp"], writes=["rg8b"])
                    S.op("dve", lambda e: e.tensor_tensor(out=rg8[:, :, 2], in0=rg8[:, :, 0], in1=rg8[:, :, 1], op=ALU.add), reads=["rg8a", "rg8b"], writes=["rg8c"])
                    S.op("dve", lambda e: e.tensor_copy(out=stat[:, 8:16], in_=rg8[:, :, 2]), reads=["rg8c", "stat8"], writes=["stat8"])
                    S.op("dve", lambda e: e.max(out=rtmp[:, 0:8], in_=stat[:, 8:16]), reads=["stat8"], writes=["rtmp"])
                    S.op("dve", lambda e: e.tensor_scalar(out=rg8[:, :, 3], in0=rg8[:, :, 2], scalar1=rtmp[:, 3:4], scalar2=None, op0=ALU.is_ge), reads=["rg8c", "rtmp"], writes=["rg8d"])
                    S.op("dve", lambda e: e.tensor_tensor(out=rt3, in0=rs3, in1=rg8[:, :, 3:4].to_broadcast([128, 8, 8]), op=ALU.mult), reads=["rsel", "rg8d"], writes=["rtmp"])
                    S.op("dve", lambda e: e.tensor_scalar(out=rg8[:, :, 2], in0=rg8[:, :, 3], scalar1=1.0e4, scalar2=-1.0e4, op0=ALU.mult, op1=ALU.add), reads=["rg8d"], writes=["rg8c"])
                    S.op("dve", lambda e: e.tensor_tensor(out=rt3, in0=rt3, in1=rg8[:, :, 2:3].to_broadcast([128, 8, 8]), op=ALU.add), reads=["rtmp", "rg8c"], writes=["rtmp"])
                    S.op("dve", lambda e: e.max(out=stat[:, 8:16], in_=rtmp[:, :]), reads=["rtmp"], writes=["stat8"])
                    S.op("dve", lambda e: e.tensor_scalar(out=rsel[:, :], in0=rtmp[:, :], scalar1=stat[:, 15:16], scalar2=None, op0=ALU.is_ge), reads=["rtmp", "stat8"], writes=["rsel"])
                    S.op("dve", lambda e: e.tensor_tensor(out=rsel[:, :], in0=rsel[:, :], in1=rsc[:, :], op=ALU.mult), reads=["rsel", "rsc"], writes=["rsel"])
                    S.op("dve", lambda e: e.tensor_reduce(out=stat[:, 7:8], in_=rsel[:, :], axis=AX.X, op=ALU.add), reads=["rsel"], writes=["stat7"])
                    S.op("dve", lambda e: e.reciprocal(out=stat[:, 7:8], in_=stat[:, 7:8]), reads=["stat7"], writes=["stat7"])
                    S.op("dve", lambda e, oi=oi: e.tensor_scalar(out=gates[:, oi, 0:NE], in0=rsel[:, :], scalar1=stat[:, 7:8], scalar2=2.5, op0=ALU.mult, op1=ALU.mult),
                         reads=["rsel", "stat7"], writes=["gates"])
                    S.op("dve", lambda e, oi=oi: e.memset(gates[:, oi, NE:NE + 1], 1.0), writes=["gates"])
                    dump("gatesall", gates[:, oi, :], [NOWN * 128, NE + 1], "gates", oi * 128, (oi + 1) * 128)
            S.emit()

        pbk = ExitStack()
        with pbk:
            acc = sb(pbk, "acc", [128, NOWN, D])
            h2T = sb(pbk, "h2T", [128, 8, NOWN * 128], BF16)
            for tl in range(NOWN):
                dma_load(h2T[:, :, tl * 128:(tl + 1) * 128], h2scr[tl].rearrange("p (k t) -> p k t", k=8), ["h2T"], rkeys=[])
            for i in range(NOWN):
                S.op("pool", lambda e, i=i: e.memset(acc[:, i, :], 0.0), writes=[("acc", i)])
            NSLOT = 3
            wg = [sb(pbk, "wg%d" % i, [128, 8, 256], BF16) for i in range(NSLOT)]
            wu = [sb(pbk, "wu%d" % i, [128, 8, 256], BF16) for i in range(NSLOT)]
            wd = [sb(pbk, "wd%d" % i, [128, 2, D], BF16) for i in range(NSLOT)]
            actT = [sb(pbk, "actT%d" % i, [128, 2, NOWN * 128], BF16) for i in range(2)]
            sg = [sb(pbk, "sg%d" % i, [128, 512]) for i in range(2)]
            sgi = [0]
            pending = []
            for ex in range(nex):
                sl = ex % NSLOT
                dma_load(wg[sl][:, :, :], wg_bf[ex].rearrange("(kc p) n -> p kc n", p=128), [("wg", sl)], rkeys=["wg_bf"])
                dma_load(wu[sl][:, :, :], wu_bf[ex].rearrange("(kc p) n -> p kc n", p=128), [("wu", sl)], rkeys=["wu_bf"])
                dma_load(wd[sl][:, :, :], wd_bf[ex].rearrange("(fc p) n -> p fc n", p=128), [("wd", sl)], rkeys=["wd_bf"])
                aT = actT[ex % 2]
                aK = ("actT", ex % 2)
                for nb in range(NOWN // 4):
                    nsl = slice(nb * 512, (nb + 1) * 512)
                    for fm in range(2):
                        fsl = slice(fm * 128, (fm + 1) * 128)
                        b0 = (2 * (nb * 2 + fm)) % 4
                        pg_, kg_ = banks[b0], ("bank", b0)
                        pu_, ku_ = banks[b0 + 1], ("bank", b0 + 1)
                        for kc in range(8):
                            S.op("pe", lambda e, kc=kc, pg_=pg_, sl=sl, fsl=fsl, nsl=nsl: e.matmul(pg_[:, :], wg[sl][:, kc, fsl], h2T[:, kc, nsl], start=(kc == 0), stop=(kc == 7)),
                                 reads=[("wg", sl), "h2T"], writes=[kg_])
                        for kc in range(8):
                            S.op("pe", lambda e, kc=kc, pu_=pu_, sl=sl, fsl=fsl, nsl=nsl: e.matmul(pu_[:, :], wu[sl][:, kc, fsl], h2T[:, kc, nsl], start=(kc == 0), stop=(kc == 7)),
                                 reads=[("wu", sl), "h2T"], writes=[ku_])
                        sgb = sg[sgi[0] % 2]
                        sgk = ("sg", sgi[0] % 2)
                        sgi[0] += 1
                        S.op("act", lambda e, pg_=pg_, sgb=sgb: e.activation(out=sgb[:, :], in_=pg_[:, :], func=AF.Silu), reads=[kg_], writes=[sgk])
                        S.op("dve", lambda e, pu_=pu_, sgb=sgb, aT=aT, fm=fm, nsl=nsl: e.tensor_tensor(out=aT[:, fm, nsl], in0=pu_[:, :], in1=sgb[:, :], op=ALU.mult),
                             reads=[ku_, sgk], writes=[aK])
                        for _ in range(4):
                            if pending:
                                pending.pop(0)()
                def mk_step(tl, half, aT=aT, aK=aK, sl=sl, ex=ex):
                    def step():
                        tsl = slice(tl * 128, (tl + 1) * 128)
                        bi = 4 + (tl * 2 + half) % 4
                        po_, ko_ = banks[bi], ("bank", bi)
                        for fm in range(2):
                            S.op("pe", lambda e, po_=po_, aT=aT, fm=fm, tsl=tsl, sl=sl, half=half: e.matmul(po_[:, :], aT[:, fm, tsl], wd[sl][:, fm, half * 512:(half + 1) * 512],
                                                                                                              start=(fm == 0), stop=(fm == 1)),
                                 reads=[aK, ("wd", sl)], writes=[ko_])
                        hsl = slice(half * 512, (half + 1) * 512)
                        S.op("dve", lambda e, po_=po_, tl=tl, ex=ex, hsl=hsl: e.scalar_tensor_tensor(out=acc[:, tl, hsl], in0=po_[:, :], scalar=gates[:, tl, ex:ex + 1], in1=acc[:, tl, hsl],
                                                                                                      op0=ALU.mult, op1=ALU.add),
                             reads=[ko_, "gates", ("acc", tl)], writes=[("acc", tl)])
                    return step
                while pending:
                    pending.pop(0)()
                pending.extend(mk_step(tl, half) for tl in range(NOWN) for half in range(2))
            while pending:
                pending.pop(0)()
            for tl in range(NOWN):
                dump("moe", acc[:, tl, :], [NOWN * 128, D], ("acc", tl), tl * 128, (tl + 1) * 128)
            xr = [sb(pbk, "xr%d" % i, [128, D]) for i in range(2)]
            fsq = sb(pbk, "fsq", [128, D])
            fst = sb(pbk, "fst", [128, 2])
            for tl in range(NOWN):
                xb = xr[tl % 2]
                xk = ("xr", tl % 2)
                S.op("sp", lambda e, xb=xb, tl=tl: e.dma_start(out=xb[:, :], in_=x1scr[tl * 128:(tl + 1) * 128, :]), reads=[], writes=[xk], dma="xr%d" % (tl % 2))
                S.op("dve", lambda e, tl=tl: e.tensor_tensor(out=acc[:, tl, :], in0=acc[:, tl, :], in1=G2, op=ALU.mult), reads=[("acc", tl), "modbc"], writes=[("acc", tl)])
                S.op("dve", lambda e, tl=tl, xb=xb: e.tensor_tensor(out=acc[:, tl, :], in0=acc[:, tl, :], in1=xb[:, :], op=ALU.add), reads=[("acc", tl), xk], writes=[("acc", tl)])
                S.op("act", lambda e, tl=tl: e.activation(out=fsq[:, :], in_=acc[:, tl, :], func=AF.Square, accum_out=fst[:, 0:1]), reads=[("acc", tl)], writes=["fsq", "fst"])
                S.op("dve", lambda e: e.tensor_scalar(out=fst[:, 1:2], in0=fst[:, 0:1], scalar1=1.0 / D, scalar2=EPS, op0=ALU.mult, op1=ALU.add), reads=["fst"], writes=["fst1"])
                S.op("act", lambda e: e.activation(out=fst[:, 1:2], in_=fst[:, 1:2], func=AF.Sqrt), reads=["fst1"], writes=["fst1"])
                S.op("dve", lambda e: e.reciprocal(out=fst[:, 1:2], in_=fst[:, 1:2]), reads=["fst1"], writes=["fst1"])
                S.op("dve", lambda e, tl=tl: e.scalar_tensor_tensor(out=acc[:, tl, :], in0=acc[:, tl, :], scalar=fst[:, 1:2], in1=fwbc[:, :], op0=ALU.mult, op1=ALU.mult),
                     reads=[("acc", tl), "fst1", "fwbc"], writes=[("acc", tl)])
                S.op("sp", lambda e, tl=tl: e.dma_start(out=y[tl * 128:(tl + 1) * 128, :], in_=acc[:, tl, :]), reads=[("acc", tl)], dma="yst")
            fin = [("d", nm, v) for nm, v in S.dma_cnt.items()]
            S.ins["sp"].append(dict(fn=None, waits=set(fin), tok=None, dma=None, idx=S.done["sp"] + len(S.ins["sp"])))
            S.emit()
    if os.environ.get('SCHED_DEBUG'):
        print('cbase', S.cbase, 'dma', S.dma_cnt, 'done', S.done)
    return nc, dbg_outs


_CACHE = {}


def prep_inputs(inp, dne=NE):
    f = lambda a: np.ascontiguousarray(np.asarray(a, dtype=np.float32))
    x = f(inp["x"])
    c = f(inp["c"])
    conv_w = f(inp["conv_w"])[0]
    convw = conv_w.reshape(4, 12, 128).transpose(2, 1, 0).reshape(128, 48)
    hg_lb = f(inp["hg_lb"])
    lbcol = np.concatenate([hg_lb[0].reshape(4, 128).T, hg_lb[1].reshape(4, 128).T], axis=1)
    shared = {
        "w_ada": f(inp["w_ada"])[0], "b_ada": f(inp["b_ada"])[0], "norm1_w": f(inp["norm1_w"])[0],
        "w_in": f(inp["w_in"])[0], "convw": f(convw), "a_log": f(inp["gdn_a_log"])[0], "dt_bias": f(inp["gdn_dt_bias"])[0],
        "gdn_nw": f(inp["gdn_norm_w"])[0], "lbcol": f(lbcol), "hg_lb": hg_lb, "hg_nw": f(inp["hg_norm_w"])[0],
        "w_out": f(inp["w_out"])[0], "norm2_w": f(inp["norm2_w"])[0], "w_router": f(inp["w_router"])[0],
        "router_bias": f(inp["router_bias"])[0], "w_gate": f(inp["w_gate"])[0][:dne], "w_up": f(inp["w_up"])[0][:dne],
        "w_down": f(inp["w_down"])[0][:dne], "ws_gate": f(inp["ws_gate"])[0], "ws_up": f(inp["ws_up"])[0],
        "ws_down": f(inp["ws_down"])[0], "final_w": f(inp["final_norm_w"]), "consts": CONST_ARR,
    }
    maps = []
    for core in range(8):
        b, j = core // 4, core % 4
        nreal = (j + 1) * 2048
        xs = np.zeros((T, D), np.float32)
        xs[T - nreal:] = x[b, :nreal]
        m = np.zeros(T, np.float32)
        m[T - nreal:] = 1.0
        d = dict(shared)
        d["xseq"] = xs
        d["tmask"] = np.ascontiguousarray(m.reshape(NT, 128).T)
        d["cvec"] = np.ascontiguousarray(c[b].reshape(8, 128).T)
        maps.append(d)
    return maps


def kernel(**inputs):
    if "nc" not in _CACHE:
        _CACHE["nc"] = build()[0]
    nc = _CACHE["nc"]
    maps = prep_inputs(inputs)
    res = run_bass_kernel_spmd(nc, maps, core_ids=list(range(8)))
    out = np.zeros((2, T, D), np.float32)
    for core in range(8):
        b, j = core // 4, core % 4
        out[b, j * 2048:(j + 1) * 2048] = res.results[core]["y"]
    return out
```
